# Optimizing a Trainium2 kernel written in Bass

```python
import math
import jax
import jax.numpy as jnp
from jax import lax
import numpy as np

D_MODEL = 1024
BATCH = 16
SEQ = 2048
DEPTH = 2

GRID_W = 64
ROPE_THETA = 10000.0
Q_BLOCK = 128
LN_EPS = 1e-5
RMS_EPS = 1e-6
MIX_WIDTH = D_MODEL
GW = MIX_WIDTH // 4
HEAD_DIM = 64
ATT_HEADS = GW // HEAD_DIM
ATT_KV_HEADS = 2
RWKV_HEADS = GW // HEAD_DIM
RWKV_DECAY_LORA = 32
RWKV_AAA_LORA = 32
RWKV_GATE_LORA = 64
RWKV_GN_EPS = 64e-5
RWKV_DECAY_SCALE = math.exp(-0.5)
HGRN_HEADS = 4
HGRN_EXPAND = 64
HGRN_VDIM = GW // HGRN_HEADS
HGRN_CHUNK = 16
MLA_HEADS = 4
MLA_NOPE = 64
MLA_ROPE = 32
MLA_V = GW // MLA_HEADS
MLA_Q_RANK = 192
MLA_KV_RANK = 128
MEM_TOKENS = 256
MEM_HEADS = 4
MOE_GROUPS = 4
MOE_PER_GROUP = 8
MOE_EXPERTS = MOE_GROUPS * MOE_PER_GROUP
MOE_TOPK = 2
MOE_HIDDEN = 256
MOE_BLOCK = 128
DN_ALPHA = (2 * DEPTH) ** 0.25
DN_BETA = (8 * DEPTH) ** -0.25
ATT_IN = ATT_HEADS * HEAD_DIM + 2 * ATT_KV_HEADS * HEAD_DIM
RWKV_IN = 3 * GW + 2 * RWKV_DECAY_LORA + 2 * RWKV_AAA_LORA + RWKV_GATE_LORA
HGRN_IN = 3 * HGRN_HEADS * HGRN_EXPAND + 2 * HGRN_HEADS * HGRN_VDIM
MLA_IN = MLA_Q_RANK + MLA_KV_RANK + MLA_ROPE
IN_WIDTH = ATT_IN + RWKV_IN + HGRN_IN + MLA_IN

kernel_name = 'hybrid_parallel_heads_hmoe_encoder'


def split_sizes(x, sizes):
    idx, acc = [], 0
    for s in sizes[:-1]:
        acc += s
        idx.append(acc)
    return jnp.split(x, idx, axis=-1)


def layer_norm(x, g, b):
    xf = x.astype(jnp.float32)
    mu = jnp.mean(xf, -1, keepdims=True)
    var = jnp.mean(jnp.square(xf - mu), -1, keepdims=True)
    return ((xf - mu) * lax.rsqrt(var + LN_EPS) * g + b).astype(x.dtype)


def rms_norm(x, g):
    xf = x.astype(jnp.float32)
    return (xf * lax.rsqrt(jnp.mean(xf * xf, -1, keepdims=True) + RMS_EPS) * g).astype(x.dtype)


def rope_tables(pos, dim, dtype):
    inv = ROPE_THETA ** (-jnp.arange(0, dim, 2, dtype=jnp.float32) / dim)
    ang = pos[:, None] * inv[None, :]
    return jnp.cos(ang).astype(dtype), jnp.sin(ang).astype(dtype)


def apply_rope_1d(x, cos, sin):
    x1, x2 = jnp.split(x, 2, axis=-1)
    c, s = cos[:, None, :], sin[:, None, :]
    return jnp.concatenate([x1 * c - x2 * s, x1 * s + x2 * c], axis=-1)


def axial_rope(x, row_pos, col_pos):
    half = x.shape[-1] // 2
    cr, sr = rope_tables(row_pos, half, x.dtype)
    cc, sc = rope_tables(col_pos, half, x.dtype)
    return jnp.concatenate([apply_rope_1d(x[..., :half], cr, sr),
                            apply_rope_1d(x[..., half:], cc, sc)], axis=-1)


def block_attention(q, k, v, scale):
    B, S, H, dk = q.shape
    Hkv, dv = k.shape[2], v.shape[-1]
    G = H // Hkv
    nb = S // Q_BLOCK
    qb = q.reshape(B, nb, Q_BLOCK, Hkv, G, dk).transpose(1, 0, 2, 3, 4, 5)

    def attend(qblk):
        s = jnp.einsum('bqkgd,bskd->bkgqs', qblk, k, preferred_element_type=jnp.float32) * scale
        p = jax.nn.softmax(s, axis=-1).astype(v.dtype)
        return jnp.einsum('bkgqs,bskd->bqkgd', p, v)

    o = lax.map(attend, qb)
    return o.transpose(1, 0, 2, 3, 4, 5).reshape(B, S, H * dv)


def gqa_axial_mixer(z, q_norm, k_norm, row_pos, col_pos):
    B, S, _ = z.shape
    q, k, v = split_sizes(z, (ATT_HEADS * HEAD_DIM, ATT_KV_HEADS * HEAD_DIM, ATT_KV_HEADS * HEAD_DIM))
    q = q.reshape(B, S, ATT_HEADS, HEAD_DIM)
    k = k.reshape(B, S, ATT_KV_HEADS, HEAD_DIM)
    v = v.reshape(B, S, ATT_KV_HEADS, HEAD_DIM)
    q = axial_rope(rms_norm(q, q_norm), row_pos, col_pos)
    k = axial_rope(rms_norm(k, k_norm), row_pos, col_pos)
    return block_attention(q, k, v, HEAD_DIM ** -0.5)


def rwkv7_bidir(z, mu_prev, mu_next, w0, w2, a0, a2, g2, k_k, k_a, r_k, gn_g, gn_b):
    B, S, _ = z.shape
    H, N = RWKV_HEADS, HEAD_DIM
    zf = z.astype(jnp.float32)
    z_prev = jnp.pad(zf, ((0, 0), (1, 0), (0, 0)))[:, :-1]
    z_next = jnp.pad(zf, ((0, 0), (0, 1), (0, 0)))[:, 1:]
    zf = zf + mu_prev * (z_prev - zf) + mu_next * (z_next - zf)
    r, k, v, w_lo, a_lo, g_lo = split_sizes(
        zf, (GW, GW, GW, 2 * RWKV_DECAY_LORA, 2 * RWKV_AAA_LORA, RWKV_GATE_LORA))
    w_lo = w_lo.reshape(B, S, 2, RWKV_DECAY_LORA)
    a_lo = a_lo.reshape(B, S, 2, RWKV_AAA_LORA)
    w_raw = w0 + jnp.einsum('bsdr,drc->bsdc', jnp.tanh(w_lo), w2)
    decay = jnp.exp(-RWKV_DECAY_SCALE * jax.nn.sigmoid(w_raw))
    a = jax.nn.sigmoid(a0 + jnp.einsum('bsdr,drc->bsdc', a_lo, a2))
    g = jax.nn.sigmoid(g_lo) @ g2
    kk = (k * k_k).reshape(B, S, H, N)
    kk = kk / jnp.maximum(jnp.sqrt(jnp.sum(kk * kk, -1, keepdims=True)), 1e-12)
    k_dir = k[:, :, None, :] * (1.0 + (a - 1.0) * k_a)

    def heads(t):
        return t.reshape(t.shape[:-1] + (H, N))

    decay, a, k_dir, r, v = heads(decay), heads(a), heads(k_dir), heads(r), heads(v)
    a_kk = a * kk[:, :, None]

    def per_dir(t):
        return jnp.stack([t[:, :, 0], t[:, ::-1, 1]], 0).transpose(2, 0, 1, 3, 4)

    def both_dirs(t):
        return jnp.stack([t, t[:, ::-1]], 0).transpose(2, 0, 1, 3, 4)

    seq_in = (both_dirs(r), per_dir(decay), per_dir(k_dir), both_dirs(v), both_dirs(kk), per_dir(a_kk))

    def step(state, inp):
        r_t, w_t, k_t, v_t, kk_t, akk_t = inp
        sa = jnp.einsum('dbhij,dbhj->dbhi', state, kk_t)
        state = (state * w_t[..., None, :] - sa[..., None] * akk_t[..., None, :]
                 + v_t[..., None] * k_t[..., None, :])
        return state, jnp.einsum('dbhij,dbhj->dbhi', state, r_t)

    state0 = jnp.zeros((2, B, H, N, N), jnp.float32)
    _, ys = lax.scan(step, state0, seq_in)
    ys = ys.transpose(1, 2, 0, 3, 4)
    y = ys[0] + ys[1, :, ::-1]
    mu = jnp.mean(y, -1, keepdims=True)
    var = jnp.mean(jnp.square(y - mu), -1, keepdims=True)
    y = ((y - mu) * lax.rsqrt(var + RWKV_GN_EPS)).reshape(B, S, GW) * gn_g + gn_b
    bonus = jnp.sum(r[:, :, None] * k_dir * r_k, axis=(2, 4))
    out = (y + (bonus[..., None] * v).reshape(B, S, GW)) * g
    return out.astype(z.dtype)


def hgrn2_chunk(q, log_f, v):
    B, S, H, Dk = q.shape
    Dv = v.shape[-1]
    C = HGRN_CHUNK
    nc = S // C
    k = -jnp.expm1(log_f)
    q, log_f, k, v = [t.reshape(B, nc, C, H, t.shape[-1]) for t in (q, log_f, k, v)]
    G = jnp.cumsum(log_f, axis=2)
    mask = jnp.tril(jnp.ones((C, C), bool))[None, None, :, :, None, None]
    pair_decay = jnp.exp(jnp.where(mask, G[:, :, :, None] - G[:, :, None, :], -jnp.inf))
    A = jnp.einsum('bnthd,bntshd,bnshd->bnhts', q, pair_decay, k)
    intra = jnp.einsum('bnhts,bnshe->bnthe', A, v)
    G_last = G[:, :, -1]
    U = jnp.einsum('bnshd,bnshe->bnhde', k * jnp.exp(G_last[:, :, None] - G), v)
    chunk_decay = jnp.exp(G_last)

    def step(state, inp):
        dec, u = inp
        return dec[..., None] * state + u, state

    state0 = jnp.zeros((B, H, Dk, Dv), jnp.float32)
    _, S_prev = lax.scan(step, state0, (chunk_decay.transpose(1, 0, 2, 3), U.transpose(1, 0, 2, 3, 4)))
    S_prev = S_prev.transpose(1, 0, 2, 3, 4)
    inter = jnp.einsum('bnthd,bnhde->bnthe', q * jnp.exp(G), S_prev)
    return (intra + inter).reshape(B, S, H, Dv)


def hgrn2_bidir(z, lb, norm_g):
    B, S, _ = z.shape
    H, Dk, Dv = HGRN_HEADS, HGRN_EXPAND, HGRN_VDIM
    zf = z.astype(jnp.float32)
    q, f_fwd, f_bwd, i, g = split_sizes(zf, (H * Dk, H * Dk, H * Dk, H * Dv, H * Dv))
    q = jax.nn.silu(q).reshape(B, S, H, Dk)
    i = i.reshape(B, S, H, Dv)
    lb = lb.astype(jnp.float32).reshape(H, Dk)
    log_lb, log_1m_lb = jnp.log(lb), jnp.log1p(-lb)

    def log_forget(fr):
        return jnp.logaddexp(log_1m_lb + jax.nn.log_sigmoid(fr.reshape(B, S, H, Dk)), log_lb)

    o = (hgrn2_chunk(q, log_forget(f_fwd), i)
         + hgrn2_chunk(q[:, ::-1], log_forget(f_bwd)[:, ::-1], i[:, ::-1])[:, ::-1])
    o = o * lax.rsqrt(jnp.mean(o * o, -1, keepdims=True) + RMS_EPS) * norm_g.reshape(H, Dv)
    return (o.reshape(B, S, H * Dv) * jax.nn.silu(g)).astype(z.dtype)


def mla_mixer(z, q_norm, kv_norm, w_uq, w_ukv, row_pos, col_pos):
    B, S, _ = z.shape
    H = MLA_HEADS
    c_q, c_kv, k_rope = split_sizes(z, (MLA_Q_RANK, MLA_KV_RANK, MLA_ROPE))
    q = (rms_norm(c_q, q_norm) @ w_uq).reshape(B, S, H, MLA_NOPE + MLA_ROPE)
    kv = (rms_norm(c_kv, kv_norm) @ w_ukv).reshape(B, S, H, MLA_NOPE + MLA_V)
    q_nope, q_rope = q[..., :MLA_NOPE], q[..., MLA_NOPE:]
    k_nope, v = kv[..., :MLA_NOPE], kv[..., MLA_NOPE:]
    q_rope = axial_rope(q_rope, row_pos, col_pos)
    k_rope = axial_rope(k_rope[:, :, None, :], row_pos, col_pos)
    q = jnp.concatenate([q_nope, q_rope], -1)
    k = jnp.concatenate([k_nope, jnp.broadcast_to(k_rope, (B, S, H, MLA_ROPE))], -1)
    return block_attention(q, k, v, (MLA_NOPE + MLA_ROPE) ** -0.5)


def memory_cross_attention(x, mem, wq, wkv, wo):
    B, S, D = x.shape
    M = mem.shape[1]
    dh = D // MEM_HEADS
    q = (x @ wq).reshape(B, S, MEM_HEADS, dh)
    k, v = jnp.split(mem @ wkv, 2, axis=-1)
    k = k.reshape(B, M, MEM_HEADS, dh)
    v = v.reshape(B, M, MEM_HEADS, dh)
    return block_attention(q, k, v, dh ** -0.5) @ wo


def grouped_expert_ffn(xt, e_ids, gates, w1, w3, w2):
    N, D = xt.shape
    K = e_ids.shape[1]
    E = w1.shape[0]
    M = N * K
    flat_e = e_ids.reshape(-1).astype(jnp.int32)
    flat_tok = jnp.arange(M, dtype=jnp.int32) // K
    counts = jnp.zeros((E,), jnp.int32).at[flat_e].add(1)
    padded = (counts + MOE_BLOCK - 1) // MOE_BLOCK * MOE_BLOCK
    pad_end = jnp.cumsum(padded)
    pad_start = pad_end - padded
    seg_start = jnp.cumsum(counts) - counts
    order = jnp.argsort(flat_e)
    sorted_e = flat_e[order]
    dest_sorted = pad_start[sorted_e] + jnp.arange(M, dtype=jnp.int32) - seg_start[sorted_e]
    dest = jnp.zeros((M,), jnp.int32).at[order].set(dest_sorted)
    n_rows = (M + MOE_BLOCK - 1) // MOE_BLOCK * MOE_BLOCK + E * MOE_BLOCK
    n_blocks = n_rows // MOE_BLOCK
    row_tok = jnp.full((n_rows,), N, jnp.int32).at[dest].set(flat_tok)
    x_rows = jnp.concatenate([xt, jnp.zeros((1, D), xt.dtype)], 0)[row_tok]
    x_rows = x_rows.reshape(n_blocks, MOE_BLOCK, D)
    block_start = jnp.arange(n_blocks, dtype=jnp.int32) * MOE_BLOCK
    block_e = jnp.minimum(jnp.searchsorted(pad_end, block_start, side='right'), E - 1)

    def expert_block(args):
        xb, e = args
        h = jax.nn.silu(xb @ w1[e]) * (xb @ w3[e])
        return h @ w2[e]

    y_rows = lax.map(expert_block, (x_rows, block_e)).reshape(n_rows, D)
    return jnp.sum(y_rows[dest].reshape(N, K, D) * gates[..., None], axis=1)


def hierarchical_moe(x, wg_group, bg_group, wg_expert, bg_expert, w1, w3, w2):
    B, S, D = x.shape
    xt = x.reshape(B * S, D)
    N = xt.shape[0]
    g_logits = (xt @ wg_group).astype(jnp.float32) + bg_group
    g_idx = jnp.argmax(g_logits, axis=-1)
    p_group = jnp.take_along_axis(jax.nn.softmax(g_logits, -1), g_idx[:, None], axis=1)
    e_logits = ((xt @ wg_expert).astype(jnp.float32) + bg_expert).reshape(N, MOE_GROUPS, MOE_PER_GROUP)
    e_sel = jnp.take_along_axis(e_logits, g_idx[:, None, None], axis=1)[:, 0]
    top_v, top_i = lax.top_k(e_sel, MOE_TOPK)
    gates = (p_group * jax.nn.softmax(top_v, -1)).astype(x.dtype)
    e_ids = g_idx[:, None].astype(jnp.int32) * MOE_PER_GROUP + top_i.astype(jnp.int32)
    return grouped_expert_ffn(xt, e_ids, gates, w1, w3, w2).reshape(B, S, D)


def setup_inputs(seed: int = 0) -> dict:
    key = jax.random.key(seed)
    ks = iter(jax.random.split(key, 48))
    L, D = DEPTH, D_MODEL

    def nrm(shape, scale=1.0):
        return scale * jax.random.normal(next(ks), shape, jnp.float32)

    def gain(shape):
        return 1.0 + nrm(shape, 0.02)

    return {
        'x': nrm((BATCH, SEQ, D)),
        'mem': nrm((BATCH, MEM_TOKENS, D)),
        'ln_in_g': gain((D,)),
        'ln_in_b': nrm((D,), 0.02),
        'w_in': nrm((L, D, IN_WIDTH), D ** -0.5),
        'attn_q_norm': gain((L, HEAD_DIM)),
        'attn_k_norm': gain((L, HEAD_DIM)),
        'rwkv_mu_prev': 0.3 + nrm((L, RWKV_IN), 0.1),
        'rwkv_mu_next': 0.3 + nrm((L, RWKV_IN), 0.1),
        'rwkv_w0': nrm((L, 2, GW), 0.5),
        'rwkv_w2': nrm((L, 2, RWKV_DECAY_LORA, GW), RWKV_DECAY_LORA ** -0.5),
        'rwkv_a0': nrm((L, 2, GW), 0.1),
        'rwkv_a2': nrm((L, 2, RWKV_AAA_LORA, GW), RWKV_AAA_LORA ** -0.5),
        'rwkv_g2': nrm((L, RWKV_GATE_LORA, GW), RWKV_GATE_LORA ** -0.5),
        'rwkv_k_k': 0.85 + nrm((L, GW), 0.02),
        'rwkv_k_a': gain((L, GW)),
        'rwkv_r_k': nrm((L, RWKV_HEADS, HEAD_DIM), 0.1),
        'rwkv_gn_g': gain((L, GW)),
        'rwkv_gn_b': nrm((L, GW), 0.02),
        'hgrn_lb_logits': nrm((L, HGRN_HEADS * HGRN_EXPAND), 0.5),
        'hgrn_norm_g': gain((L, HGRN_HEADS * HGRN_VDIM)),
        'mla_q_norm': gain((L, MLA_Q_RANK)),
        'mla_kv_norm': gain((L, MLA_KV_RANK)),
        'mla_w_uq': nrm((L, MLA_Q_RANK, MLA_HEADS * (MLA_NOPE + MLA_ROPE)), MLA_Q_RANK ** -0.5),
        'mla_w_ukv': nrm((L, MLA_KV_RANK, MLA_HEADS * (MLA_NOPE + MLA_V)), MLA_KV_RANK ** -0.5),
        'w_out': nrm((L, MIX_WIDTH, D), DN_BETA * MIX_WIDTH ** -0.5),
        'ln1_g': gain((L, D)),
        'ln1_b': nrm((L, D), 0.02),
        'mem_wq': nrm((L, D, D), D ** -0.5),
        'mem_wkv': nrm((L, D, 2 * D), D ** -0.5),
        'mem_wo': nrm((L, D, D), DN_BETA * D ** -0.5),
        'ln2_g': gain((L, D)),
        'ln2_b': nrm((L, D), 0.02),
        'moe_wg_group': nrm((L, D, MOE_GROUPS), D ** -0.5),
        'moe_bg_group': nrm((L, MOE_GROUPS), 0.01),
        'moe_wg_expert': nrm((L, D, MOE_EXPERTS), D ** -0.5),
        'moe_bg_expert': nrm((L, MOE_EXPERTS), 0.01),
        'moe_w1': nrm((L, MOE_EXPERTS, D, MOE_HIDDEN), D ** -0.5),
        'moe_w3': nrm((L, MOE_EXPERTS, D, MOE_HIDDEN), D ** -0.5),
        'moe_w2': nrm((L, MOE_EXPERTS, MOE_HIDDEN, D), DN_BETA * MOE_HIDDEN ** -0.5),
        'ln3_g': gain((L, D)),
        'ln3_b': nrm((L, D), 0.02),
    }


def reference(x, mem, ln_in_g, ln_in_b, w_in, attn_q_norm, attn_k_norm,
              rwkv_mu_prev, rwkv_mu_next, rwkv_w0, rwkv_w2, rwkv_a0, rwkv_a2, rwkv_g2,
              rwkv_k_k, rwkv_k_a, rwkv_r_k, rwkv_gn_g, rwkv_gn_b,
              hgrn_lb_logits, hgrn_norm_g,
              mla_q_norm, mla_kv_norm, mla_w_uq, mla_w_ukv,
              w_out, ln1_g, ln1_b,
              mem_wq, mem_wkv, mem_wo, ln2_g, ln2_b,
              moe_wg_group, moe_bg_group, moe_wg_expert, moe_bg_expert,
              moe_w1, moe_w3, moe_w2, ln3_g, ln3_b):
    S = x.shape[1]
    ROWS = S // GRID_W
    row_pos = jnp.repeat(jnp.arange(ROWS, dtype=jnp.float32), GRID_W)
    col_pos = jnp.tile(jnp.arange(GRID_W, dtype=jnp.float32), ROWS)
    lb_all = jnp.cumsum(jax.nn.softmax(hgrn_lb_logits.astype(jnp.float32), axis=0), axis=0)
    lb_all = lb_all - lb_all[0]

    x = layer_norm(x, ln_in_g, ln_in_b)
    for l in range(DEPTH):
        h = x @ w_in[l]
        z_att, z_rwkv, z_hgrn, z_mla = split_sizes(h, (ATT_IN, RWKV_IN, HGRN_IN, MLA_IN))
        y_att = gqa_axial_mixer(z_att, attn_q_norm[l], attn_k_norm[l], row_pos, col_pos)
        y_rwkv = rwkv7_bidir(z_rwkv, rwkv_mu_prev[l], rwkv_mu_next[l], rwkv_w0[l], rwkv_w2[l],
                             rwkv_a0[l], rwkv_a2[l], rwkv_g2[l], rwkv_k_k[l], rwkv_k_a[l],
                             rwkv_r_k[l], rwkv_gn_g[l], rwkv_gn_b[l])
        y_hgrn = hgrn2_bidir(z_hgrn, lb_all[l], hgrn_norm_g[l])
        y_mla = mla_mixer(z_mla, mla_q_norm[l], mla_kv_norm[l], mla_w_uq[l], mla_w_ukv[l],
                          row_pos, col_pos)
        mix = jnp.concatenate([y_att, y_rwkv, y_hgrn, y_mla], axis=-1) @ w_out[l]
        x = layer_norm(DN_ALPHA * x + mix, ln1_g[l], ln1_b[l])
        x = layer_norm(DN_ALPHA * x + memory_cross_attention(x, mem, mem_wq[l], mem_wkv[l], mem_wo[l]),
                       ln2_g[l], ln2_b[l])
        x = layer_norm(DN_ALPHA * x + hierarchical_moe(x, moe_wg_group[l], moe_bg_group[l],
                                                       moe_wg_expert[l], moe_bg_expert[l],
                                                       moe_w1[l], moe_w3[l], moe_w2[l]),
                       ln3_g[l], ln3_b[l])
    return x
```

```python
import math
import os
CUT = int(os.environ.get('CUT', '99'))
import numpy as np
import concourse.bass as bass
import concourse.mybir as mybir
from concourse.bass_utils import run_bass_kernel_spmd

F32 = mybir.dt.float32
BF16 = mybir.dt.bfloat16
AF = mybir.ActivationFunctionType
ALU = mybir.AluOpType
AX = mybir.AxisListType

ENGS = ["pe", "act", "dve", "pool", "sp"]
WKEYS = {"out", "accum_out", "ap"}
EPOCH = 24000
NDSEM = 56
NDSEM_HW = 40


class Buf:
    __slots__ = ("name", "writes", "reads")

    def __init__(self, name=""):
        self.name = name
        self.writes = {}
        self.reads = {}


class T:
    __slots__ = ("ap", "bufs")

    def __init__(self, ap, bufs):
        self.ap = ap
        self.bufs = bufs

    def __getitem__(self, k):
        return T(self.ap[k], self.bufs)

    def re(self, s, **kw):
        return T(self.ap.rearrange(s, **kw), self.bufs)

    def bc(self, shape):
        return T(self.ap.broadcast_to(shape), self.bufs)

    def un(self, ax):
        return T(self.ap.unsqueeze(ax), self.bufs)

    def bitcast(self, dt):
        return T(self.ap.bitcast(dt), self.bufs)

    @property
    def shape(self):
        return self.ap.shape


def _key(ev):
    return (ev[0], ev[1])


class Prog:
    def __init__(self, nc, arena_words):
        self.nc = nc
        self.stream = {e: [] for e in ENGS}
        self.tick = {e: 0 for e in ENGS}
        self.esems = {e: [] for e in ENGS}
        self.seen = {e: {} for e in ENGS}
        self.dsems = [nc.alloc_semaphore(f"dsem{i}") for i in range(NDSEM)]
        self.dtot = [0] * NDSEM
        self.drr = 0
        self.drr_sw = 0
        self.pe_pending = []
        self.arena = nc.alloc_sbuf_tensor("arena", [128, arena_words], F32)
        self.arena_words = arena_words
        self.aoff = 0
        self.persist = 0
        self.psum = [T(nc.alloc_psum_tensor(f"psb{i}", [128, 512], F32)[:, :], [Buf(f"ps{i}")]) for i in range(8)]
        self.nbuf = 0
        self.ninstr = 0
        self.cache = {}
        self.pr = 0
        self.nrot = 8

    def alloc(self, free_shape, dtype=F32, name=None):
        n = int(np.prod(free_shape))
        words = (n + 1) // 2 if dtype == BF16 else n
        words = (words + 7) // 8 * 8
        assert self.aoff + words <= self.arena_words, f"arena overflow {self.aoff}+{words} ({name})"
        ap = self.arena[:, self.aoff:self.aoff + words]
        self.aoff += words
        if dtype == BF16:
            ap = ap.bitcast(BF16)[:, 0:n]
        else:
            ap = ap[:, 0:n]
        if len(free_shape) == 2:
            ap = ap.rearrange("p (a b) -> p a b", a=free_shape[0])
        elif len(free_shape) == 3:
            ap = ap.rearrange("p (a b c) -> p a b c", a=free_shape[0], b=free_shape[1])
        elif len(free_shape) == 4:
            ap = ap.rearrange("p (a b c d) -> p a b c d", a=free_shape[0], b=free_shape[1], c=free_shape[2])
        self.nbuf += 1
        return T(ap, [Buf(name or f"b{self.nbuf}")])

    def tmp(self, name, free_shape, dtype=F32, slot=0):
        k = (name, slot)
        if k not in self.cache:
            self.cache[k] = self.alloc(free_shape, dtype, f"{name}{slot}")
        return self.cache[k]

    def ps(self):
        p = self.psum[self.pr % self.nrot]
        self.pr += 1
        return p

    def dram(self, name, shape, dtype=F32):
        ap = self.nc.dram_tensor(name, list(shape), dtype, kind="Internal").ap()
        return DT(ap)

    def _esem(self, P, ep):
        while len(self.esems[P]) <= ep:
            self.esems[P].append(self.nc.alloc_semaphore(f"es_{P}_{len(self.esems[P])}"))
        return self.esems[P][ep]

    def _wait(self, X, ev):
        k = _key(ev)
        v = ev[2]
        if self.seen[X].get(k, 0) >= v:
            return
        self.seen[X][k] = v
        if ev[0] == "e":
            ep = (v - 1) // EPOCH
            sem = self._esem(ev[1], ep)
            val = v - ep * EPOCH
        else:
            sem = self.dsems[ev[1]]
            val = v
        self.stream[X].append(lambda e, sem=sem, val=val: e.wait_ge(sem, val))

    def _deps(self, X, wbufs, rbufs):
        for b in rbufs:
            for ev in b.writes.values():
                self._wait(X, ev)
        for b in wbufs:
            for ev in list(b.writes.values()) + list(b.reads.values()):
                if ev[0] == "e" and ev[1] == X and X == "pe":
                    continue
                self._wait(X, ev)

    def _record(self, ev, wbufs, rbufs):
        k = _key(ev)
        for b in wbufs:
            if b.reads:
                b.writes = {}
                b.reads = {}
            b.writes[k] = ev
        for b in rbufs:
            b.reads[k] = ev

    def op(self, X, call, wbufs, rbufs, inc=True):
        self._deps(X, wbufs, rbufs)
        self.ninstr += 1
        if not inc:
            assert X == "pe"
            self.pe_pending.append((wbufs, rbufs))
            self.stream[X].append(lambda e: call(e))
            return
        self.tick[X] += 1
        t = self.tick[X]
        ep = (t - 1) // EPOCH
        sem = self._esem(X, ep)
        self.stream[X].append(lambda e: call(e).then_inc(sem, 1))
        ev = ("e", X, t)
        if X == "pe" and self.pe_pending:
            for (w, r) in self.pe_pending:
                self._record(ev, w, r)
            self.pe_pending = []
        self._record(ev, wbufs, rbufs)

    def ins(self, X, fname, **kw):
        wb, rb, real = [], [], {}
        for k, v in kw.items():
            if isinstance(v, T):
                (wb if k in WKEYS else rb).extend(v.bufs)
                real[k] = v.ap
            else:
                real[k] = v
        eng = {"pe": "tensor", "act": "scalar", "dve": "vector", "pool": "gpsimd", "sp": "sync"}[X]
        self.op(X, lambda e: getattr(e, fname)(**real), wb, rb)

    def v(self, fname, **kw):
        self.ins("dve", fname, **kw)

    def a(self, fname, **kw):
        self.ins("act", fname, **kw)

    def g(self, fname, **kw):
        self.ins("pool", fname, **kw)

    def mm(self, out, lhsT, rhs, start=True, stop=True, inc=None, **kw):
        if inc is None:
            inc = stop
        o, l, r = out.ap, lhsT.ap, rhs.ap
        self.op("pe", lambda e: e.matmul(o, lhsT=l, rhs=r, start=start, stop=stop, **kw),
                list(out.bufs), list(lhsT.bufs) + list(rhs.bufs), inc=inc)

    def tr(self, out, in_, ident, inc=True):
        o, i, d = out.ap, in_.ap, ident.ap
        self.op("pe", lambda e: e.transpose(out=o, in_=i, identity=d),
                list(out.bufs), list(in_.bufs) + list(ident.bufs), inc=inc)

    def dma(self, Q, out, in_, **kw):
        wb, rb = list(out.bufs), list(in_.bufs)
        self._deps(Q, wb, rb)
        if Q == "pool":
            s = NDSEM_HW + self.drr_sw
            self.drr_sw = (self.drr_sw + 1) % (NDSEM - NDSEM_HW)
        else:
            s = self.drr
            self.drr = (self.drr + 1) % NDSEM_HW
        if self.dtot[s] > 0:
            self._wait(Q, ("d", s, self.dtot[s]))
        self.dtot[s] += 16
        sem = self.dsems[s]
        o, i = out.ap, in_.ap
        self.ninstr += 1
        self.stream[Q].append(lambda e: e.dma_start(out=o, in_=i, **kw).then_inc(sem, 16))
        ev = ("d", s, self.dtot[s])
        self._record(ev, wb, rb)

    def barrier(self):
        for X in ENGS:
            for Pn in ENGS:
                if Pn != X and self.tick[Pn] > 0:
                    self._wait(X, ("e", Pn, self.tick[Pn]))
            for s in range(NDSEM):
                if self.dtot[s] > 0:
                    self._wait(X, ("d", s, self.dtot[s]))

    def phase(self):
        self.barrier()
        self.aoff = self.persist
        self.cache = {}

    def emit(self):
        nc = self.nc
        with nc.Block() as block:
            @block.tensor
            def _(e):
                for f in self.stream["pe"]:
                    f(e)

            @block.scalar
            def _(e):
                for f in self.stream["act"]:
                    f(e)

            @block.vector
            def _(e):
                for f in self.stream["dve"]:
                    f(e)

            @block.gpsimd
            def _(e):
                for f in self.stream["pool"]:
                    f(e)

            @block.sync
            def _(e):
                for f in self.stream["sp"]:
                    f(e)


class DT:
    def __init__(self, ap):
        self.ap = ap
        self.regs = {}

    def t(self, keys, idx=None):
        if not isinstance(keys, (list, tuple)) or (isinstance(keys, tuple)):
            keys = [keys]
        bufs = []
        for k in keys:
            if k not in self.regs:
                self.regs[k] = Buf(str(k))
            bufs.append(self.regs[k])
        ap = self.ap if idx is None else self.ap[idx]
        return T(ap, bufs)


def ext(ap):
    return T(ap, [])


D = 1024
S = 2048
NB = 2
NT = NB * S
NTILE = NT // 128
TPS = S // 128
DEPTH = 2
INW = 3104
ATT_IN, RWKV_IN, HGRN_IN, MLA_IN = 512, 960, 1280, 352
OFF_ATT, OFF_RWKV, OFF_HGRN, OFF_MLA = 0, 512, 1472, 2752
DN_ALPHA = (2 * DEPTH) ** 0.25
LN_EPS = 1e-5
RMS_EPS = 1e-6

WEIGHT_NAMES = [
    'ln_in_g', 'ln_in_b', 'w_in', 'attn_q_norm', 'attn_k_norm', 'rwkv_mu_prev', 'rwkv_mu_next',
    'rwkv_w0', 'rwkv_w2', 'rwkv_a0', 'rwkv_a2', 'rwkv_g2', 'rwkv_k_k', 'rwkv_k_a', 'rwkv_r_k',
    'rwkv_gn_g', 'rwkv_gn_b', 'hgrn_lb_logits', 'hgrn_norm_g', 'mla_q_norm', 'mla_kv_norm',
    'mla_w_uq', 'mla_w_ukv', 'w_out', 'ln1_g', 'ln1_b', 'mem_wq', 'mem_wkv', 'mem_wo', 'ln2_g',
    'ln2_b', 'moe_wg_group', 'moe_bg_group', 'moe_wg_expert', 'moe_bg_expert', 'moe_w1', 'moe_w3',
    'moe_w2', 'ln3_g', 'ln3_b']


def host_consts():
    c = {}
    c["c_ident"] = np.eye(128, dtype=np.float32)
    return c


ROPE_THETA = 10000.0
TL = [NTILE]


def _rope_tab(half_dim, nrep):
    t = np.arange(S)
    row = (t // 64).astype(np.float64)
    col = (t % 64).astype(np.float64)
    nf = half_dim // 2
    inv = ROPE_THETA ** (-np.arange(0, half_dim, 2, dtype=np.float64) / half_dim)
    ar = row[:, None] * inv[None, :]
    ac = col[:, None] * inv[None, :]
    Ct = np.concatenate([np.cos(ar), np.cos(ar), np.cos(ac), np.cos(ac)], axis=1)
    St = np.concatenate([-np.sin(ar), np.sin(ar), -np.sin(ac), np.sin(ac)], axis=1)
    return (np.tile(Ct, (1, nrep)).astype(np.float32), np.tile(St, (1, nrep)).astype(np.float32))


def host_consts():
    c = {}
    c["c_ident"] = np.eye(128, dtype=np.float32)
    c["c_ropeC6"], c["c_ropeS6"] = _rope_tab(32, 6)
    c["c_ropeC5"], c["c_ropeS5"] = _rope_tab(16, 5)
    sc, bd = _scan_consts()
    c["c_scan"] = np.ascontiguousarray(sc.transpose(1, 0, 2))
    c["c_bd"] = bd
    rm = np.zeros((128, 2), np.float32)
    rm[:64, 0] = 1.0
    rm[64:, 1] = 1.0
    c["c_rowmask"] = np.ascontiguousarray(np.repeat(rm[:, :, None], 256, axis=2))
    return c


def kernel_shapes():
    L = DEPTH
    s = {
        "x": (NT, D), "mem": (NB * 256, D),
        "c_ident": (128, 128), "c_ropeC6": (S, 384), "c_ropeS6": (S, 384),
        "c_ropeC5": (S, 160), "c_ropeS5": (S, 160), "c_scan": (128, 2, 640), "c_bd": (128, 128), "c_rowmask": (128, 2, 256),
        "ln_in_g": (D,), "ln_in_b": (D,), "w_in": (L, D, INW),
        "attn_q_norm": (L, 64), "attn_k_norm": (L, 64),
        "rwkv_mu_prev": (L, 960), "rwkv_mu_next": (L, 960), "rwkv_w0": (L, 2, 256), "rwkv_w2": (L, 2, 32, 256),
        "rwkv_a0": (L, 2, 256), "rwkv_a2": (L, 2, 32, 256), "rwkv_g2": (L, 64, 256), "rwkv_k_k": (L, 256),
        "rwkv_k_a": (L, 256), "rwkv_r_k": (L, 4, 64), "rwkv_gn_g": (L, 256), "rwkv_gn_b": (L, 256),
        "hgrn_lb_logits": (L, 256), "hgrn_norm_g": (L, 256),
        "mla_q_norm": (L, 192), "mla_kv_norm": (L, 128), "mla_w_uq": (L, 192, 384), "mla_w_ukv": (L, 128, 512),
        "w_out": (L, D, D), "ln1_g": (L, D), "ln1_b": (L, D),
        "mem_wq": (L, D, D), "mem_wkv": (L, D, 2 * D), "mem_wo": (L, D, D), "ln2_g": (L, D), "ln2_b": (L, D),
        "moe_wg_group": (L, D, 4), "moe_bg_group": (L, 4), "moe_wg_expert": (L, D, 32), "moe_bg_expert": (L, 32),
        "moe_w1": (L, 32, D, 256), "moe_w3": (L, 32, D, 256), "moe_w2": (L, 32, 256, D),
        "ln3_g": (L, D), "ln3_b": (L, D),
    }
    return s


class Ctx:
    pass


def load_cast(P, dst, src_ap, q="pool", maxcols=1024):
    n = src_ap.shape[-1]
    c0 = 0
    while c0 < n:
        c1 = min(n, c0 + maxcols)
        P.dma(q, dst[..., c0:c1], ext(src_ap[..., c0:c1]))
        c0 = c1


def rows(t):
    return (slice(t * 128, (t + 1) * 128),)


def layer_norm_tile(P, C, xin, g_bc, b_bc, out, sl):
    st = P.tmp("lnst", [4], F32, sl)
    xc = P.tmp("lnxc", [D], F32, sl)
    junk = P.tmp("lnjunk", [D], F32, sl)
    P.a("activation", out=junk, in_=xin, func=AF.Identity, scale=1.0 / D, accum_out=st[:, 0:1])
    P.v("tensor_scalar", out=xc, in0=xin, scalar1=st[:, 0:1], scalar2=None, op0=ALU.subtract)
    P.a("activation", out=junk, in_=xc, func=AF.Square, scale=1.0 / 32.0, accum_out=st[:, 1:2])
    P.a("activation", out=st[:, 2:3], in_=st[:, 1:2], func=AF.Sqrt, bias=C.eps_ln[:, 0:1], scale=1.0)
    P.v("reciprocal", out=st[:, 3:4], in_=st[:, 2:3])
    P.v("scalar_tensor_tensor", out=xc, in0=xc, scalar=st[:, 3:4], in1=g_bc, op0=ALU.mult, op1=ALU.mult)
    P.g("tensor_tensor", out=out, in0=xc, in1=b_bc, op=ALU.add)


def transpose_to(P, C, xb, ncols, dst_fn):
    kc = 0
    nk = (ncols + 127) // 128
    while kc < nk:
        n = min(4, nk - kc)
        ps = P.ps()
        psb = ps.bitcast(BF16)
        for j in range(n):
            c0 = (kc + j) * 128
            c1 = min(ncols, c0 + 128)
            P.tr(psb[0:c1 - c0, j * 128:(j + 1) * 128], xb[:, c0:c1], C.identb, inc=(j == n - 1))
        for j in range(n):
            c0 = (kc + j) * 128
            c1 = min(ncols, c0 + 128)
            dst = dst_fn(kc + j, c1 - c0)
            if j % 2 == 0:
                P.v("tensor_copy", out=dst, in_=psb[0:c1 - c0, j * 128:(j + 1) * 128])
            else:
                P.a("copy", out=dst, in_=psb[0:c1 - c0, j * 128:(j + 1) * 128])
        kc += n


def to_xT(P, C, xn, tile, sl):
    xb = P.tmp("xb", [D], BF16, sl)
    P.a("copy", out=xb, in_=xn)
    transpose_to(P, C, xb, D, lambda kc, r: C.XT[:, kc, tile * 128:(tile + 1) * 128])


def attention(P, C, nkt, chunks_fn, v_fn, dv, scale, out_fn, heads, tag):
    gpb = max(1, 512 // (dv + 1))
    it = 0
    for b in range(NB):
        for h in range(heads):
            ch = chunks_fn(b, h)
            for qb in range(4):
                sl = it % 2
                it += 1
                PT = P.tmp("PT" + tag, [nkt, 512], BF16, sl)
                for kt in range(nkt):
                    ps = P.ps()
                    for ci, (K_, Q_) in enumerate(ch):
                        P.mm(ps, lhsT=K_[:, kt * 128:(kt + 1) * 128], rhs=Q_[:, qb * 512:(qb + 1) * 512],
                             start=(ci == 0), stop=(ci == len(ch) - 1))
                    P.a("activation", out=PT[:, kt, :], in_=ps, func=AF.Exp, scale=scale)
                o_sb = P.tmp("osb" + tag, [4, dv], F32, sl)
                rd = P.tmp("rd" + tag, [4], F32, sl)
                qi = 0
                while qi < 4:
                    pso = P.ps()
                    ng = min(gpb, 4 - qi)
                    for g in range(ng):
                        for kt in range(nkt):
                            P.mm(pso[:, g * (dv + 1):(g + 1) * (dv + 1)],
                                 lhsT=PT[:, kt, (qi + g) * 128:(qi + g + 1) * 128], rhs=v_fn(b, h, kt),
                                 start=(kt == 0), stop=(kt == nkt - 1), inc=(kt == nkt - 1 and g == ng - 1))
                    pv = pso[:, 0:ng * (dv + 1)].re("p (g d) -> p g d", g=ng)
                    P.v("reciprocal", out=rd[:, qi:qi + ng], in_=pv[:, :, dv])
                    P.v("tensor_tensor", out=o_sb[:, qi:qi + ng, :], in0=pv[:, :, 0:dv],
                        in1=rd[:, qi:qi + ng].un(2).bc([128, ng, dv]), op=ALU.mult)
                    qi += ng
                out_fn(b, h, qb, o_sb)


def rope_apply(P, xin, rc, rs, out, nblk, half, names, sl, outre=None):
    n = nblk * 2 * half
    t1 = P.tmp(names + "t1", [n], F32, sl)
    t2 = P.tmp(names + "t2", [n], F32, sl)
    P.v("tensor_tensor", out=t1, in0=xin, in1=rc, op=ALU.mult)
    xs = xin.re("p (b x d) -> p b x d", x=2, d=half)[:, :, ::-1, :]
    P.g("tensor_tensor", out=t2.re("p (b x d) -> p b x d", x=2, d=half), in0=xs,
        in1=rs.re("p (b x d) -> p b x d", x=2, d=half), op=ALU.mult)
    if outre is not None:
        P.v("tensor_tensor", out=out, in0=t1.re(outre[0], **outre[1]), in1=t2.re(outre[0], **outre[1]), op=ALU.add)
    else:
        P.v("tensor_tensor", out=out, in0=t1, in1=t2, op=ALU.add)


def stage_gqa(P, C, W, l, hbuf, mixd):
    P.phase()
    QT = P.alloc([4, NT], BF16, "QT")
    KT = P.alloc([2, NT], BF16, "KT")
    VS = P.alloc([NTILE, 2, 65], BF16, "VS")
    gain6 = P.alloc([6, 64], F32, "gain6")
    eps = P.alloc([1], F32, "epsr")
    P.g("memset", ap=eps, constant=RMS_EPS)
    P.g("memset", ap=VS, constant=1.0)
    for j in range(6):
        src = W["attn_q_norm"] if j < 4 else W["attn_k_norm"]
        P.dma("sp", gain6[:, j, :], ext(src[l].partition_broadcast(128)))
    for sl in range(2):
        P.g("memset", ap=P.tmp("qr", [6, 128], BF16, sl), constant=0.0)
    for t in range(TL[0]):
        sl = t % 2
        ts = t % TPS
        z = P.tmp("z", [512], F32, sl)
        rc = P.tmp("rc", [384], F32, sl)
        rs = P.tmp("rs", [384], F32, sl)
        P.dma("sp", z, hbuf.t(t, (slice(t * 128, (t + 1) * 128), slice(OFF_ATT, OFF_ATT + 512))))
        P.dma("sp", rc, ext(W["c_ropeC6"][ts * 128:(ts + 1) * 128, :]))
        P.dma("sp", rs, ext(W["c_ropeS6"][ts * 128:(ts + 1) * 128, :]))
        sq = P.tmp("sq", [384], F32, sl)
        ss = P.tmp("ss", [6], F32, sl)
        ss2 = P.tmp("ss2", [6], F32, sl)
        rstd = P.tmp("rstd", [6], F32, sl)
        P.a("activation", out=sq, in_=z[:, 0:384], func=AF.Square)
        P.v("tensor_reduce", out=ss, in_=sq.re("p (h d) -> p h d", d=64), axis=AX.X, op=ALU.add)
        P.a("activation", out=ss2, in_=ss, func=AF.Sqrt, scale=1.0 / 64.0, bias=eps[:, 0:1])
        P.v("reciprocal", out=rstd, in_=ss2)
        qn = P.tmp("qn", [384], F32, sl)
        P.v("tensor_tensor", out=qn.re("p (h d) -> p h d", d=64), in0=z[:, 0:384].re("p (h d) -> p h d", d=64),
            in1=rstd.un(2).bc([128, 6, 64]), op=ALU.mult)
        P.g("tensor_tensor", out=qn, in0=qn, in1=gain6.re("p h d -> p (h d)"), op=ALU.mult)
        qr = P.tmp("qr", [6, 128], BF16, sl)
        rope_apply(P, qn, rc, rs, qr[:, :, 0:64], 12, 16, "g", sl, outre=("p (h d) -> p h d", dict(d=64)))
        ps = P.ps()
        psb = ps.bitcast(BF16)
        for j in range(6):
            P.tr(psb[:, j * 128:(j + 1) * 128], qr[:, j, :], C.identb, inc=(j == 5))
        if CUT >= 8:
            P.v("tensor_copy", out=QT[:, :, t * 128:(t + 1) * 128],
                in_=psb[:, 0:512].re("p (h t) -> p h t", h=4))
        if CUT >= 9:
            P.v("tensor_copy", out=KT[:, :, t * 128:(t + 1) * 128],
                in_=psb[:, 512:768].re("p (h t) -> p h t", h=2))
        if CUT >= 10:
            P.v("tensor_copy", out=VS[:, t, :, 0:64], in_=z[:, 384:512].re("p (h d) -> p h d", d=64))

    def chunks(b, h):
        return [(KT[:, h // 2, b * S:(b + 1) * S], QT[:, h, b * S:(b + 1) * S])]

    def vfn(b, h, kt):
        return VS[:, b * TPS + kt, h // 2, :]

    def outfn(b, h, qb, o_sb):
        r0 = b * S + qb * 512
        keys = [(r0 // 128 + i, "att") for i in range(4)]
        dst = mixd.t(keys, (slice(r0, r0 + 512), slice(h * 64, (h + 1) * 64))).re("(q p) d -> p q d", p=128)
        P.dma("sp", dst, o_sb)

    if getattr(C, "stop", None) == "gqaprep":
        C.dbgT = (QT, KT, VS)
        return
    attention(P, C, TPS, chunks, vfn, 64, 1.0 / 8.0, outfn, 4, "g")


def stage_mla(P, C, W, l, hbuf, mixd):
    P.phase()
    QT = P.alloc([4, NT], BF16, "mQT")
    KT = P.alloc([4, NT], BF16, "mKT")
    VS = P.alloc([NTILE, 4, 65], BF16, "mVS")
    wuq = P.alloc([2, 384], BF16, "wuq")
    wukv = P.alloc([512], BF16, "wukv")
    gq = P.alloc([192], F32, "gq")
    gkv = P.alloc([128], F32, "gkv")
    eps = P.alloc([1], F32, "epsm")
    P.g("memset", ap=eps, constant=RMS_EPS)
    P.g("memset", ap=VS, constant=1.0)
    P.g("memset", ap=wuq, constant=0.0)
    P.dma("pool", wuq[:, 0, :], ext(W["mla_w_uq"][l, 0:128, :]))
    P.dma("pool", wuq[0:64, 1, :], ext(W["mla_w_uq"][l, 128:192, :]))
    P.dma("pool", wukv, ext(W["mla_w_ukv"][l]))
    P.dma("sp", gq, ext(W["mla_q_norm"][l].partition_broadcast(128)))
    P.dma("sp", gkv, ext(W["mla_kv_norm"][l].partition_broadcast(128)))
    for sl in range(2):
        P.g("memset", ap=P.tmp("cz", [3, 128], BF16, sl), constant=0.0)
        P.g("memset", ap=P.tmp("qz", [8, 128], BF16, sl), constant=0.0)
    for t in range(TL[0]):
        sl = t % 2
        ts = t % TPS
        z = P.tmp("mz", [352], F32, sl)
        rc = P.tmp("mrc", [160], F32, sl)
        rs = P.tmp("mrs", [160], F32, sl)
        P.dma("sp", z, hbuf.t(t, (slice(t * 128, (t + 1) * 128), slice(OFF_MLA, OFF_MLA + 352))))
        P.dma("sp", rc, ext(W["c_ropeC5"][ts * 128:(ts + 1) * 128, :]))
        P.dma("sp", rs, ext(W["c_ropeS5"][ts * 128:(ts + 1) * 128, :]))
        junk = P.tmp("mjunk", [192], F32, sl)
        st = P.tmp("mst", [6], F32, sl)
        P.a("activation", out=junk, in_=z[:, 0:192], func=AF.Square, accum_out=st[:, 0:1])
        P.a("activation", out=junk[:, 0:128], in_=z[:, 192:320], func=AF.Square, accum_out=st[:, 1:2])
        P.a("activation", out=st[:, 2:3], in_=st[:, 0:1], func=AF.Sqrt, scale=1.0 / 192.0, bias=eps[:, 0:1])
        P.a("activation", out=st[:, 3:4], in_=st[:, 1:2], func=AF.Sqrt, scale=1.0 / 128.0, bias=eps[:, 0:1])
        P.v("reciprocal", out=st[:, 4:6], in_=st[:, 2:4])
        cz = P.tmp("cz", [3, 128], BF16, sl)
        czf = cz.re("p a b -> p (a b)")
        P.v("scalar_tensor_tensor", out=czf[:, 0:192], in0=z[:, 0:192], scalar=st[:, 4:5], in1=gq,
            op0=ALU.mult, op1=ALU.mult)
        P.v("scalar_tensor_tensor", out=czf[:, 256:384], in0=z[:, 192:320], scalar=st[:, 5:6], in1=gkv,
            op0=ALU.mult, op1=ALU.mult)
        ps = P.ps()
        psb = ps.bitcast(BF16)
        for j in range(3):
            P.tr(psb[:, j * 128:(j + 1) * 128], cz[:, j, :], C.identb, inc=(j == 2))
        cT = P.tmp("cT", [3, 128], BF16, sl)
        P.v("tensor_copy", out=cT, in_=psb[:, 0:384].re("p (a b) -> p a b", a=3))
        psq = P.ps()
        P.mm(psq[:, 0:384], lhsT=cT[:, 0, :], rhs=wuq[:, 0, :], start=True, stop=False)
        P.mm(psq[:, 0:384], lhsT=cT[:, 1, :], rhs=wuq[:, 1, :], start=False, stop=True)
        pskv = P.ps()
        P.mm(pskv, lhsT=cT[:, 2, :], rhs=wukv, start=True, stop=True)
        q3 = psq[:, 0:384].re("p (h d) -> p h d", h=4)
        kv3 = pskv.re("p (h d) -> p h d", h=4)
        rin = P.tmp("rin", [5, 32], F32, sl)
        rout = P.tmp("rout", [5, 32], BF16, sl)
        P.v("tensor_copy", out=rin[:, 0:4, :], in_=q3[:, :, 64:96])
        P.g("tensor_copy", out=rin[:, 4, :], in_=z[:, 320:352])
        rope_apply(P, rin.re("p a b -> p (a b)"), rc, rs, rout.re("p a b -> p (a b)"), 10, 8, "m", sl)
        qz = P.tmp("qz", [8, 128], BF16, sl)
        P.v("tensor_copy", out=qz[:, 0:4, 0:64], in_=q3[:, :, 0:64])
        P.g("tensor_copy", out=qz[:, 0:4, 64:96], in_=rout[:, 0:4, :])
        P.v("tensor_copy", out=qz[:, 4:8, 0:64], in_=kv3[:, :, 0:64])
        for h in range(4):
            P.g("tensor_copy", out=qz[:, 4 + h, 64:96], in_=rout[:, 4, :])
        P.v("tensor_copy", out=VS[:, t, :, 0:64], in_=kv3[:, :, 64:128])
        for half in range(2):
            ps2 = P.ps()
            psb2 = ps2.bitcast(BF16)
            for j in range(4):
                P.tr(psb2[:, j * 128:(j + 1) * 128], qz[:, half * 4 + j, :], C.identb, inc=(j == 3))
            dst = QT if half == 0 else KT
            P.v("tensor_copy", out=dst[:, :, t * 128:(t + 1) * 128],
                in_=psb2[:, 0:512].re("p (h t) -> p h t", h=4))

    def chunks(b, h):
        return [(KT[:, h, b * S:(b + 1) * S], QT[:, h, b * S:(b + 1) * S])]

    def vfn(b, h, kt):
        return VS[:, b * TPS + kt, h, :]

    def outfn(b, h, qb, o_sb):
        r0 = b * S + qb * 512
        keys = [(r0 // 128 + i, "mla") for i in range(4)]
        dst = mixd.t(keys, (slice(r0, r0 + 512), slice(768 + h * 64, 768 + (h + 1) * 64))).re("(q p) d -> p q d", p=128)
        P.dma("sp", dst, o_sb)

    if C.stop == "mlaprep":
        return
    attention(P, C, TPS, chunks, vfn, 64, 96.0 ** -0.5, outfn, 4, "m")


def _scan_consts():
    t = np.arange(128)
    ch = t // 64
    same = (ch[:, None] == ch[None, :])
    out = np.zeros((2, 128, 640), np.float32)
    for d in range(2):
        if d == 0:
            tri = same & (t[:, None] <= t[None, :])
            mid = ch * 64 + 31
            strictT = same & (t[:, None] < t[None, :])
            inclT = same & (t[:, None] <= t[None, :])
        else:
            tri = same & (t[:, None] >= t[None, :])
            mid = ch * 64 + 32
            strictT = same & (t[:, None] > t[None, :])
            inclT = same & (t[:, None] >= t[None, :])
        tri = tri.astype(np.float32)
        tric = tri - tri[:, mid]
        out[d, :, 0:128] = tri
        out[d, :, 128:256] = tric
        out[d, :, 256:384] = strictT
        out[d, :, 384:512] = inclT
        out[d, :, 512:640] = strictT.T
    return out, same.astype(np.float32)


def scan(P, C, sin, cols, yscr, delta, tag):
    W = C.W
    C.scanc = P.alloc([2, 640], F32, "scanc")
    C.bdmask = P.alloc([128], F32, "bdmask")
    C.rowmaskF2 = P.alloc([2, 256], F32, "rowmask")
    C.rowmaskF = C.rowmaskF2[:, :, 0:128]
    P.dma("sp", C.scanc, ext(W["c_scan"]))
    P.dma("sp", C.bdmask, ext(W["c_bd"]))
    P.dma("sp", C.rowmaskF2, ext(W["c_rowmask"]))
    for b in range(NB):
        for d in range(2):
            cst = C.scanc[:, d, :]
            tri, tric = cst[:, 0:128], cst[:, 128:256]
            mG, mL = cst[:, 256:512], cst[:, 512:640]
            Pbd = P.tmp("Pbd" + tag, [2, 128], F32, b * 2 + d)
            P.g("memset", ap=Pbd, constant=0.0)
            for s_ in range(2):
                P.g("memset", ap=P.tmp("sQm" + tag, [3, 4, 128], F32, s_), constant=0.0)
            for i in range(TPS):
                ti = i if d == 0 else TPS - 1 - i
                t = b * TPS + ti
                sl = i % 2
                cd = cols[d]
                names = ["lw", "r", "k", "v"] + (["a", "b"] if delta else [])
                X = {}
                for nm in names:
                    X[nm] = P.tmp("sx" + nm + tag, [256], F32, sl)
                    P.dma("sp", X[nm], sin.t(t, (slice(t * 128, (t + 1) * 128), slice(cd[nm], cd[nm] + 256))))
                psG = P.ps()
                P.mm(psG[:, 0:256], lhsT=tri, rhs=X["lw"])
                psGc = P.ps()
                P.mm(psGc[:, 0:256], lhsT=tric, rhs=X["lw"])
                NQ = 8
                Q = P.tmp("sQ" + tag, [NQ, 256], F32, sl)
                eGn = P.tmp("seGn" + tag, [256], F32, sl)
                P.a("activation", out=Q[:, 6, :], in_=psG[:, 0:256], func=AF.Exp)
                P.a("activation", out=Q[:, 7, :], in_=psGc[:, 0:256], func=AF.Exp)
                P.a("activation", out=eGn, in_=psGc[:, 0:256], func=AF.Exp, scale=-1.0)
                P.v("tensor_tensor", out=Q[:, 1, :], in0=X["r"], in1=Q[:, 7, :], op=ALU.mult)
                P.g("tensor_tensor", out=Q[:, 5, :], in0=X["r"], in1=Q[:, 6, :], op=ALU.mult)
                P.v("tensor_tensor", out=Q[:, 3, :], in0=X["k"], in1=eGn, op=ALU.mult)
                used = [1, 3, 5, 6, 7]
                if delta:
                    gx = P.tmp("sgx" + tag, [2, 256], F32, sl)
                    P.v("tensor_tensor", out=gx[:, 0, :], in0=psGc[:, 0:256], in1=X["lw"], op=ALU.subtract)
                    P.v("tensor_tensor", out=gx[:, 1, :], in0=psG[:, 0:256], in1=X["lw"], op=ALU.subtract)
                    P.a("activation", out=gx[:, 0, :], in_=gx[:, 0, :], func=AF.Exp)
                    P.a("activation", out=gx[:, 1, :], in_=gx[:, 1, :], func=AF.Exp)
                    P.v("scalar_tensor_tensor", out=Q[:, 0, :], in0=X["a"], scalar=-1.0, in1=gx[:, 0, :],
                        op0=ALU.mult, op1=ALU.mult)
                    P.v("scalar_tensor_tensor", out=Q[:, 4, :], in0=X["a"], scalar=-1.0, in1=gx[:, 1, :],
                        op0=ALU.mult, op1=ALU.mult)
                    P.g("tensor_tensor", out=Q[:, 2, :], in0=X["b"], in1=eGn, op=ALU.mult)
                    used = [0, 1, 2, 3, 4, 5, 6, 7]
                if CUT < 1:
                    continue
                FT = P.tmp("sFT" + tag, [NQ, 2, 128], F32, sl)
                j = 0
                pend = []
                for q in used:
                    for pr in range(2):
                        if j % 4 == 0:
                            pst = P.ps()
                        P.tr(pst[:, (j % 4) * 128:(j % 4 + 1) * 128], Q[:, q, pr * 128:(pr + 1) * 128], C.identf,
                             inc=(j % 4 == 3 or (q == used[-1] and pr == 1)))
                        pend.append((q, pr, pst, j % 4))
                        j += 1
                FTm = P.tmp("sFTm" + tag, [3, 2, 2, 128], F32, sl)
                for idx, (q, pr, pst, jj) in enumerate(pend):
                    if idx % 2 == 0:
                        P.v("tensor_copy", out=FT[:, q, pr, :], in_=pst[:, jj * 128:(jj + 1) * 128])
                    else:
                        P.a("copy", out=FT[:, q, pr, :], in_=pst[:, jj * 128:(jj + 1) * 128])
                Qm = P.tmp("sQm" + tag, [3, 4, 128], F32, sl)
                mq = [0, 1, 2] if delta else [1]
                for q in mq:
                    for hh in range(2):
                        dst = Qm[:, q, :, :].re("p (pr hh) c -> p pr hh c", hh=2)[:, :, hh, hh * 64:(hh + 1) * 64]
                        src = Q[:, q, :].re("p (pr hh d) -> p pr hh d", pr=2, hh=2)[:, :, hh, :]
                        if hh == 0:
                            P.v("tensor_copy", out=dst, in_=src)
                        else:
                            P.g("tensor_copy", out=dst, in_=src)
                j = 0
                pend2 = []
                for q in mq:
                    for ph in range(4):
                        if j % 4 == 0:
                            pst = P.ps()
                        P.tr(pst[:, (j % 4) * 128:(j % 4 + 1) * 128], Qm[:, q, ph, :], C.identf, inc=(j % 4 == 3))
                        pend2.append((q, ph, pst, j % 4))
                        j += 1
                for idx, (q, ph, pst, jj) in enumerate(pend2):
                    if idx % 2 == 0:
                        P.v("tensor_copy", out=FTm[:, q, ph // 2, ph % 2, :], in_=pst[:, jj * 128:(jj + 1) * 128])
                    else:
                        P.a("copy", out=FTm[:, q, ph // 2, ph % 2, :], in_=pst[:, jj * 128:(jj + 1) * 128])
                Vm = P.tmp("sVm" + tag, [2, 256], F32, sl)
                for cc in range(2 if os.environ.get("VAR", "") != "b" else 0):
                    P.v("tensor_tensor", out=Vm[:, cc, :], in0=X["v"], in1=C.rowmaskF[:, cc, :].un(1).bc([128, 2, 128]).re("p a b -> p (a b)") if False else C.rowmaskF2[:, cc, :], op=ALU.mult)

                def fth(q, h):
                    return FT[64 * (h % 2):64 * (h % 2) + 64, q, h // 2, :]

                if CUT < 2:
                    continue
                MrkT = P.tmp("sMrk" + tag, [4, 128], F32, sl)
                if delta:
                    GA = P.tmp("sGA" + tag, [4, 256], F32, 0)
                    GB = P.tmp("sGB" + tag, [4, 256], F32, 0)
                    Lab = P.tmp("sLab" + tag, [4, 128], F32, 0)
                    for (src, dstT) in ((2, GA), (3, GB)):
                        for hp in range(2):
                            ps = P.ps()
                            for hh in range(2):
                                h = hp * 2 + hh
                                rhs = FTm[:, 0:2, hp, hh, :]
                                P.mm(ps[:, hh * 256:(hh + 1) * 256], lhsT=FT[:, src, hp, :], rhs=rhs,
                                     start=True, stop=True, inc=(hh == 1))
                            P.v("tensor_tensor", out=dstT[:, hp * 2:hp * 2 + 2, :],
                                in0=ps.re("p (h c) -> p h c", h=2), in1=mG.un(1).bc([128, 2, 256]), op=ALU.mult)
                    ps = P.ps()
                    for h in range(4):
                        P.mm(ps[:, h * 128:(h + 1) * 128], lhsT=FT[:, 0, h // 2, :], rhs=FTm[:, 2, h // 2, h % 2, :],
                             start=True, stop=True, inc=(h == 3))
                    P.v("tensor_tensor", out=Lab, in0=ps.re("p (h c) -> p h c", h=4),
                        in1=mL.un(1).bc([128, 4, 128]), op=ALU.mult)
                    A_l = [Lab]
                    AT_l = [GA[:, :, 0:128]]
                    for lv in range(1, 6):
                        An = P.tmp(f"sA{lv}" + tag, [4, 128], F32, 0)
                        ATn = P.tmp(f"sAT{lv}" + tag, [4, 128], F32, 0)
                        ps1 = P.ps()
                        ps2 = P.ps()
                        for h in range(4):
                            P.mm(ps1[:, h * 128:(h + 1) * 128], lhsT=A_l[-1][:, h, :], rhs=AT_l[-1][:, h, :],
                                 start=True, stop=True, inc=(h == 3))
                        for h in range(4):
                            P.mm(ps2[:, h * 128:(h + 1) * 128], lhsT=AT_l[-1][:, h, :], rhs=A_l[-1][:, h, :],
                                 start=True, stop=True, inc=(h == 3))
                        P.v("tensor_copy", out=ATn, in_=ps1.re("p (h c) -> p h c", h=4))
                        P.a("copy", out=An.re("p h c -> p (h c)"), in_=ps2)
                        A_l.append(An)
                        AT_l.append(ATn)
                    MrbT = GA[:, :, 128:256]
                    LakT = GB[:, :, 0:128]
                    MrkTv = GB[:, :, 128:256]
                else:
                    ps = P.ps()
                    for h in range(4):
                        P.mm(ps[:, h * 128:(h + 1) * 128], lhsT=FT[:, 3, h // 2, :], rhs=FTm[:, 1, h // 2, h % 2, :],
                             start=True, stop=True, inc=(h == 3))
                    P.v("tensor_tensor", out=MrkT, in0=ps.re("p (h c) -> p h c", h=4),
                        in1=mG[:, 128:256].un(1).bc([128, 4, 128]), op=ALU.mult)
                    MrkTv = MrkT
                if CUT < 3:
                    continue
                Y = P.tmp("sY" + tag, [256], F32, sl)
                V = X["v"]
                for ci in ((0, 1) if d == 0 else (1, 0)):
                    p0 = 64 * ci
                    clast = p0 + 63 if d == 0 else p0
                    if delta:
                        U = P.tmp("sU" + tag, [256], F32, ci)
                        psx = P.ps()
                        for pr in range(2):
                            P.mm(psx[:, pr * 128:(pr + 1) * 128], lhsT=FT[:, 4, pr, :], rhs=Pbd[:, pr, :],
                                 start=True, stop=False, inc=False)
                            for hh in range(2):
                                h = pr * 2 + hh
                                P.mm(psx[:, h * 64:(h + 1) * 64], lhsT=LakT[:, h, :], rhs=V[:, h * 64:(h + 1) * 64],
                                     start=False, stop=(hh == 1), inc=(hh == 1 and pr == 1))
                        P.v("tensor_copy", out=U, in_=psx[:, 0:256])
                        for lv in range(6):
                            psu = P.ps()
                            for h in range(4):
                                P.mm(psu[:, h * 64:(h + 1) * 64], lhsT=AT_l[lv][:, h, :], rhs=U[:, h * 64:(h + 1) * 64],
                                     start=True, stop=True, inc=(h == 3))
                            P.v("tensor_tensor", out=U, in0=U, in1=psu[:, 0:256], op=ALU.add)
                    psy = P.ps()
                    for pr in range(2):
                        P.mm(psy[:, pr * 128:(pr + 1) * 128], lhsT=FT[:, 5, pr, :], rhs=Pbd[:, pr, :],
                             start=True, stop=False, inc=False)
                        for hh in range(2):
                            h = pr * 2 + hh
                            last = (hh == 1)
                            if delta:
                                P.mm(psy[:, h * 64:(h + 1) * 64], lhsT=MrbT[:, h, :], rhs=U[:, h * 64:(h + 1) * 64],
                                     start=False, stop=False, inc=False)
                            P.mm(psy[:, h * 64:(h + 1) * 64], lhsT=MrkTv[:, h, :], rhs=V[:, h * 64:(h + 1) * 64],
                                 start=False, stop=last, inc=(last and pr == 1))
                    P.v("tensor_copy", out=Y[p0:p0 + 64, :], in_=psy[p0:p0 + 64, 0:256])
                    if delta:
                        Um = P.tmp("sUm" + tag, [256], F32, ci)
                        P.v("tensor_tensor", out=Um, in0=U, in1=C.rowmaskF2[:, ci, :], op=ALU.mult)
                    if CUT < 4:
                        continue
                    for pr in range(2):
                        psp = P.ps()
                        cs = slice(pr * 128, (pr + 1) * 128)
                        if delta:
                            P.mm(psp[:, 0:128], lhsT=Q[:, 2, cs], rhs=Um[:, cs], start=True, stop=False, inc=False)
                        P.mm(psp[:, 0:128], lhsT=Q[:, 3, cs], rhs=Vm[:, ci, cs], start=(not delta), stop=True)
                        t1 = P.tmp("st1" + tag, [128], F32, pr)
                        P.v("tensor_scalar", out=t1, in0=psp[:, 0:128], scalar1=FT[:, 7, pr, clast:clast + 1], scalar2=None,
                            op0=ALU.mult)
                        P.g("tensor_tensor", out=t1, in0=t1, in1=C.bdmask, op=ALU.mult)
                        P.v("scalar_tensor_tensor", out=Pbd[:, pr, :], in0=Pbd[:, pr, :],
                            scalar=FT[:, 6, pr, clast:clast + 1], in1=t1, op0=ALU.mult, op1=ALU.add)
                if CUT < 5:
                    continue
                P.dma("sp", yscr.t((d, t), (slice(d * NT + t * 128, d * NT + (t + 1) * 128),)), Y)


def bc_load(P, name, src_ap, n):
    tl = P.alloc([n], F32, name)
    P.dma("sp", tl, ext(src_ap.partition_broadcast(128)))
    return tl


def stage_rwkv(P, C, W, l, hbuf, mixd, sin, aux, yscr):
    P.phase()
    mp = bc_load(P, "mp", W["rwkv_mu_prev"][l], 960)
    mn = bc_load(P, "mn", W["rwkv_mu_next"][l], 960)
    w0 = bc_load(P, "w0", W["rwkv_w0"][l].rearrange("a b -> (a b)"), 512)
    a0 = bc_load(P, "a0", W["rwkv_a0"][l].rearrange("a b -> (a b)"), 512)
    kk_ = bc_load(P, "k_k", W["rwkv_k_k"][l], 256)
    ka = bc_load(P, "k_a", W["rwkv_k_a"][l], 256)
    rk = bc_load(P, "r_k", W["rwkv_r_k"][l].rearrange("a b -> (a b)"), 256)
    omk = P.alloc([256], F32, "omk")
    P.v("tensor_scalar", out=omk, in0=ka, scalar1=-1.0, scalar2=1.0, op0=ALU.mult, op1=ALU.add)
    Wblk = P.alloc([1024], F32, "Wblk")
    G2 = P.alloc([256], F32, "G2")
    P.g("memset", ap=Wblk, constant=0.0)
    P.g("memset", ap=G2, constant=0.0)
    for dd in range(2):
        P.dma("sp", Wblk[32 * dd:32 * dd + 32, dd * 256:(dd + 1) * 256], ext(W["rwkv_w2"][l, dd]))
        P.dma("sp", Wblk[64 + 32 * dd:96 + 32 * dd, 512 + dd * 256:512 + (dd + 1) * 256], ext(W["rwkv_a2"][l, dd]))
    P.dma("sp", G2[0:64, :], ext(W["rwkv_g2"][l]))
    tiny = P.alloc([1], F32, "tiny")
    for sl in range(2):
        P.g("memset", ap=P.tmp("lo", [256], F32, sl), constant=0.0)
    o0 = OFF_RWKV
    for t in range(TL[0]):
        sl = t % 2
        ts = t % TPS
        r0 = t * 128
        zc = P.tmp("zc", [960], F32, sl)
        zp = P.tmp("zp", [960], F32, sl)
        zn = P.tmp("zn", [960], F32, sl)
        P.dma("sp", zc, hbuf.t(t, (slice(r0, r0 + 128), slice(o0, o0 + 960))))
        if ts == 0:
            P.g("memset", ap=zp, constant=0.0)
            P.dma("sp", zp[1:128, :], hbuf.t(t, (slice(r0, r0 + 127), slice(o0, o0 + 960))))
        else:
            P.dma("sp", zp, hbuf.t([t - 1, t], (slice(r0 - 1, r0 + 127), slice(o0, o0 + 960))))
        if ts == TPS - 1:
            P.g("memset", ap=zn, constant=0.0)
            P.dma("sp", zn[0:127, :], hbuf.t(t, (slice(r0 + 1, r0 + 128), slice(o0, o0 + 960))))
        else:
            P.dma("sp", zn, hbuf.t([t, t + 1], (slice(r0 + 1, r0 + 129), slice(o0, o0 + 960))))
        P.v("tensor_tensor", out=zp, in0=zp, in1=zc, op=ALU.subtract)
        P.g("tensor_tensor", out=zn, in0=zn, in1=zc, op=ALU.subtract)
        P.v("tensor_tensor", out=zp, in0=zp, in1=mp, op=ALU.mult)
        P.g("tensor_tensor", out=zn, in0=zn, in1=mn, op=ALU.mult)
        P.v("tensor_tensor", out=zc, in0=zc, in1=zp, op=ALU.add)
        P.v("tensor_tensor", out=zc, in0=zc, in1=zn, op=ALU.add)
        zf = zc
        O = P.tmp("rwO", [2304], F32, sl)
        AX_ = P.tmp("rwA", [260], F32, sl)
        lo = P.tmp("lo", [256], F32, sl)
        P.a("activation", out=lo[:, 0:64], in_=zf[:, 768:832], func=AF.Tanh)
        P.g("tensor_copy", out=lo[:, 64:128], in_=zf[:, 832:896])
        P.a("activation", out=lo[:, 128:192], in_=zf[:, 896:960], func=AF.Sigmoid)
        pst = P.ps()
        P.tr(pst[:, 0:128], lo[:, 0:128], C.identf, inc=False)
        P.tr(pst[:, 128:256], lo[:, 128:256], C.identf)
        loT = P.tmp("loT", [256], F32, sl)
        P.v("tensor_copy", out=loT, in_=pst[:, 0:256])
        psw = P.ps()
        P.mm(psw, lhsT=loT[:, 0:128], rhs=Wblk[:, 0:512])
        psa = P.ps()
        P.mm(psa, lhsT=loT[:, 0:128], rhs=Wblk[:, 512:1024])
        psg = P.ps()
        P.mm(psg[:, 0:256], lhsT=loT[:, 128:256], rhs=G2)
        wr = P.tmp("wr", [512], F32, sl)
        ar = P.tmp("ar", [512], F32, sl)
        P.v("tensor_tensor", out=wr, in0=psw, in1=w0, op=ALU.add)
        P.a("activation", out=wr, in_=wr, func=AF.Sigmoid)
        P.v("tensor_scalar", out=O[:, 0:512], in0=wr, scalar1=-math.exp(-0.5), scalar2=None, op0=ALU.mult)
        P.v("tensor_tensor", out=ar, in0=psa, in1=a0, op=ALU.add)
        P.a("activation", out=ar, in_=ar, func=AF.Sigmoid)
        P.a("copy", out=AX_[:, 0:256], in_=psg[:, 0:256])
        r_ = zf[:, 0:256]
        k_ = zf[:, 256:512]
        v_ = zf[:, 512:768]
        P.g("tensor_copy", out=O[:, 512:768], in_=r_)
        P.g("tensor_copy", out=O[:, 1280:1536], in_=v_)
        kkr = P.tmp("kkr", [256], F32, sl)
        sq = P.tmp("rsq", [256], F32, sl)
        st = P.tmp("rst", [16], F32, sl)
        P.v("tensor_tensor", out=kkr, in0=k_, in1=kk_, op=ALU.mult)
        P.a("activation", out=sq, in_=kkr, func=AF.Square)
        P.v("tensor_reduce", out=st[:, 0:4], in_=sq.re("p (h d) -> p h d", d=64), axis=AX.X, op=ALU.add)
        P.a("activation", out=st[:, 4:8], in_=st[:, 0:4], func=AF.Sqrt)
        P.v("tensor_scalar", out=st[:, 4:8], in0=st[:, 4:8], scalar1=1e-12, scalar2=None, op0=ALU.max)
        P.v("reciprocal", out=st[:, 8:12], in_=st[:, 4:8])
        P.v("tensor_tensor", out=O[:, 1536:1792].re("p (h d) -> p h d", d=64), in0=kkr.re("p (h d) -> p h d", d=64),
            in1=st[:, 8:12].un(2).bc([128, 4, 64]), op=ALU.mult)
        for dd in range(2):
            tmp = P.tmp("rtmp", [256], F32, dd)
            P.v("tensor_tensor", out=tmp, in0=ar[:, dd * 256:(dd + 1) * 256], in1=ka, op=ALU.mult)
            P.g("tensor_tensor", out=tmp, in0=tmp, in1=omk, op=ALU.add)
            P.v("tensor_tensor", out=O[:, 768 + dd * 256:1024 + dd * 256], in0=tmp, in1=k_, op=ALU.mult)
            P.g("tensor_tensor", out=O[:, 1792 + dd * 256:2048 + dd * 256], in0=ar[:, dd * 256:(dd + 1) * 256],
                in1=O[:, 1536:1792], op=ALU.mult)
        bt = P.tmp("rbt", [256], F32, sl)
        P.v("tensor_tensor", out=bt, in0=O[:, 768:1024], in1=O[:, 1024:1280], op=ALU.add)
        P.v("tensor_tensor", out=bt, in0=bt, in1=r_, op=ALU.mult)
        P.v("tensor_tensor", out=bt, in0=bt, in1=rk, op=ALU.mult)
        P.v("tensor_reduce", out=AX_[:, 256:260], in_=bt.re("p (h d) -> p h d", d=64), axis=AX.X, op=ALU.add)
        P.dma("sp", sin.t(t, (slice(r0, r0 + 128), slice(0, 2304))), O)
        P.dma("sp", aux.t(t, (slice(r0, r0 + 128), slice(0, 260))), AX_)
    if C.stop == "rwkvprep":
        return
    P.phase()
    cols = [dict(lw=0, r=512, k=768, v=1280, a=1536, b=1792), dict(lw=256, r=512, k=1024, v=1280, a=1536, b=2048)]
    scan(P, C, sin, cols, yscr, True, "w")
    P.phase()
    gng = bc_load(P, "gng", W["rwkv_gn_g"][l], 256)
    gnb = bc_load(P, "gnb", W["rwkv_gn_b"][l], 256)
    epsg = P.alloc([1], F32, "epsg")
    P.g("memset", ap=epsg, constant=64e-5)
    for t in range(TL[0]):
        sl = t % 2
        r0 = t * 128
        y0 = P.tmp("fy0", [256], F32, sl)
        y1 = P.tmp("fy1", [256], F32, sl)
        vv = P.tmp("fv", [256], F32, sl)
        ax = P.tmp("fax", [260], F32, sl)
        P.dma("sp", y0, yscr.t((0, t), (slice(r0, r0 + 128),)))
        P.dma("sp", y1, yscr.t((1, t), (slice(NT + r0, NT + r0 + 128),)))
        P.dma("sp", vv, sin.t(t, (slice(r0, r0 + 128), slice(1280, 1536))))
        P.dma("sp", ax, aux.t(t, (slice(r0, r0 + 128), slice(0, 260))))
        st = P.tmp("fst", [16], F32, sl)
        sq = P.tmp("fsq", [256], F32, sl)
        P.v("tensor_tensor", out=y0, in0=y0, in1=y1, op=ALU.add)
        y3 = y0.re("p (h d) -> p h d", d=64)
        P.v("tensor_reduce", out=st[:, 0:4], in_=y3, axis=AX.X, op=ALU.add)
        P.v("tensor_scalar", out=st[:, 0:4], in0=st[:, 0:4], scalar1=1.0 / 64.0, scalar2=None, op0=ALU.mult)
        P.v("tensor_tensor", out=y3, in0=y3, in1=st[:, 0:4].un(2).bc([128, 4, 64]), op=ALU.subtract)
        P.a("activation", out=sq, in_=y0, func=AF.Square)
        P.v("tensor_reduce", out=st[:, 4:8], in_=sq.re("p (h d) -> p h d", d=64), axis=AX.X, op=ALU.add)
        P.a("activation", out=st[:, 8:12], in_=st[:, 4:8], func=AF.Sqrt, scale=1.0 / 64.0, bias=epsg[:, 0:1])
        P.v("reciprocal", out=st[:, 12:16], in_=st[:, 8:12])
        P.v("tensor_tensor", out=y3, in0=y3, in1=st[:, 12:16].un(2).bc([128, 4, 64]), op=ALU.mult)
        P.g("tensor_tensor", out=y0, in0=y0, in1=gng, op=ALU.mult)
        P.g("tensor_tensor", out=y0, in0=y0, in1=gnb, op=ALU.add)
        P.v("tensor_tensor", out=vv.re("p (h d) -> p h d", d=64), in0=vv.re("p (h d) -> p h d", d=64),
            in1=ax[:, 256:260].un(2).bc([128, 4, 64]), op=ALU.mult)
        P.v("tensor_tensor", out=y0, in0=y0, in1=vv, op=ALU.add)
        P.v("tensor_tensor", out=y0, in0=y0, in1=ax[:, 0:256], op=ALU.mult)
        P.dma("sp", mixd.t((t, "rwkv"), (slice(r0, r0 + 128), slice(256, 512))), y0)


def stage_hgrn(P, C, W, l, hbuf, mixd, sin, aux, yscr):
    P.phase()
    lb = P.alloc([256], F32, "lb")
    oml = P.alloc([256], F32, "oml")
    if l == 0:
        P.g("memset", ap=lb, constant=0.0)
    else:
        l0 = bc_load(P, "lbl0", W["hgrn_lb_logits"][0], 256)
        l1 = bc_load(P, "lbl1", W["hgrn_lb_logits"][1], 256)
        P.v("tensor_tensor", out=l1, in0=l1, in1=l0, op=ALU.subtract)
        P.a("activation", out=lb, in_=l1, func=AF.Sigmoid)
    P.v("tensor_scalar", out=oml, in0=lb, scalar1=-1.0, scalar2=1.0, op0=ALU.mult, op1=ALU.add)
    o0 = OFF_HGRN
    for t in range(TL[0]):
        sl = t % 2
        r0 = t * 128
        z = P.tmp("hz", [1280], F32, sl)
        P.dma("sp", z, hbuf.t(t, (slice(r0, r0 + 128), slice(o0, o0 + 1280))))
        O = P.tmp("hO", [1536], F32, sl)
        P.a("activation", out=O[:, 512:768], in_=z[:, 0:256], func=AF.Silu)
        sg = P.tmp("hsg", [512], F32, sl)
        P.a("activation", out=sg, in_=z[:, 256:768], func=AF.Sigmoid)
        for dd in range(2):
            s_ = sg[:, dd * 256:(dd + 1) * 256]
            P.v("tensor_tensor", out=s_, in0=s_, in1=oml, op=ALU.mult)
            P.g("tensor_tensor", out=s_, in0=s_, in1=lb, op=ALU.add)
        P.a("activation", out=O[:, 0:512], in_=sg, func=AF.Ln)
        P.v("tensor_scalar", out=O[:, 768:1280], in0=sg, scalar1=-1.0, scalar2=1.0, op0=ALU.mult, op1=ALU.add)
        P.g("tensor_copy", out=O[:, 1280:1536], in_=z[:, 768:1024])
        P.dma("sp", sin.t(t, (slice(r0, r0 + 128), slice(0, 1536))), O)
        P.dma("sp", aux.t(t, (slice(r0, r0 + 128), slice(0, 256))), z[:, 1024:1280])
    if C.stop == "hgrnprep":
        return
    P.phase()
    cols = [dict(lw=0, r=512, k=768, v=1280), dict(lw=256, r=512, k=1024, v=1280)]
    scan(P, C, sin, cols, yscr, False, "h")
    if CUT < 6:
        return
    P.phase()
    ng = bc_load(P, "hng", W["hgrn_norm_g"][l], 256)
    epsr = P.alloc([1], F32, "epsh")
    P.g("memset", ap=epsr, constant=RMS_EPS)
    for t in range(TL[0]):
        sl = t % 2
        r0 = t * 128
        y0 = P.tmp("gy0", [256], F32, sl)
        y1 = P.tmp("gy1", [256], F32, sl)
        gg = P.tmp("gg", [256], F32, sl)
        P.dma("sp", y0, yscr.t((0, t), (slice(r0, r0 + 128),)))
        P.dma("sp", y1, yscr.t((1, t), (slice(NT + r0, NT + r0 + 128),)))
        P.dma("sp", gg, aux.t(t, (slice(r0, r0 + 128), slice(0, 256))))
        st = P.tmp("gst", [12], F32, sl)
        sq = P.tmp("gsq", [256], F32, sl)
        P.v("tensor_tensor", out=y0, in0=y0, in1=y1, op=ALU.add)
        P.a("activation", out=sq, in_=y0, func=AF.Square)
        P.v("tensor_reduce", out=st[:, 0:4], in_=sq.re("p (h d) -> p h d", d=64), axis=AX.X, op=ALU.add)
        P.a("activation", out=st[:, 4:8], in_=st[:, 0:4], func=AF.Sqrt, scale=1.0 / 64.0, bias=epsr[:, 0:1])
        P.v("reciprocal", out=st[:, 8:12], in_=st[:, 4:8])
        y3 = y0.re("p (h d) -> p h d", d=64)
        P.v("tensor_tensor", out=y3, in0=y3, in1=st[:, 8:12].un(2).bc([128, 4, 64]), op=ALU.mult)
        P.g("tensor_tensor", out=y0, in0=y0, in1=ng, op=ALU.mult)
        P.a("activation", out=gg, in_=gg, func=AF.Silu)
        P.v("tensor_tensor", out=y0, in0=y0, in1=gg, op=ALU.mult)
        P.dma("sp", mixd.t((t, "hgrn"), (slice(r0, r0 + 128), slice(512, 768))), y0)


def stage_proj_ln(P, C, W, load_src, Kdim, w_ap, g_ap, b_ap, xres, tag):
    P.phase()
    nk = Kdim // 128
    wsb = P.alloc([nk, D], BF16, "w" + tag)
    load_cast(P, wsb, w_ap.rearrange("(kc p) n -> p kc n", p=128))
    g_bc = bc_load(P, "g" + tag, g_ap, D)
    b_bc = bc_load(P, "b" + tag, b_ap, D)
    for t in range(TL[0]):
        sl = t % 2
        src = load_src(t, sl)
        sT = P.tmp("sT" + tag, [nk, 128], BF16, sl)
        transpose_to(P, C, src, Kdim, lambda kc, r: sT[:, kc, :])
        xin = P.tmp("rx" + tag, [D], F32, sl)
        P.dma("sp", xin, xres.t(t, rows(t)))
        pre = P.tmp("pre" + tag, [D], F32, sl)
        for half in range(2):
            ps = P.ps()
            for kc in range(nk):
                P.mm(ps, lhsT=sT[:, kc, :], rhs=wsb[:, kc, half * 512:(half + 1) * 512], start=(kc == 0),
                     stop=(kc == nk - 1))
            P.v("scalar_tensor_tensor", out=pre[:, half * 512:(half + 1) * 512], in0=xin[:, half * 512:(half + 1) * 512],
                scalar=DN_ALPHA, in1=ps, op0=ALU.mult, op1=ALU.add)
        xo = P.tmp("xo" + tag, [D], F32, sl)
        layer_norm_tile(P, C, pre, g_bc, b_bc, xo, sl)
        P.dma("sp", xres.t(t, rows(t)), xo)
        to_xT(P, C, xo, t, sl)


def stage_wout(P, C, W, l, mixd, xres):
    def load_src(t, sl):
        m = P.tmp("mixf", [D], F32, sl)
        keys = [(t, n) for n in ("att", "rwkv", "hgrn", "mla")]
        P.dma("sp", m, mixd.t(keys, rows(t)))
        mb = P.tmp("mixb", [D], BF16, sl)
        P.a("copy", out=mb, in_=m)
        return mb
    stage_proj_ln(P, C, W, load_src, D, W["w_out"][l], W["ln1_g"][l], W["ln1_b"][l], xres, "wo")


def stage_cross(P, C, W, l, xres, catt):
    P.phase()
    KTm = P.alloc([8, NB * 256], BF16, "KTm")
    VSm = P.alloc([NB * 2, 4, 257], BF16, "VSm")
    QTm = P.alloc([8, NT], BF16, "QTm")
    P.g("memset", ap=VSm, constant=1.0)
    mark = P.aoff
    wkv = P.alloc([8, 2 * D], BF16, "wkv")
    load_cast(P, wkv, W["mem_wkv"][l].rearrange("(kc p) n -> p kc n", p=128))
    memT = P.alloc([8, NB * 256], BF16, "memT")
    for mt in range(NB * 2):
        sl = mt % 2
        mf = P.tmp("memf", [D], F32, sl)
        P.dma("sp", mf, ext(W["mem"][mt * 128:(mt + 1) * 128, :]))
        mb = P.tmp("memb", [D], BF16, sl)
        P.a("copy", out=mb, in_=mf)
        transpose_to(P, C, mb, D, lambda kc, r: memT[:, kc, mt * 128:(mt + 1) * 128])
    for hc in range(8):
        ps = P.ps()
        for kc in range(8):
            P.mm(ps, lhsT=wkv[:, kc, hc * 128:(hc + 1) * 128], rhs=memT[:, kc, :], start=(kc == 0), stop=(kc == 7))
        P.v("tensor_copy", out=KTm[:, hc, :], in_=ps)
    for mt in range(NB * 2):
        for half in range(2):
            ps = P.ps()
            for kc in range(8):
                P.mm(ps, lhsT=memT[:, kc, mt * 128:(mt + 1) * 128], rhs=wkv[:, kc, D + half * 512:D + (half + 1) * 512],
                     start=(kc == 0), stop=(kc == 7))
            P.v("tensor_copy", out=VSm[:, mt, half * 2:half * 2 + 2, 0:256], in_=ps.re("p (h d) -> p h d", h=2))
    P.barrier()
    P.aoff = mark
    P.cache = {}
    wq = P.alloc([8, D], BF16, "wq")
    load_cast(P, wq, W["mem_wq"][l].rearrange("(kc p) n -> p kc n", p=128))
    it = 0
    for hc in range(8):
        for tb in range(TL[0] // 4):
            ps = P.ps()
            for kc in range(8):
                P.mm(ps, lhsT=wq[:, kc, hc * 128:(hc + 1) * 128], rhs=C.XT[:, kc, tb * 512:(tb + 1) * 512],
                     start=(kc == 0), stop=(kc == 7))
            if it % 2 == 0:
                P.v("tensor_copy", out=QTm[:, hc, tb * 512:(tb + 1) * 512], in_=ps)
            else:
                P.a("copy", out=QTm[:, hc, tb * 512:(tb + 1) * 512], in_=ps)
            it += 1
    P.barrier()
    P.aoff = mark
    P.cache = {}

    def chunks(b, h):
        return [(KTm[:, h * 2 + c, b * 256:(b + 1) * 256], QTm[:, h * 2 + c, b * S:(b + 1) * S]) for c in range(2)]

    def vfn(b, h, kt):
        return VSm[:, b * 2 + kt, h, :]

    def outfn(b, h, qb, o_sb):
        r0 = b * S + qb * 512
        keys = [(r0 // 128 + i, h) for i in range(4)]
        dst = catt.t(keys, (slice(r0, r0 + 512), slice(h * 256, (h + 1) * 256))).re("(q p) d -> p q d", p=128)
        P.dma("sp", dst, o_sb)

    attention(P, C, 2, chunks, vfn, 256, 1.0 / 16.0, outfn, 4, "c")

    def load_src(t, sl):
        m = P.tmp("caf", [D], F32, sl)
        P.dma("sp", m, catt.t([(t, h) for h in range(4)], rows(t)))
        mb = P.tmp("cab", [D], BF16, sl)
        P.a("copy", out=mb, in_=m)
        return mb
    stage_proj_ln(P, C, W, load_src, D, W["mem_wo"][l], W["ln2_g"][l], W["ln2_b"][l], xres, "co")


EG = 4


def stage_moe(P, C, W, l, xres, yacc):
    P.phase()
    gates = P.alloc([NTILE, 32], F32, "gates")
    wg = P.alloc([8, 36], F32, "wg")
    P.dma("sp", wg[:, :, 0:4], ext(W["moe_wg_group"][l].rearrange("(kc p) n -> p kc n", p=128)))
    P.dma("sp", wg[:, :, 4:36], ext(W["moe_wg_expert"][l].rearrange("(kc p) n -> p kc n", p=128)))
    bg = P.alloc([36], F32, "bg")
    P.dma("sp", bg[:, 0:4], ext(W["moe_bg_group"][l].partition_broadcast(128)))
    P.dma("sp", bg[:, 4:36], ext(W["moe_bg_expert"][l].partition_broadcast(128)))
    mark = P.aoff
    BIG = 1.0e30
    for t in range(TL[0]):
        sl = t % 2
        xf = P.tmp("mxf", [D], F32, sl)
        P.dma("sp", xf, xres.t(t, rows(t)))
        xTf = P.tmp("mxT", [8, 128], F32, sl)
        for half in range(2):
            pst = P.ps()
            for j in range(4):
                kc = half * 4 + j
                P.tr(pst[:, j * 128:(j + 1) * 128], xf[:, kc * 128:(kc + 1) * 128], C.identf, inc=(j == 3))
            P.v("tensor_copy", out=xTf[:, half * 4:half * 4 + 4, :], in_=pst.re("p (a b) -> p a b", a=4))
        psl = P.ps()
        for kc in range(8):
            P.mm(psl[:, 0:36], lhsT=xTf[:, kc, :], rhs=wg[:, kc, :], start=(kc == 0), stop=(kc == 7))
        lg = P.tmp("mlg", [36], F32, sl)
        P.v("tensor_tensor", out=lg, in0=psl[:, 0:36], in1=bg, op=ALU.add)
        st = P.tmp("mst", [16], F32, sl)
        gm = P.tmp("mgm", [4], F32, sl)
        junk = P.tmp("mjk", [4], F32, sl)
        P.v("tensor_reduce", out=st[:, 0:1], in_=lg[:, 0:4], axis=AX.X, op=ALU.max)
        P.v("tensor_scalar", out=gm, in0=lg[:, 0:4], scalar1=st[:, 0:1], scalar2=None, op0=ALU.is_ge)
        P.v("tensor_scalar", out=st[:, 1:2], in0=st[:, 0:1], scalar1=-1.0, scalar2=None, op0=ALU.mult)
        P.a("activation", out=junk, in_=lg[:, 0:4], func=AF.Exp, bias=st[:, 1:2], scale=1.0, accum_out=st[:, 2:3])
        P.v("reciprocal", out=st[:, 3:4], in_=st[:, 2:3])
        em = P.tmp("mem_", [32], F32, sl)
        pen = P.tmp("mpen", [4], F32, sl)
        P.v("tensor_scalar", out=pen, in0=gm, scalar1=-1.0, scalar2=BIG, op0=ALU.add, op1=ALU.mult)
        P.v("tensor_tensor", out=em.re("p (g e) -> p g e", g=4), in0=lg[:, 4:36].re("p (g e) -> p g e", g=4),
            in1=pen.un(2).bc([128, 4, 8]), op=ALU.add)
        m1 = P.tmp("mm1", [32], F32, sl)
        m2 = P.tmp("mm2", [32], F32, sl)
        em2 = P.tmp("mem2", [32], F32, sl)
        P.v("tensor_reduce", out=st[:, 4:5], in_=em, axis=AX.X, op=ALU.max)
        P.v("tensor_scalar", out=m1, in0=em, scalar1=st[:, 4:5], scalar2=None, op0=ALU.is_ge)
        P.v("scalar_tensor_tensor", out=em2, in0=m1, scalar=-BIG, in1=em, op0=ALU.mult, op1=ALU.add)
        P.v("tensor_reduce", out=st[:, 5:6], in_=em2, axis=AX.X, op=ALU.max)
        P.v("tensor_scalar", out=m2, in0=em2, scalar1=st[:, 5:6], scalar2=None, op0=ALU.is_ge)
        P.v("tensor_tensor", out=st[:, 6:7], in0=st[:, 5:6], in1=st[:, 4:5], op=ALU.subtract)
        P.a("activation", out=st[:, 7:8], in_=st[:, 6:7], func=AF.Exp)
        P.v("tensor_scalar", out=st[:, 8:9], in0=st[:, 7:8], scalar1=1.0, scalar2=None, op0=ALU.add)
        P.v("reciprocal", out=st[:, 9:10], in_=st[:, 8:9])
        P.v("tensor_tensor", out=st[:, 10:11], in0=st[:, 7:8], in1=st[:, 9:10], op=ALU.mult)
        P.v("tensor_tensor", out=st[:, 9:11], in0=st[:, 9:11], in1=st[:, 3:4].bc([128, 2]), op=ALU.mult)
        P.v("tensor_scalar", out=m1, in0=m1, scalar1=st[:, 9:10], scalar2=None, op0=ALU.mult)
        P.v("scalar_tensor_tensor", out=gates[:, t, :], in0=m2, scalar=st[:, 10:11], in1=m1, op0=ALU.mult, op1=ALU.add)
    if C.stop == "moegate":
        C.gates = gates
        return
    P.barrier()
    P.aoff = mark
    P.cache = {}
    P.nrot = 6
    ngrp = 32 // EG
    for g in range(ngrp):
        gs = g % 2
        w13 = P.tmp("w13", [EG, 8, 512], BF16, gs)
        w2 = P.tmp("w2", [EG, 2, D], BF16, gs)
        for e in range(EG):
            ee = g * EG + e
            P.dma("pool", w13[:, e, :, 0:256], ext(W["moe_w1"][l, ee].rearrange("(kc p) n -> p kc n", p=128)))
            P.dma("pool", w13[:, e, :, 256:512], ext(W["moe_w3"][l, ee].rearrange("(kc p) n -> p kc n", p=128)))
            P.dma("pool", w2[:, e, :, :], ext(W["moe_w2"][l, ee].rearrange("(c p) n -> p c n", p=128)))
        for t in range(TL[0]):
            sl = t % 2
            psy = [P.psum[6], P.psum[7]]
            for e in range(EG):
                ee = g * EG + e
                ps = P.ps()
                for kc in range(8):
                    P.mm(ps, lhsT=C.XT[:, kc, t * 128:(t + 1) * 128], rhs=w13[:, e, kc, :], start=(kc == 0), stop=(kc == 7))
                sa = P.tmp("msa", [256], F32, e % 2)
                P.a("activation", out=sa, in_=ps[:, 0:256], func=AF.Silu)
                hb = P.tmp("mhb", [256], BF16, e % 2)
                P.v("scalar_tensor_tensor", out=hb, in0=ps[:, 256:512], scalar=gates[:, t, ee:ee + 1], in1=sa,
                    op0=ALU.mult, op1=ALU.mult)
                pt = P.ps()
                ptb = pt.bitcast(BF16)
                P.tr(ptb[:, 0:128], hb[:, 0:128], C.identb, inc=False)
                P.tr(ptb[:, 128:256], hb[:, 128:256], C.identb)
                hT = P.tmp("mhT", [2, 128], BF16, e % 2)
                P.v("tensor_copy", out=hT, in_=ptb[:, 0:256].re("p (c t) -> p c t", c=2))
                for c in range(2):
                    for half in range(2):
                        first = (e == 0 and c == 0)
                        last = (e == EG - 1 and c == 1)
                        P.mm(psy[half], lhsT=hT[:, c, :], rhs=w2[:, e, c, half * 512:(half + 1) * 512], start=first, stop=last,
                             inc=(last and half == 1))
            ya = P.tmp("mya", [D], F32, sl)
            if g == 0:
                P.v("tensor_copy", out=ya[:, 0:512], in_=psy[0])
                P.a("copy", out=ya[:, 512:1024], in_=psy[1])
            else:
                yo = P.tmp("myo", [D], F32, sl)
                P.dma("sp", yo, yacc.t(t, rows(t)))
                P.v("tensor_tensor", out=ya[:, 0:512], in0=yo[:, 0:512], in1=psy[0], op=ALU.add)
                P.v("tensor_tensor", out=ya[:, 512:1024], in0=yo[:, 512:1024], in1=psy[1], op=ALU.add)
            P.dma("sp", yacc.t(t, rows(t)), ya)
    P.nrot = 8
    P.phase()
    g_bc = bc_load(P, "g3", W["ln3_g"][l], D)
    b_bc = bc_load(P, "b3", W["ln3_b"][l], D)
    for t in range(TL[0]):
        sl = t % 2
        xin = P.tmp("x3i", [D], F32, sl)
        yy = P.tmp("y3i", [D], F32, sl)
        P.dma("sp", xin, xres.t(t, rows(t)))
        P.dma("sp", yy, yacc.t(t, rows(t)))
        P.v("scalar_tensor_tensor", out=yy, in0=xin, scalar=DN_ALPHA, in1=yy, op0=ALU.mult, op1=ALU.add)
        xo = P.tmp("x3o", [D], F32, sl)
        layer_norm_tile(P, C, yy, g_bc, b_bc, xo, sl)
        P.dma("sp", xres.t(t, rows(t)), xo)
        to_xT(P, C, xo, t, sl)


def stage_ln_in(P, C, W, xres):
    P.phase()
    g_bc = P.alloc([D], F32, "g_bc")
    b_bc = P.alloc([D], F32, "b_bc")
    P.dma("sp", g_bc, ext(W["ln_in_g"].partition_broadcast(128)))
    P.dma("sp", b_bc, ext(W["ln_in_b"].partition_broadcast(128)))
    for t in range(TL[0]):
        sl = t % 2
        xin = P.tmp("xin", [D], F32, sl)
        xo = P.tmp("xo", [D], F32, sl)
        P.dma("sp", xin, ext(W["x"][t * 128:(t + 1) * 128, :]))
        layer_norm_tile(P, C, xin, g_bc, b_bc, xo, sl)
        P.dma("sp", xres.t(t, rows(t)), xo)
        to_xT(P, C, xo, t, sl)


def stage_win(P, C, W, l, hbuf):
    P.phase()
    win = P.alloc([8, INW], BF16, "win")
    load_cast(P, win, W["w_in"][l].rearrange("(kc p) n -> p kc n", p=128), maxcols=776)
    for t in range(TL[0]):
        sl = t % 2
        ho = P.tmp("ho", [INW], F32, sl)
        n0 = 0
        while n0 < INW:
            n1 = min(INW, n0 + 512)
            ps = P.ps()
            for kc in range(8):
                P.mm(ps[:, 0:n1 - n0], C.XT[:, kc, t * 128:(t + 1) * 128], win[:, kc, n0:n1],
                     start=(kc == 0), stop=(kc == 7))
            if (n0 // 512) % 2 == 0:
                P.v("tensor_copy", out=ho[:, n0:n1], in_=ps[:, 0:n1 - n0])
            else:
                P.a("copy", out=ho[:, n0:n1], in_=ps[:, 0:n1 - n0])
            n0 = n1
        P.dma("sp", hbuf.t(t, rows(t)), ho)


def dump(P, nc, name, dt, shape, keys, c0=0, c1=None):
    c1 = shape[1] if c1 is None else c1
    dd = nc.dram_tensor("dbg_" + name, [shape[0], c1 - c0], F32, kind="ExternalOutput").ap()
    nrow = shape[0]
    step = 512
    ob = Buf()
    for i, r0 in enumerate(range(0, nrow, 128)):
        bt = P.tmp("dump" + name, [c1 - c0], F32, i % 2)
        P.dma("sp", bt, dt.t(list(keys), (slice(r0, r0 + 128), slice(c0, c1))))
        P.dma("sp", T(dd[r0:r0 + 128], [ob]), bt)


def build(n_layers=DEPTH, dbg=(), stop_after=None):
    nc = bass.Bass("TRN2", target_bir_lowering=False)
    P = Prog(nc, 208000 // 4)
    C = Ctx()
    C.stop = stop_after
    W = {}
    for name, shp in kernel_shapes().items():
        W[name] = nc.dram_tensor(name, list(shp), F32, kind="ExternalInput").ap()
    out_d = nc.dram_tensor("out", [NT, D], F32, kind="ExternalOutput").ap()

    C.identf = P.alloc([128], F32, "identf")
    C.identb = P.alloc([128], BF16, "identb")
    C.eps_ln = P.alloc([1], F32, "eps_ln")
    C.XT = P.alloc([8, NT], BF16, "XT")
    P.persist = P.aoff
    C.W = W
    P.dma("sp", C.identf, ext(W["c_ident"]))
    P.v("tensor_copy", out=C.identb, in_=C.identf)
    P.g("memset", ap=C.eps_ln, constant=LN_EPS)

    xres = P.dram("xres", [NT, D])
    hbuf = P.dram("hbuf", [NT, INW])
    mixd = P.dram("mixd", [NT, D])
    sin = P.dram("sin", [NT, 2304])
    aux = P.dram("aux", [NT, 260])
    yscr = P.dram("yscr", [2 * NT, 256])
    catt = P.dram("catt", [NT, D])
    yacc = P.dram("yacc", [NT, D])

    def done(tag):
        return stop_after == tag

    stage_ln_in(P, C, W, xres)
    for l in range(n_layers):
        stage_win(P, C, W, l, hbuf)
        if done("win"):
            break
        if "hgrn" in str(stop_after):
            stage_hgrn(P, C, W, l, hbuf, mixd, sin, aux, yscr)
            break
        if "rwkv" in str(stop_after):
            stage_rwkv(P, C, W, l, hbuf, mixd, sin, aux, yscr)
            break
        stage_gqa(P, C, W, l, hbuf, mixd)
        if done("gqa") or done("gqaprep"):
            break
        stage_mla(P, C, W, l, hbuf, mixd)
        if done("mla") or done("mlaprep"):
            break
        stage_hgrn(P, C, W, l, hbuf, mixd, sin, aux, yscr)
        stage_rwkv(P, C, W, l, hbuf, mixd, sin, aux, yscr)
        stage_wout(P, C, W, l, mixd, xres)
        if done("x1"):
            break
        stage_cross(P, C, W, l, xres, catt)
        if done("x2"):
            break
        stage_moe(P, C, W, l, xres, yacc)
        if done("x3"):
            break

    P.phase()
    allt = list(range(NTILE))
    if "h" in dbg:
        dump(P, nc, "h", hbuf, [NT, INW], allt)
    if "qt" in dbg:
        QT, KT, VS = C.dbgT
        dq = nc.dram_tensor("dbg_qt", [64, 4 * NT], BF16, kind="ExternalOutput").ap()
        P.dma("sp", T(dq, [Buf()]), QT[0:64].re("p h t -> p (h t)"))
        dk = nc.dram_tensor("dbg_kt", [64, 2 * NT], BF16, kind="ExternalOutput").ap()
        P.dma("sp", T(dk, [Buf()]), KT[0:64].re("p h t -> p (h t)"))
    if "mix" in dbg:
        keys = [(t, m) for t in allt for m in dbg["mix"]]
        dump(P, nc, "mix", mixd, [NT, D], keys, dbg.get("c0", 0), dbg.get("c1", D))
    ob = Buf()
    for t in range(TL[0]):
        bt = P.tmp("outb", [D], F32, t % 2)
        P.dma("sp", bt, xres.t(t, rows(t)))
        P.dma("sp", T(out_d[t * 128:(t + 1) * 128, :], [ob]), bt)
    P.barrier()
    P.emit()
    return nc, P


def prep_inputs(inputs, cores):
    consts = host_consts()
    maps = []
    shapes = kernel_shapes()
    for c in cores:
        m = {}
        m["x"] = np.ascontiguousarray(np.asarray(inputs["x"])[c * NB:(c + 1) * NB].reshape(NT, D))
        m["mem"] = np.ascontiguousarray(np.asarray(inputs["mem"])[c * NB:(c + 1) * NB].reshape(NB * 256, D))
        for k, shp in shapes.items():
            if k in ("x", "mem"):
                continue
            if k in consts:
                m[k] = consts[k]
            else:
                m[k] = np.ascontiguousarray(np.asarray(inputs[k], dtype=np.float32).reshape(shp))
        maps.append(m)
    return maps


def run(inputs, cores, n_layers=DEPTH, dbg=(), stop_after=None):
    nc, P = build(n_layers, dbg, stop_after)
    maps = prep_inputs(inputs, cores)
    res = run_bass_kernel_spmd(nc, maps, core_ids=list(range(len(cores))))
    return res.results, P


def kernel(**inputs):
    results, _ = run(inputs, list(range(8)))
    out = np.concatenate([r["out"].reshape(NB, S, D) for r in results], axis=0)
    return out.astype(np.float32)
```

```python
import math
import os
CUT = int(os.environ.get('CUT', '99'))
import numpy as np
import concourse.bass as bass
import concourse.mybir as mybir
from concourse.bass_utils import run_bass_kernel_spmd

F32 = mybir.dt.float32
BF16 = mybir.dt.bfloat16
AF = mybir.ActivationFunctionType
ALU = mybir.AluOpType
AX = mybir.AxisListType

ENGS = ["pe", "act", "dve", "pool", "sp"]
WKEYS = {"out", "accum_out", "ap"}
EPOCH = 24000
NDSEM = 56
NDSEM_HW = 40


class Buf:
    __slots__ = ("name", "writes", "reads")

    def __init__(self, name=""):
        self.name = name
        self.writes = {}
        self.reads = {}


class T:
    __slots__ = ("ap", "bufs")

    def __init__(self, ap, bufs):
        self.ap = ap
        self.bufs = bufs

    def __getitem__(self, k):
        return T(self.ap[k], self.bufs)

    def re(self, s, **kw):
        return T(self.ap.rearrange(s, **kw), self.bufs)

    def bc(self, shape):
        return T(self.ap.broadcast_to(shape), self.bufs)

    def un(self, ax):
        return T(self.ap.unsqueeze(ax), self.bufs)

    def bitcast(self, dt):
        return T(self.ap.bitcast(dt), self.bufs)

    @property
    def shape(self):
        return self.ap.shape


def _key(ev):
    return (ev[0], ev[1])


class Prog:
    def __init__(self, nc, arena_words):
        self.nc = nc
        self.stream = {e: [] for e in ENGS}
        self.tick = {e: 0 for e in ENGS}
        self.esems = {e: [] for e in ENGS}
        self.seen = {e: {} for e in ENGS}
        self.dsems = [nc.alloc_semaphore(f"dsem{i}") for i in range(NDSEM)]
        self.dtot = [0] * NDSEM
        self.drr = 0
        self.drr_sw = 0
        self.pe_pending = []
        self.arena = nc.alloc_sbuf_tensor("arena", [128, arena_words], F32)
        self.arena_words = arena_words
        self.aoff = 0
        self.persist = 0
        self.psum = [T(nc.alloc_psum_tensor(f"psb{i}", [128, 512], F32)[:, :], [Buf(f"ps{i}")]) for i in range(8)]
        self.nbuf = 0
        self.ninstr = 0
        self.cache = {}
        self.pr = 0
        self.nrot = 8

    def alloc(self, free_shape, dtype=F32, name=None):
        n = int(np.prod(free_shape))
        words = (n + 1) // 2 if dtype == BF16 else n
        words = (words + 7) // 8 * 8
        assert self.aoff + words <= self.arena_words, f"arena overflow {self.aoff}+{words} ({name})"
        ap = self.arena[:, self.aoff:self.aoff + words]
        self.aoff += words
        if dtype == BF16:
            ap = ap.bitcast(BF16)[:, 0:n]
        else:
            ap = ap[:, 0:n]
        if len(free_shape) == 2:
            ap = ap.rearrange("p (a b) -> p a b", a=free_shape[0])
        elif len(free_shape) == 3:
            ap = ap.rearrange("p (a b c) -> p a b c", a=free_shape[0], b=free_shape[1])
        elif len(free_shape) == 4:
            ap = ap.rearrange("p (a b c d) -> p a b c d", a=free_shape[0], b=free_shape[1], c=free_shape[2])
        self.nbuf += 1
        return T(ap, [Buf(name or f"b{self.nbuf}")])

    def tmp(self, name, free_shape, dtype=F32, slot=0):
        k = (name, slot)
        if k not in self.cache:
            self.cache[k] = self.alloc(free_shape, dtype, f"{name}{slot}")
        return self.cache[k]

    def ps(self):
        p = self.psum[self.pr % self.nrot]
        self.pr += 1
        return p

    def dram(self, name, shape, dtype=F32):
        ap = self.nc.dram_tensor(name, list(shape), dtype, kind="Internal").ap()
        return DT(ap)

    def _esem(self, P, ep):
        while len(self.esems[P]) <= ep:
            self.esems[P].append(self.nc.alloc_semaphore(f"es_{P}_{len(self.esems[P])}"))
        return self.esems[P][ep]

    def _wait(self, X, ev):
        k = _key(ev)
        v = ev[2]
        if self.seen[X].get(k, 0) >= v:
            return
        self.seen[X][k] = v
        if ev[0] == "e":
            ep = (v - 1) // EPOCH
            sem = self._esem(ev[1], ep)
            val = v - ep * EPOCH
        else:
            sem = self.dsems[ev[1]]
            val = v
        self.stream[X].append(lambda e, sem=sem, val=val: e.wait_ge(sem, val))

    def _deps(self, X, wbufs, rbufs):
        for b in rbufs:
            for ev in b.writes.values():
                self._wait(X, ev)
        for b in wbufs:
            for ev in list(b.writes.values()) + list(b.reads.values()):
                if ev[0] == "e" and ev[1] == X and X == "pe":
                    continue
                self._wait(X, ev)

    def _record(self, ev, wbufs, rbufs):
        k = _key(ev)
        for b in wbufs:
            if b.reads:
                b.writes = {}
                b.reads = {}
            b.writes[k] = ev
        for b in rbufs:
            b.reads[k] = ev

    def op(self, X, call, wbufs, rbufs, inc=True):
        self._deps(X, wbufs, rbufs)
        self.ninstr += 1
        if not inc:
            assert X == "pe"
            self.pe_pending.append((wbufs, rbufs))
            self.stream[X].append(lambda e: call(e))
            return
        self.tick[X] += 1
        t = self.tick[X]
        ep = (t - 1) // EPOCH
        sem = self._esem(X, ep)
        self.stream[X].append(lambda e: call(e).then_inc(sem, 1))
        ev = ("e", X, t)
        if X == "pe" and self.pe_pending:
            for (w, r) in self.pe_pending:
                self._record(ev, w, r)
            self.pe_pending = []
        self._record(ev, wbufs, rbufs)

    def ins(self, X, fname, **kw):
        wb, rb, real = [], [], {}
        for k, v in kw.items():
            if isinstance(v, T):
                (wb if k in WKEYS else rb).extend(v.bufs)
                real[k] = v.ap
            else:
                real[k] = v
        eng = {"pe": "tensor", "act": "scalar", "dve": "vector", "pool": "gpsimd", "sp": "sync"}[X]
        self.op(X, lambda e: getattr(e, fname)(**real), wb, rb)

    def v(self, fname, **kw):
        self.ins("dve", fname, **kw)

    def a(self, fname, **kw):
        self.ins("act", fname, **kw)

    def g(self, fname, **kw):
        self.ins("pool", fname, **kw)

    def mm(self, out, lhsT, rhs, start=True, stop=True, inc=None, **kw):
        if inc is None:
            inc = stop
        o, l, r = out.ap, lhsT.ap, rhs.ap
        self.op("pe", lambda e: e.matmul(o, lhsT=l, rhs=r, start=start, stop=stop, **kw),
                list(out.bufs), list(lhsT.bufs) + list(rhs.bufs), inc=inc)

    def tr(self, out, in_, ident, inc=True):
        o, i, d = out.ap, in_.ap, ident.ap
        self.op("pe", lambda e: e.transpose(out=o, in_=i, identity=d),
                list(out.bufs), list(in_.bufs) + list(ident.bufs), inc=inc)

    def dma(self, Q, out, in_, **kw):
        wb, rb = list(out.bufs), list(in_.bufs)
        self._deps(Q, wb, rb)
        if Q == "pool":
            s = NDSEM_HW + self.drr_sw
            self.drr_sw = (self.drr_sw + 1) % (NDSEM - NDSEM_HW)
        else:
            s = self.drr
            self.drr = (self.drr + 1) % NDSEM_HW
        if self.dtot[s] > 0:
            self._wait(Q, ("d", s, self.dtot[s]))
        self.dtot[s] += 16
        sem = self.dsems[s]
        o, i = out.ap, in_.ap
        self.ninstr += 1
        self.stream[Q].append(lambda e: e.dma_start(out=o, in_=i, **kw).then_inc(sem, 16))
        ev = ("d", s, self.dtot[s])
        self._record(ev, wb, rb)

    def barrier(self):
        for X in ENGS:
            for Pn in ENGS:
                if Pn != X and self.tick[Pn] > 0:
                    self._wait(X, ("e", Pn, self.tick[Pn]))
            for s in range(NDSEM):
                if self.dtot[s] > 0:
                    self._wait(X, ("d", s, self.dtot[s]))

    def phase(self):
        self.barrier()
        self.aoff = self.persist
        self.cache = {}

    def emit(self):
        nc = self.nc
        with nc.Block() as block:
            @block.tensor
            def _(e):
                for f in self.stream["pe"]:
                    f(e)

            @block.scalar
            def _(e):
                for f in self.stream["act"]:
                    f(e)

            @block.vector
            def _(e):
                for f in self.stream["dve"]:
                    f(e)

            @block.gpsimd
            def _(e):
                for f in self.stream["pool"]:
                    f(e)

            @block.sync
            def _(e):
                for f in self.stream["sp"]:
                    f(e)


class DT:
    def __init__(self, ap):
        self.ap = ap
        self.regs = {}

    def t(self, keys, idx=None):
        if not isinstance(keys, (list, tuple)) or (isinstance(keys, tuple)):
            keys = [keys]
        bufs = []
        for k in keys:
            if k not in self.regs:
                self.regs[k] = Buf(str(k))
            bufs.append(self.regs[k])
        ap = self.ap if idx is None else self.ap[idx]
        return T(ap, bufs)


def ext(ap):
    return T(ap, [])


D = 1024
S = 2048
NB = 2
NT = NB * S
NTILE = NT // 128
TPS = S // 128
DEPTH = 2
INW = 3104
ATT_IN, RWKV_IN, HGRN_IN, MLA_IN = 512, 960, 1280, 352
OFF_ATT, OFF_RWKV, OFF_HGRN, OFF_MLA = 0, 512, 1472, 2752
DN_ALPHA = (2 * DEPTH) ** 0.25
LN_EPS = 1e-5
RMS_EPS = 1e-6

WEIGHT_NAMES = [
    'ln_in_g', 'ln_in_b', 'w_in', 'attn_q_norm', 'attn_k_norm', 'rwkv_mu_prev', 'rwkv_mu_next',
    'rwkv_w0', 'rwkv_w2', 'rwkv_a0', 'rwkv_a2', 'rwkv_g2', 'rwkv_k_k', 'rwkv_k_a', 'rwkv_r_k',
    'rwkv_gn_g', 'rwkv_gn_b', 'hgrn_lb_logits', 'hgrn_norm_g', 'mla_q_norm', 'mla_kv_norm',
    'mla_w_uq', 'mla_w_ukv', 'w_out', 'ln1_g', 'ln1_b', 'mem_wq', 'mem_wkv', 'mem_wo', 'ln2_g',
    'ln2_b', 'moe_wg_group', 'moe_bg_group', 'moe_wg_expert', 'moe_bg_expert', 'moe_w1', 'moe_w3',
    'moe_w2', 'ln3_g', 'ln3_b']


def host_consts():
    c = {}
    c["c_ident"] = np.eye(128, dtype=np.float32)
    return c


ROPE_THETA = 10000.0
TL = [NTILE]


def _rope_tab(half_dim, nrep):
    t = np.arange(S)
    row = (t // 64).astype(np.float64)
    col = (t % 64).astype(np.float64)
    nf = half_dim // 2
    inv = ROPE_THETA ** (-np.arange(0, half_dim, 2, dtype=np.float64) / half_dim)
    ar = row[:, None] * inv[None, :]
    ac = col[:, None] * inv[None, :]
    Ct = np.concatenate([np.cos(ar), np.cos(ar), np.cos(ac), np.cos(ac)], axis=1)
    St = np.concatenate([-np.sin(ar), np.sin(ar), -np.sin(ac), np.sin(ac)], axis=1)
    return (np.tile(Ct, (1, nrep)).astype(np.float32), np.tile(St, (1, nrep)).astype(np.float32))


def host_consts():
    c = {}
    c["c_ident"] = np.eye(128, dtype=np.float32)
    c["c_ropeC6"], c["c_ropeS6"] = _rope_tab(32, 6)
    c["c_ropeC5"], c["c_ropeS5"] = _rope_tab(16, 5)
    sc, bd = _scan_consts()
    c["c_scan"] = np.ascontiguousarray(sc.transpose(1, 0, 2))
    c["c_bd"] = bd
    rm = np.zeros((128, 2), np.float32)
    rm[:64, 0] = 1.0
    rm[64:, 1] = 1.0
    c["c_rowmask"] = np.ascontiguousarray(np.repeat(rm[:, :, None], 256, axis=2))
    return c


def kernel_shapes():
    L = DEPTH
    s = {
        "x": (NT, D), "mem": (NB * 256, D),
        "c_ident": (128, 128), "c_ropeC6": (S, 384), "c_ropeS6": (S, 384),
        "c_ropeC5": (S, 160), "c_ropeS5": (S, 160), "c_scan": (128, 2, 640), "c_bd": (128, 128), "c_rowmask": (128, 2, 256),
        "ln_in_g": (D,), "ln_in_b": (D,), "w_in": (L, D, INW),
        "attn_q_norm": (L, 64), "attn_k_norm": (L, 64),
        "rwkv_mu_prev": (L, 960), "rwkv_mu_next": (L, 960), "rwkv_w0": (L, 2, 256), "rwkv_w2": (L, 2, 32, 256),
        "rwkv_a0": (L, 2, 256), "rwkv_a2": (L, 2, 32, 256), "rwkv_g2": (L, 64, 256), "rwkv_k_k": (L, 256),
        "rwkv_k_a": (L, 256), "rwkv_r_k": (L, 4, 64), "rwkv_gn_g": (L, 256), "rwkv_gn_b": (L, 256),
        "hgrn_lb_logits": (L, 256), "hgrn_norm_g": (L, 256),
        "mla_q_norm": (L, 192), "mla_kv_norm": (L, 128), "mla_w_uq": (L, 192, 384), "mla_w_ukv": (L, 128, 512),
        "w_out": (L, D, D), "ln1_g": (L, D), "ln1_b": (L, D),
        "mem_wq": (L, D, D), "mem_wkv": (L, D, 2 * D), "mem_wo": (L, D, D), "ln2_g": (L, D), "ln2_b": (L, D),
        "moe_wg_group": (L, D, 4), "moe_bg_group": (L, 4), "moe_wg_expert": (L, D, 32), "moe_bg_expert": (L, 32),
        "moe_w1": (L, 32, D, 256), "moe_w3": (L, 32, D, 256), "moe_w2": (L, 32, 256, D),
        "ln3_g": (L, D), "ln3_b": (L, D),
    }
    return s


class Ctx:
    pass


def load_cast(P, dst, src_ap, q="pool", maxcols=1024):
    n = src_ap.shape[-1]
    c0 = 0
    while c0 < n:
        c1 = min(n, c0 + maxcols)
        P.dma(q, dst[..., c0:c1], ext(src_ap[..., c0:c1]))
        c0 = c1


def rows(t):
    return (slice(t * 128, (t + 1) * 128),)


def layer_norm_tile(P, C, xin, g_bc, b_bc, out, sl):
    st = P.tmp("lnst", [4], F32, sl)
    xc = P.tmp("lnxc", [D], F32, sl)
    junk = P.tmp("lnjunk", [D], F32, sl)
    P.a("activation", out=junk, in_=xin, func=AF.Identity, scale=1.0 / D, accum_out=st[:, 0:1])
    P.v("tensor_scalar", out=xc, in0=xin, scalar1=st[:, 0:1], scalar2=None, op0=ALU.subtract)
    P.a("activation", out=junk, in_=xc, func=AF.Square, scale=1.0 / 32.0, accum_out=st[:, 1:2])
    P.a("activation", out=st[:, 2:3], in_=st[:, 1:2], func=AF.Sqrt, bias=C.eps_ln[:, 0:1], scale=1.0)
    P.v("reciprocal", out=st[:, 3:4], in_=st[:, 2:3])
    P.v("scalar_tensor_tensor", out=xc, in0=xc, scalar=st[:, 3:4], in1=g_bc, op0=ALU.mult, op1=ALU.mult)
    P.g("tensor_tensor", out=out, in0=xc, in1=b_bc, op=ALU.add)


def transpose_to(P, C, xb, ncols, dst_fn):
    kc = 0
    nk = (ncols + 127) // 128
    while kc < nk:
        n = min(4, nk - kc)
        ps = P.ps()
        psb = ps.bitcast(BF16)
        for j in range(n):
            c0 = (kc + j) * 128
            c1 = min(ncols, c0 + 128)
            P.tr(psb[0:c1 - c0, j * 128:(j + 1) * 128], xb[:, c0:c1], C.identb, inc=(j == n - 1))
        for j in range(n):
            c0 = (kc + j) * 128
            c1 = min(ncols, c0 + 128)
            dst = dst_fn(kc + j, c1 - c0)
            if j % 2 == 0:
                P.v("tensor_copy", out=dst, in_=psb[0:c1 - c0, j * 128:(j + 1) * 128])
            else:
                P.a("copy", out=dst, in_=psb[0:c1 - c0, j * 128:(j + 1) * 128])
        kc += n


def to_xT(P, C, xn, tile, sl):
    xb = P.tmp("xb", [D], BF16, sl)
    P.a("copy", out=xb, in_=xn)
    transpose_to(P, C, xb, D, lambda kc, r: C.XT[:, kc, tile * 128:(tile + 1) * 128])


def attention(P, C, nkt, chunks_fn, v_fn, dv, scale, out_fn, heads, tag):
    gpb = max(1, 512 // (dv + 1))
    it = 0
    for b in range(NB):
        for h in range(heads):
            ch = chunks_fn(b, h)
            for qb in range(4):
                sl = it % 2
                it += 1
                PT = P.tmp("PT" + tag, [nkt, 512], BF16, sl)
                for kt in range(nkt):
                    ps = P.ps()
                    for ci, (K_, Q_) in enumerate(ch):
                        P.mm(ps, lhsT=K_[:, kt * 128:(kt + 1) * 128], rhs=Q_[:, qb * 512:(qb + 1) * 512],
                             start=(ci == 0), stop=(ci == len(ch) - 1))
                    P.a("activation", out=PT[:, kt, :], in_=ps, func=AF.Exp, scale=scale)
                o_sb = P.tmp("osb" + tag, [4, dv], F32, sl)
                rd = P.tmp("rd" + tag, [4], F32, sl)
                qi = 0
                while qi < 4:
                    pso = P.ps()
                    ng = min(gpb, 4 - qi)
                    for g in range(ng):
                        for kt in range(nkt):
                            P.mm(pso[:, g * (dv + 1):(g + 1) * (dv + 1)],
                                 lhsT=PT[:, kt, (qi + g) * 128:(qi + g + 1) * 128], rhs=v_fn(b, h, kt),
                                 start=(kt == 0), stop=(kt == nkt - 1), inc=(kt == nkt - 1 and g == ng - 1))
                    pv = pso[:, 0:ng * (dv + 1)].re("p (g d) -> p g d", g=ng)
                    P.v("reciprocal", out=rd[:, qi:qi + ng], in_=pv[:, :, dv])
                    P.v("tensor_tensor", out=o_sb[:, qi:qi + ng, :], in0=pv[:, :, 0:dv],
                        in1=rd[:, qi:qi + ng].un(2).bc([128, ng, dv]), op=ALU.mult)
                    qi += ng
                out_fn(b, h, qb, o_sb)


def rope_apply(P, xin, rc, rs, out, nblk, half, names, sl, outre=None):
    n = nblk * 2 * half
    t1 = P.tmp(names + "t1", [n], F32, sl)
    t2 = P.tmp(names + "t2", [n], F32, sl)
    P.v("tensor_tensor", out=t1, in0=xin, in1=rc, op=ALU.mult)
    xs = xin.re("p (b x d) -> p b x d", x=2, d=half)[:, :, ::-1, :]
    P.g("tensor_tensor", out=t2.re("p (b x d) -> p b x d", x=2, d=half), in0=xs,
        in1=rs.re("p (b x d) -> p b x d", x=2, d=half), op=ALU.mult)
    if outre is not None:
        P.v("tensor_tensor", out=out, in0=t1.re(outre[0], **outre[1]), in1=t2.re(outre[0], **outre[1]), op=ALU.add)
    else:
        P.v("tensor_tensor", out=out, in0=t1, in1=t2, op=ALU.add)


def stage_gqa(P, C, W, l, hbuf, mixd):
    P.phase()
    QT = P.alloc([4, NT], BF16, "QT")
    KT = P.alloc([2, NT], BF16, "KT")
    VS = P.alloc([NTILE, 2, 65], BF16, "VS")
    gain6 = P.alloc([6, 64], F32, "gain6")
    eps = P.alloc([1], F32, "epsr")
    P.g("memset", ap=eps, constant=RMS_EPS)
    P.g("memset", ap=VS, constant=1.0)
    for j in range(6):
        src = W["attn_q_norm"] if j < 4 else W["attn_k_norm"]
        P.dma("sp", gain6[:, j, :], ext(src[l].partition_broadcast(128)))
    for sl in range(2):
        P.g("memset", ap=P.tmp("qr", [6, 128], BF16, sl), constant=0.0)
    for t in range(TL[0]):
        sl = t % 2
        ts = t % TPS
        z = P.tmp("z", [512], F32, sl)
        rc = P.tmp("rc", [384], F32, sl)
        rs = P.tmp("rs", [384], F32, sl)
        P.dma("sp", z, hbuf.t(t, (slice(t * 128, (t + 1) * 128), slice(OFF_ATT, OFF_ATT + 512))))
        P.dma("sp", rc, ext(W["c_ropeC6"][ts * 128:(ts + 1) * 128, :]))
        P.dma("sp", rs, ext(W["c_ropeS6"][ts * 128:(ts + 1) * 128, :]))
        sq = P.tmp("sq", [384], F32, sl)
        ss = P.tmp("ss", [6], F32, sl)
        ss2 = P.tmp("ss2", [6], F32, sl)
        rstd = P.tmp("rstd", [6], F32, sl)
        P.a("activation", out=sq, in_=z[:, 0:384], func=AF.Square)
        P.v("tensor_reduce", out=ss, in_=sq.re("p (h d) -> p h d", d=64), axis=AX.X, op=ALU.add)
        P.a("activation", out=ss2, in_=ss, func=AF.Sqrt, scale=1.0 / 64.0, bias=eps[:, 0:1])
        P.v("reciprocal", out=rstd, in_=ss2)
        qn = P.tmp("qn", [384], F32, sl)
        P.v("tensor_tensor", out=qn.re("p (h d) -> p h d", d=64), in0=z[:, 0:384].re("p (h d) -> p h d", d=64),
            in1=rstd.un(2).bc([128, 6, 64]), op=ALU.mult)
        P.g("tensor_tensor", out=qn, in0=qn, in1=gain6.re("p h d -> p (h d)"), op=ALU.mult)
        qr = P.tmp("qr", [6, 128], BF16, sl)
        rope_apply(P, qn, rc, rs, qr[:, :, 0:64], 12, 16, "g", sl, outre=("p (h d) -> p h d", dict(d=64)))
        ps = P.ps()
        psb = ps.bitcast(BF16)
        for j in range(6):
            P.tr(psb[:, j * 128:(j + 1) * 128], qr[:, j, :], C.identb, inc=(j == 5))
        if CUT >= 8:
            P.v("tensor_copy", out=QT[:, :, t * 128:(t + 1) * 128],
                in_=psb[:, 0:512].re("p (h t) -> p h t", h=4))
        if CUT >= 9:
            P.v("tensor_copy", out=KT[:, :, t * 128:(t + 1) * 128],
                in_=psb[:, 512:768].re("p (h t) -> p h t", h=2))
        if CUT >= 10:
            P.v("tensor_copy", out=VS[:, t, :, 0:64], in_=z[:, 384:512].re("p (h d) -> p h d", d=64))

    def chunks(b, h):
        return [(KT[:, h // 2, b * S:(b + 1) * S], QT[:, h, b * S:(b + 1) * S])]

    def vfn(b, h, kt):
        return VS[:, b * TPS + kt, h // 2, :]

    def outfn(b, h, qb, o_sb):
        r0 = b * S + qb * 512
        keys = [(r0 // 128 + i, "att") for i in range(4)]
        dst = mixd.t(keys, (slice(r0, r0 + 512), slice(h * 64, (h + 1) * 64))).re("(q p) d -> p q d", p=128)
        P.dma("sp", dst, o_sb)

    if getattr(C, "stop", None) == "gqaprep":
        C.dbgT = (QT, KT, VS)
        return
    attention(P, C, TPS, chunks, vfn, 64, 1.0 / 8.0, outfn, 4, "g")


def stage_mla(P, C, W, l, hbuf, mixd):
    P.phase()
    QT = P.alloc([4, NT], BF16, "mQT")
    KT = P.alloc([4, NT], BF16, "mKT")
    VS = P.alloc([NTILE, 4, 65], BF16, "mVS")
    wuq = P.alloc([2, 384], BF16, "wuq")
    wukv = P.alloc([512], BF16, "wukv")
    gq = P.alloc([192], F32, "gq")
    gkv = P.alloc([128], F32, "gkv")
    eps = P.alloc([1], F32, "epsm")
    P.g("memset", ap=eps, constant=RMS_EPS)
    P.g("memset", ap=VS, constant=1.0)
    P.g("memset", ap=wuq, constant=0.0)
    P.dma("pool", wuq[:, 0, :], ext(W["mla_w_uq"][l, 0:128, :]))
    P.dma("pool", wuq[0:64, 1, :], ext(W["mla_w_uq"][l, 128:192, :]))
    P.dma("pool", wukv, ext(W["mla_w_ukv"][l]))
    P.dma("sp", gq, ext(W["mla_q_norm"][l].partition_broadcast(128)))
    P.dma("sp", gkv, ext(W["mla_kv_norm"][l].partition_broadcast(128)))
    for sl in range(2):
        P.g("memset", ap=P.tmp("cz", [3, 128], BF16, sl), constant=0.0)
        P.g("memset", ap=P.tmp("qz", [8, 128], BF16, sl), constant=0.0)
    for t in range(TL[0]):
        sl = t % 2
        ts = t % TPS
        z = P.tmp("mz", [352], F32, sl)
        rc = P.tmp("mrc", [160], F32, sl)
        rs = P.tmp("mrs", [160], F32, sl)
        P.dma("sp", z, hbuf.t(t, (slice(t * 128, (t + 1) * 128), slice(OFF_MLA, OFF_MLA + 352))))
        P.dma("sp", rc, ext(W["c_ropeC5"][ts * 128:(ts + 1) * 128, :]))
        P.dma("sp", rs, ext(W["c_ropeS5"][ts * 128:(ts + 1) * 128, :]))
        junk = P.tmp("mjunk", [192], F32, sl)
        st = P.tmp("mst", [6], F32, sl)
        P.a("activation", out=junk, in_=z[:, 0:192], func=AF.Square, accum_out=st[:, 0:1])
        P.a("activation", out=junk[:, 0:128], in_=z[:, 192:320], func=AF.Square, accum_out=st[:, 1:2])
        P.a("activation", out=st[:, 2:3], in_=st[:, 0:1], func=AF.Sqrt, scale=1.0 / 192.0, bias=eps[:, 0:1])
        P.a("activation", out=st[:, 3:4], in_=st[:, 1:2], func=AF.Sqrt, scale=1.0 / 128.0, bias=eps[:, 0:1])
        P.v("reciprocal", out=st[:, 4:6], in_=st[:, 2:4])
        cz = P.tmp("cz", [3, 128], BF16, sl)
        czf = cz.re("p a b -> p (a b)")
        P.v("scalar_tensor_tensor", out=czf[:, 0:192], in0=z[:, 0:192], scalar=st[:, 4:5], in1=gq,
            op0=ALU.mult, op1=ALU.mult)
        P.v("scalar_tensor_tensor", out=czf[:, 256:384], in0=z[:, 192:320], scalar=st[:, 5:6], in1=gkv,
            op0=ALU.mult, op1=ALU.mult)
        ps = P.ps()
        psb = ps.bitcast(BF16)
        for j in range(3):
            P.tr(psb[:, j * 128:(j + 1) * 128], cz[:, j, :], C.identb, inc=(j == 2))
        cT = P.tmp("cT", [3, 128], BF16, sl)
        P.v("tensor_copy", out=cT, in_=psb[:, 0:384].re("p (a b) -> p a b", a=3))
        psq = P.ps()
        P.mm(psq[:, 0:384], lhsT=cT[:, 0, :], rhs=wuq[:, 0, :], start=True, stop=False)
        P.mm(psq[:, 0:384], lhsT=cT[:, 1, :], rhs=wuq[:, 1, :], start=False, stop=True)
        pskv = P.ps()
        P.mm(pskv, lhsT=cT[:, 2, :], rhs=wukv, start=True, stop=True)
        q3 = psq[:, 0:384].re("p (h d) -> p h d", h=4)
        kv3 = pskv.re("p (h d) -> p h d", h=4)
        rin = P.tmp("rin", [5, 32], F32, sl)
        rout = P.tmp("rout", [5, 32], BF16, sl)
        P.v("tensor_copy", out=rin[:, 0:4, :], in_=q3[:, :, 64:96])
        P.g("tensor_copy", out=rin[:, 4, :], in_=z[:, 320:352])
        rope_apply(P, rin.re("p a b -> p (a b)"), rc, rs, rout.re("p a b -> p (a b)"), 10, 8, "m", sl)
        qz = P.tmp("qz", [8, 128], BF16, sl)
        P.v("tensor_copy", out=qz[:, 0:4, 0:64], in_=q3[:, :, 0:64])
        P.g("tensor_copy", out=qz[:, 0:4, 64:96], in_=rout[:, 0:4, :])
        P.v("tensor_copy", out=qz[:, 4:8, 0:64], in_=kv3[:, :, 0:64])
        for h in range(4):
            P.g("tensor_copy", out=qz[:, 4 + h, 64:96], in_=rout[:, 4, :])
        P.v("tensor_copy", out=VS[:, t, :, 0:64], in_=kv3[:, :, 64:128])
        for half in range(2):
            ps2 = P.ps()
            psb2 = ps2.bitcast(BF16)
            for j in range(4):
                P.tr(psb2[:, j * 128:(j + 1) * 128], qz[:, half * 4 + j, :], C.identb, inc=(j == 3))
            dst = QT if half == 0 else KT
            P.v("tensor_copy", out=dst[:, :, t * 128:(t + 1) * 128],
                in_=psb2[:, 0:512].re("p (h t) -> p h t", h=4))

    def chunks(b, h):
        return [(KT[:, h, b * S:(b + 1) * S], QT[:, h, b * S:(b + 1) * S])]

    def vfn(b, h, kt):
        return VS[:, b * TPS + kt, h, :]

    def outfn(b, h, qb, o_sb):
        r0 = b * S + qb * 512
        keys = [(r0 // 128 + i, "mla") for i in range(4)]
        dst = mixd.t(keys, (slice(r0, r0 + 512), slice(768 + h * 64, 768 + (h + 1) * 64))).re("(q p) d -> p q d", p=128)
        P.dma("sp", dst, o_sb)

    if C.stop == "mlaprep":
        return
    attention(P, C, TPS, chunks, vfn, 64, 96.0 ** -0.5, outfn, 4, "m")


def _scan_consts():
    t = np.arange(128)
    ch = t // 64
    same = (ch[:, None] == ch[None, :])
    out = np.zeros((2, 128, 640), np.float32)
    for d in range(2):
        if d == 0:
            tri = same & (t[:, None] <= t[None, :])
            mid = ch * 64 + 31
            strictT = same & (t[:, None] < t[None, :])
            inclT = same & (t[:, None] <= t[None, :])
        else:
            tri = same & (t[:, None] >= t[None, :])
            mid = ch * 64 + 32
            strictT = same & (t[:, None] > t[None, :])
            inclT = same & (t[:, None] >= t[None, :])
        tri = tri.astype(np.float32)
        tric = tri - tri[:, mid]
        out[d, :, 0:128] = tri
        out[d, :, 128:256] = tric
        out[d, :, 256:384] = strictT
        out[d, :, 384:512] = inclT
        out[d, :, 512:640] = strictT.T
    return out, same.astype(np.float32)


def scan(P, C, sin, cols, yscr, delta, tag):
    W = C.W
    C.scanc = P.alloc([2, 640], F32, "scanc")
    C.bdmask = P.alloc([128], F32, "bdmask")
    C.rowmaskF2 = P.alloc([2, 256], F32, "rowmask")
    C.rowmaskF = C.rowmaskF2[:, :, 0:128]
    P.dma("sp", C.scanc, ext(W["c_scan"]))
    P.dma("sp", C.bdmask, ext(W["c_bd"]))
    P.dma("sp", C.rowmaskF2, ext(W["c_rowmask"]))
    for d in range(2):
        alive = [scan_chain(P, C, sin, cols, yscr, delta, tag + str(b), b, d) for b in range(NB)]
        while alive:
            for g in list(alive):
                try:
                    next(g)
                except StopIteration:
                    alive.remove(g)


def scan_chain(P, C, sin, cols, yscr, delta, tag, b, d):
    cst = C.scanc[:, d, :]
    tri, tric = cst[:, 0:128], cst[:, 128:256]
    mG, mL = cst[:, 256:512], cst[:, 512:640]
    Pbd = P.tmp("Pbd" + tag, [2, 128], F32, 0)
    P.g("memset", ap=Pbd, constant=0.0)
    P.g("memset", ap=P.tmp("sQm" + tag, [3, 4, 128], F32, 0), constant=0.0)
    for i in range(TPS):
        ti = i if d == 0 else TPS - 1 - i
        t = b * TPS + ti
        sl = 0
        xs = i % 2
        cd = cols[d]
        names = ["lw", "r", "k", "v"] + (["a", "b"] if delta else [])
        X = {}
        for nm in names:
            X[nm] = P.tmp("sx" + nm + tag, [256], F32, xs)
            P.dma("sp", X[nm], sin.t(t, (slice(t * 128, (t + 1) * 128), slice(cd[nm], cd[nm] + 256))))
        psG = P.ps()
        P.mm(psG[:, 0:256], lhsT=tri, rhs=X["lw"])
        psGc = P.ps()
        P.mm(psGc[:, 0:256], lhsT=tric, rhs=X["lw"])
        NQ = 8
        Q = P.tmp("sQ" + tag, [NQ, 256], F32, sl)
        eGn = P.tmp("seGn" + tag, [256], F32, sl)
        P.a("activation", out=Q[:, 6, :], in_=psG[:, 0:256], func=AF.Exp)
        P.a("activation", out=Q[:, 7, :], in_=psGc[:, 0:256], func=AF.Exp)
        P.a("activation", out=eGn, in_=psGc[:, 0:256], func=AF.Exp, scale=-1.0)
        P.v("tensor_tensor", out=Q[:, 1, :], in0=X["r"], in1=Q[:, 7, :], op=ALU.mult)
        P.g("tensor_tensor", out=Q[:, 5, :], in0=X["r"], in1=Q[:, 6, :], op=ALU.mult)
        P.v("tensor_tensor", out=Q[:, 3, :], in0=X["k"], in1=eGn, op=ALU.mult)
        used = [1, 3, 5, 6, 7]
        if delta:
            gx = P.tmp("sgx" + tag, [2, 256], F32, sl)
            P.v("tensor_tensor", out=gx[:, 0, :], in0=psGc[:, 0:256], in1=X["lw"], op=ALU.subtract)
            P.v("tensor_tensor", out=gx[:, 1, :], in0=psG[:, 0:256], in1=X["lw"], op=ALU.subtract)
            P.a("activation", out=gx[:, 0, :], in_=gx[:, 0, :], func=AF.Exp)
            P.a("activation", out=gx[:, 1, :], in_=gx[:, 1, :], func=AF.Exp)
            P.v("scalar_tensor_tensor", out=Q[:, 0, :], in0=X["a"], scalar=-1.0, in1=gx[:, 0, :],
                op0=ALU.mult, op1=ALU.mult)
            P.v("scalar_tensor_tensor", out=Q[:, 4, :], in0=X["a"], scalar=-1.0, in1=gx[:, 1, :],
                op0=ALU.mult, op1=ALU.mult)
            P.g("tensor_tensor", out=Q[:, 2, :], in0=X["b"], in1=eGn, op=ALU.mult)
            used = [0, 1, 2, 3, 4, 5, 6, 7]
        yield
        FT = P.tmp("sFT" + tag, [NQ, 2, 128], F32, sl)
        j = 0
        pend = []
        for q in used:
            for pr in range(2):
                if j % 4 == 0:
                    pst = P.ps()
                P.tr(pst[:, (j % 4) * 128:(j % 4 + 1) * 128], Q[:, q, pr * 128:(pr + 1) * 128], C.identf,
                     inc=(j % 4 == 3 or (q == used[-1] and pr == 1)))
                pend.append((q, pr, pst, j % 4))
                j += 1
        FTm = P.tmp("sFTm" + tag, [3, 2, 2, 128], F32, sl)
        for idx, (q, pr, pst, jj) in enumerate(pend):
            if idx % 2 == 0:
                P.v("tensor_copy", out=FT[:, q, pr, :], in_=pst[:, jj * 128:(jj + 1) * 128])
            else:
                P.a("copy", out=FT[:, q, pr, :], in_=pst[:, jj * 128:(jj + 1) * 128])
        yield
        Qm = P.tmp("sQm" + tag, [3, 4, 128], F32, sl)
        mq = [0, 1, 2] if delta else [1]
        for q in mq:
            for hh in range(2):
                dst = Qm[:, q, :, :].re("p (pr hh) c -> p pr hh c", hh=2)[:, :, hh, hh * 64:(hh + 1) * 64]
                src = Q[:, q, :].re("p (pr hh d) -> p pr hh d", pr=2, hh=2)[:, :, hh, :]
                if hh == 0:
                    P.v("tensor_copy", out=dst, in_=src)
                else:
                    P.g("tensor_copy", out=dst, in_=src)
        j = 0
        pend2 = []
        for q in mq:
            for ph in range(4):
                if j % 4 == 0:
                    pst = P.ps()
                P.tr(pst[:, (j % 4) * 128:(j % 4 + 1) * 128], Qm[:, q, ph, :], C.identf, inc=(j % 4 == 3))
                pend2.append((q, ph, pst, j % 4))
                j += 1
        for idx, (q, ph, pst, jj) in enumerate(pend2):
            if idx % 2 == 0:
                P.v("tensor_copy", out=FTm[:, q, ph // 2, ph % 2, :], in_=pst[:, jj * 128:(jj + 1) * 128])
            else:
                P.a("copy", out=FTm[:, q, ph // 2, ph % 2, :], in_=pst[:, jj * 128:(jj + 1) * 128])
        Vm = P.tmp("sVm" + tag, [2, 256], F32, sl)
        for cc in range(2 if os.environ.get("VAR", "") != "b" else 0):
            P.v("tensor_tensor", out=Vm[:, cc, :], in0=X["v"], in1=C.rowmaskF[:, cc, :].un(1).bc([128, 2, 128]).re("p a b -> p (a b)") if False else C.rowmaskF2[:, cc, :], op=ALU.mult)

        def fth(q, h):
            return FT[64 * (h % 2):64 * (h % 2) + 64, q, h // 2, :]

        yield
        MrkT = P.tmp("sMrk" + tag, [4, 128], F32, sl)
        if delta:
            GA = P.tmp("sGA" + tag, [4, 256], F32, 0)
            GB = P.tmp("sGB" + tag, [4, 256], F32, 0)
            Lab = P.tmp("sLab" + tag, [4, 128], F32, 0)
            for (src, dstT) in ((2, GA), (3, GB)):
                for hp in range(2):
                    ps = P.ps()
                    for hh in range(2):
                        h = hp * 2 + hh
                        rhs = FTm[:, 0:2, hp, hh, :]
                        P.mm(ps[:, hh * 256:(hh + 1) * 256], lhsT=FT[:, src, hp, :], rhs=rhs,
                             start=True, stop=True, inc=(hh == 1))
                    P.v("tensor_tensor", out=dstT[:, hp * 2:hp * 2 + 2, :],
                        in0=ps.re("p (h c) -> p h c", h=2), in1=mG.un(1).bc([128, 2, 256]), op=ALU.mult)
                    yield
            ps = P.ps()
            for h in range(4):
                P.mm(ps[:, h * 128:(h + 1) * 128], lhsT=FT[:, 0, h // 2, :], rhs=FTm[:, 2, h // 2, h % 2, :],
                     start=True, stop=True, inc=(h == 3))
            P.v("tensor_tensor", out=Lab, in0=ps.re("p (h c) -> p h c", h=4),
                in1=mL.un(1).bc([128, 4, 128]), op=ALU.mult)
            yield
            A_l = [Lab]
            AT_l = [GA[:, :, 0:128]]
            for lv in range(1, 6):
                An = P.tmp(f"sA{lv}" + tag, [4, 128], F32, 0)
                ATn = P.tmp(f"sAT{lv}" + tag, [4, 128], F32, 0)
                ps1 = P.ps()
                ps2 = P.ps()
                for h in range(4):
                    P.mm(ps1[:, h * 128:(h + 1) * 128], lhsT=A_l[-1][:, h, :], rhs=AT_l[-1][:, h, :],
                         start=True, stop=True, inc=(h == 3))
                for h in range(4):
                    P.mm(ps2[:, h * 128:(h + 1) * 128], lhsT=AT_l[-1][:, h, :], rhs=A_l[-1][:, h, :],
                         start=True, stop=True, inc=(h == 3))
                P.v("tensor_copy", out=ATn, in_=ps1.re("p (h c) -> p h c", h=4))
                P.a("copy", out=An.re("p h c -> p (h c)"), in_=ps2)
                A_l.append(An)
                AT_l.append(ATn)
                yield
            MrbT = GA[:, :, 128:256]
            LakT = GB[:, :, 0:128]
            MrkTv = GB[:, :, 128:256]
        else:
            ps = P.ps()
            for h in range(4):
                P.mm(ps[:, h * 128:(h + 1) * 128], lhsT=FT[:, 3, h // 2, :], rhs=FTm[:, 1, h // 2, h % 2, :],
                     start=True, stop=True, inc=(h == 3))
            P.v("tensor_tensor", out=MrkT, in0=ps.re("p (h c) -> p h c", h=4),
                in1=mG[:, 128:256].un(1).bc([128, 4, 128]), op=ALU.mult)
            MrkTv = MrkT
        yield
        Y = P.tmp("sY" + tag, [256], F32, sl)
        V = X["v"]
        for ci in ((0, 1) if d == 0 else (1, 0)):
            p0 = 64 * ci
            clast = p0 + 63 if d == 0 else p0
            if delta:
                U = P.tmp("sU" + tag, [256], F32, ci)
                psx = P.ps()
                for pr in range(2):
                    P.mm(psx[:, pr * 128:(pr + 1) * 128], lhsT=FT[:, 4, pr, :], rhs=Pbd[:, pr, :],
                         start=True, stop=False, inc=False)
                    for hh in range(2):
                        h = pr * 2 + hh
                        P.mm(psx[:, h * 64:(h + 1) * 64], lhsT=LakT[:, h, :], rhs=V[:, h * 64:(h + 1) * 64],
                             start=False, stop=(hh == 1), inc=(hh == 1 and pr == 1))
                P.v("tensor_copy", out=U, in_=psx[:, 0:256])
                yield
                for lv in range(6):
                    psu = P.ps()
                    for h in range(4):
                        P.mm(psu[:, h * 64:(h + 1) * 64], lhsT=AT_l[lv][:, h, :], rhs=U[:, h * 64:(h + 1) * 64],
                             start=True, stop=True, inc=(h == 3))
                    P.v("tensor_tensor", out=U, in0=U, in1=psu[:, 0:256], op=ALU.add)
                    yield
            psy = P.ps()
            for pr in range(2):
                P.mm(psy[:, pr * 128:(pr + 1) * 128], lhsT=FT[:, 5, pr, :], rhs=Pbd[:, pr, :],
                     start=True, stop=False, inc=False)
                for hh in range(2):
                    h = pr * 2 + hh
                    last = (hh == 1)
                    if delta:
                        P.mm(psy[:, h * 64:(h + 1) * 64], lhsT=MrbT[:, h, :], rhs=U[:, h * 64:(h + 1) * 64],
                             start=False, stop=False, inc=False)
                    P.mm(psy[:, h * 64:(h + 1) * 64], lhsT=MrkTv[:, h, :], rhs=V[:, h * 64:(h + 1) * 64],
                         start=False, stop=last, inc=(last and pr == 1))
            P.v("tensor_copy", out=Y[p0:p0 + 64, :], in_=psy[p0:p0 + 64, 0:256])
            yield
            if delta:
                Um = P.tmp("sUm" + tag, [256], F32, ci)
                P.v("tensor_tensor", out=Um, in0=U, in1=C.rowmaskF2[:, ci, :], op=ALU.mult)
            for pr in range(2):
                psp = P.ps()
                cs = slice(pr * 128, (pr + 1) * 128)
                if delta:
                    P.mm(psp[:, 0:128], lhsT=Q[:, 2, cs], rhs=Um[:, cs], start=True, stop=False, inc=False)
                P.mm(psp[:, 0:128], lhsT=Q[:, 3, cs], rhs=Vm[:, ci, cs], start=(not delta), stop=True)
                t1 = P.tmp("st1" + tag, [128], F32, pr)
                P.v("tensor_scalar", out=t1, in0=psp[:, 0:128], scalar1=FT[:, 7, pr, clast:clast + 1], scalar2=None,
                    op0=ALU.mult)
                P.g("tensor_tensor", out=t1, in0=t1, in1=C.bdmask, op=ALU.mult)
                P.v("scalar_tensor_tensor", out=Pbd[:, pr, :], in0=Pbd[:, pr, :],
                    scalar=FT[:, 6, pr, clast:clast + 1], in1=t1, op0=ALU.mult, op1=ALU.add)
                yield
        P.dma("sp", yscr.t((d, t), (slice(d * NT + t * 128, d * NT + (t + 1) * 128),)), Y)
        yield


def bc_load(P, name, src_ap, n):
    tl = P.alloc([n], F32, name)
    P.dma("sp", tl, ext(src_ap.partition_broadcast(128)))
    return tl


def stage_rwkv(P, C, W, l, hbuf, mixd, sin, aux, yscr):
    P.phase()
    mp = bc_load(P, "mp", W["rwkv_mu_prev"][l], 960)
    mn = bc_load(P, "mn", W["rwkv_mu_next"][l], 960)
    w0 = bc_load(P, "w0", W["rwkv_w0"][l].rearrange("a b -> (a b)"), 512)
    a0 = bc_load(P, "a0", W["rwkv_a0"][l].rearrange("a b -> (a b)"), 512)
    kk_ = bc_load(P, "k_k", W["rwkv_k_k"][l], 256)
    ka = bc_load(P, "k_a", W["rwkv_k_a"][l], 256)
    rk = bc_load(P, "r_k", W["rwkv_r_k"][l].rearrange("a b -> (a b)"), 256)
    omk = P.alloc([256], F32, "omk")
    P.v("tensor_scalar", out=omk, in0=ka, scalar1=-1.0, scalar2=1.0, op0=ALU.mult, op1=ALU.add)
    Wblk = P.alloc([1024], F32, "Wblk")
    G2 = P.alloc([256], F32, "G2")
    P.g("memset", ap=Wblk, constant=0.0)
    P.g("memset", ap=G2, constant=0.0)
    for dd in range(2):
        P.dma("sp", Wblk[32 * dd:32 * dd + 32, dd * 256:(dd + 1) * 256], ext(W["rwkv_w2"][l, dd]))
        P.dma("sp", Wblk[64 + 32 * dd:96 + 32 * dd, 512 + dd * 256:512 + (dd + 1) * 256], ext(W["rwkv_a2"][l, dd]))
    P.dma("sp", G2[0:64, :], ext(W["rwkv_g2"][l]))
    tiny = P.alloc([1], F32, "tiny")
    for sl in range(2):
        P.g("memset", ap=P.tmp("lo", [256], F32, sl), constant=0.0)
    o0 = OFF_RWKV
    for t in range(TL[0]):
        sl = t % 2
        ts = t % TPS
        r0 = t * 128
        zc = P.tmp("zc", [960], F32, sl)
        zp = P.tmp("zp", [960], F32, sl)
        zn = P.tmp("zn", [960], F32, sl)
        P.dma("sp", zc, hbuf.t(t, (slice(r0, r0 + 128), slice(o0, o0 + 960))))
        if ts == 0:
            P.g("memset", ap=zp, constant=0.0)
            P.dma("sp", zp[1:128, :], hbuf.t(t, (slice(r0, r0 + 127), slice(o0, o0 + 960))))
        else:
            P.dma("sp", zp, hbuf.t([t - 1, t], (slice(r0 - 1, r0 + 127), slice(o0, o0 + 960))))
        if ts == TPS - 1:
            P.g("memset", ap=zn, constant=0.0)
            P.dma("sp", zn[0:127, :], hbuf.t(t, (slice(r0 + 1, r0 + 128), slice(o0, o0 + 960))))
        else:
            P.dma("sp", zn, hbuf.t([t, t + 1], (slice(r0 + 1, r0 + 129), slice(o0, o0 + 960))))
        P.v("tensor_tensor", out=zp, in0=zp, in1=zc, op=ALU.subtract)
        P.g("tensor_tensor", out=zn, in0=zn, in1=zc, op=ALU.subtract)
        P.v("tensor_tensor", out=zp, in0=zp, in1=mp, op=ALU.mult)
        P.g("tensor_tensor", out=zn, in0=zn, in1=mn, op=ALU.mult)
        P.v("tensor_tensor", out=zc, in0=zc, in1=zp, op=ALU.add)
        P.v("tensor_tensor", out=zc, in0=zc, in1=zn, op=ALU.add)
        zf = zc
        O = P.tmp("rwO", [2304], F32, sl)
        AX_ = P.tmp("rwA", [260], F32, sl)
        lo = P.tmp("lo", [256], F32, sl)
        P.a("activation", out=lo[:, 0:64], in_=zf[:, 768:832], func=AF.Tanh)
        P.g("tensor_copy", out=lo[:, 64:128], in_=zf[:, 832:896])
        P.a("activation", out=lo[:, 128:192], in_=zf[:, 896:960], func=AF.Sigmoid)
        pst = P.ps()
        P.tr(pst[:, 0:128], lo[:, 0:128], C.identf, inc=False)
        P.tr(pst[:, 128:256], lo[:, 128:256], C.identf)
        loT = P.tmp("loT", [256], F32, sl)
        P.v("tensor_copy", out=loT, in_=pst[:, 0:256])
        psw = P.ps()
        P.mm(psw, lhsT=loT[:, 0:128], rhs=Wblk[:, 0:512])
        psa = P.ps()
        P.mm(psa, lhsT=loT[:, 0:128], rhs=Wblk[:, 512:1024])
        psg = P.ps()
        P.mm(psg[:, 0:256], lhsT=loT[:, 128:256], rhs=G2)
        wr = P.tmp("wr", [512], F32, sl)
        ar = P.tmp("ar", [512], F32, sl)
        P.v("tensor_tensor", out=wr, in0=psw, in1=w0, op=ALU.add)
        P.a("activation", out=wr, in_=wr, func=AF.Sigmoid)
        P.v("tensor_scalar", out=O[:, 0:512], in0=wr, scalar1=-math.exp(-0.5), scalar2=None, op0=ALU.mult)
        P.v("tensor_tensor", out=ar, in0=psa, in1=a0, op=ALU.add)
        P.a("activation", out=ar, in_=ar, func=AF.Sigmoid)
        P.a("copy", out=AX_[:, 0:256], in_=psg[:, 0:256])
        r_ = zf[:, 0:256]
        k_ = zf[:, 256:512]
        v_ = zf[:, 512:768]
        P.g("tensor_copy", out=O[:, 512:768], in_=r_)
        P.g("tensor_copy", out=O[:, 1280:1536], in_=v_)
        kkr = P.tmp("kkr", [256], F32, sl)
        sq = P.tmp("rsq", [256], F32, sl)
        st = P.tmp("rst", [16], F32, sl)
        P.v("tensor_tensor", out=kkr, in0=k_, in1=kk_, op=ALU.mult)
        P.a("activation", out=sq, in_=kkr, func=AF.Square)
        P.v("tensor_reduce", out=st[:, 0:4], in_=sq.re("p (h d) -> p h d", d=64), axis=AX.X, op=ALU.add)
        P.a("activation", out=st[:, 4:8], in_=st[:, 0:4], func=AF.Sqrt)
        P.v("tensor_scalar", out=st[:, 4:8], in0=st[:, 4:8], scalar1=1e-12, scalar2=None, op0=ALU.max)
        P.v("reciprocal", out=st[:, 8:12], in_=st[:, 4:8])
        P.v("tensor_tensor", out=O[:, 1536:1792].re("p (h d) -> p h d", d=64), in0=kkr.re("p (h d) -> p h d", d=64),
            in1=st[:, 8:12].un(2).bc([128, 4, 64]), op=ALU.mult)
        for dd in range(2):
            tmp = P.tmp("rtmp", [256], F32, dd)
            P.v("tensor_tensor", out=tmp, in0=ar[:, dd * 256:(dd + 1) * 256], in1=ka, op=ALU.mult)
            P.g("tensor_tensor", out=tmp, in0=tmp, in1=omk, op=ALU.add)
            P.v("tensor_tensor", out=O[:, 768 + dd * 256:1024 + dd * 256], in0=tmp, in1=k_, op=ALU.mult)
            P.g("tensor_tensor", out=O[:, 1792 + dd * 256:2048 + dd * 256], in0=ar[:, dd * 256:(dd + 1) * 256],
                in1=O[:, 1536:1792], op=ALU.mult)
        bt = P.tmp("rbt", [256], F32, sl)
        P.v("tensor_tensor", out=bt, in0=O[:, 768:1024], in1=O[:, 1024:1280], op=ALU.add)
        P.v("tensor_tensor", out=bt, in0=bt, in1=r_, op=ALU.mult)
        P.v("tensor_tensor", out=bt, in0=bt, in1=rk, op=ALU.mult)
        P.v("tensor_reduce", out=AX_[:, 256:260], in_=bt.re("p (h d) -> p h d", d=64), axis=AX.X, op=ALU.add)
        P.dma("sp", sin.t(t, (slice(r0, r0 + 128), slice(0, 2304))), O)
        P.dma("sp", aux.t(t, (slice(r0, r0 + 128), slice(0, 260))), AX_)
    if C.stop == "rwkvprep":
        return
    P.phase()
    cols = [dict(lw=0, r=512, k=768, v=1280, a=1536, b=1792), dict(lw=256, r=512, k=1024, v=1280, a=1536, b=2048)]
    P.aoff = C.xt_off
    scan(P, C, sin, cols, yscr, True, "w")
    P.phase()
    gng = bc_load(P, "gng", W["rwkv_gn_g"][l], 256)
    gnb = bc_load(P, "gnb", W["rwkv_gn_b"][l], 256)
    epsg = P.alloc([1], F32, "epsg")
    P.g("memset", ap=epsg, constant=64e-5)
    for t in range(TL[0]):
        sl = t % 2
        r0 = t * 128
        y0 = P.tmp("fy0", [256], F32, sl)
        y1 = P.tmp("fy1", [256], F32, sl)
        vv = P.tmp("fv", [256], F32, sl)
        ax = P.tmp("fax", [260], F32, sl)
        P.dma("sp", y0, yscr.t((0, t), (slice(r0, r0 + 128),)))
        P.dma("sp", y1, yscr.t((1, t), (slice(NT + r0, NT + r0 + 128),)))
        P.dma("sp", vv, sin.t(t, (slice(r0, r0 + 128), slice(1280, 1536))))
        P.dma("sp", ax, aux.t(t, (slice(r0, r0 + 128), slice(0, 260))))
        st = P.tmp("fst", [16], F32, sl)
        sq = P.tmp("fsq", [256], F32, sl)
        P.v("tensor_tensor", out=y0, in0=y0, in1=y1, op=ALU.add)
        y3 = y0.re("p (h d) -> p h d", d=64)
        P.v("tensor_reduce", out=st[:, 0:4], in_=y3, axis=AX.X, op=ALU.add)
        P.v("tensor_scalar", out=st[:, 0:4], in0=st[:, 0:4], scalar1=1.0 / 64.0, scalar2=None, op0=ALU.mult)
        P.v("tensor_tensor", out=y3, in0=y3, in1=st[:, 0:4].un(2).bc([128, 4, 64]), op=ALU.subtract)
        P.a("activation", out=sq, in_=y0, func=AF.Square)
        P.v("tensor_reduce", out=st[:, 4:8], in_=sq.re("p (h d) -> p h d", d=64), axis=AX.X, op=ALU.add)
        P.a("activation", out=st[:, 8:12], in_=st[:, 4:8], func=AF.Sqrt, scale=1.0 / 64.0, bias=epsg[:, 0:1])
        P.v("reciprocal", out=st[:, 12:16], in_=st[:, 8:12])
        P.v("tensor_tensor", out=y3, in0=y3, in1=st[:, 12:16].un(2).bc([128, 4, 64]), op=ALU.mult)
        P.g("tensor_tensor", out=y0, in0=y0, in1=gng, op=ALU.mult)
        P.g("tensor_tensor", out=y0, in0=y0, in1=gnb, op=ALU.add)
        P.v("tensor_tensor", out=vv.re("p (h d) -> p h d", d=64), in0=vv.re("p (h d) -> p h d", d=64),
            in1=ax[:, 256:260].un(2).bc([128, 4, 64]), op=ALU.mult)
        P.v("tensor_tensor", out=y0, in0=y0, in1=vv, op=ALU.add)
        P.v("tensor_tensor", out=y0, in0=y0, in1=ax[:, 0:256], op=ALU.mult)
        P.dma("sp", mixd.t((t, "rwkv"), (slice(r0, r0 + 128), slice(256, 512))), y0)


def stage_hgrn(P, C, W, l, hbuf, mixd, sin, aux, yscr):
    P.phase()
    lb = P.alloc([256], F32, "lb")
    oml = P.alloc([256], F32, "oml")
    if l == 0:
        P.g("memset", ap=lb, constant=0.0)
    else:
        l0 = bc_load(P, "lbl0", W["hgrn_lb_logits"][0], 256)
        l1 = bc_load(P, "lbl1", W["hgrn_lb_logits"][1], 256)
        P.v("tensor_tensor", out=l1, in0=l1, in1=l0, op=ALU.subtract)
        P.a("activation", out=lb, in_=l1, func=AF.Sigmoid)
    P.v("tensor_scalar", out=oml, in0=lb, scalar1=-1.0, scalar2=1.0, op0=ALU.mult, op1=ALU.add)
    o0 = OFF_HGRN
    for t in range(TL[0]):
        sl = t % 2
        r0 = t * 128
        z = P.tmp("hz", [1280], F32, sl)
        P.dma("sp", z, hbuf.t(t, (slice(r0, r0 + 128), slice(o0, o0 + 1280))))
        O = P.tmp("hO", [1536], F32, sl)
        P.a("activation", out=O[:, 512:768], in_=z[:, 0:256], func=AF.Silu)
        sg = P.tmp("hsg", [512], F32, sl)
        P.a("activation", out=sg, in_=z[:, 256:768], func=AF.Sigmoid)
        for dd in range(2):
            s_ = sg[:, dd * 256:(dd + 1) * 256]
            P.v("tensor_tensor", out=s_, in0=s_, in1=oml, op=ALU.mult)
            P.g("tensor_tensor", out=s_, in0=s_, in1=lb, op=ALU.add)
        P.a("activation", out=O[:, 0:512], in_=sg, func=AF.Ln)
        P.v("tensor_scalar", out=O[:, 768:1280], in0=sg, scalar1=-1.0, scalar2=1.0, op0=ALU.mult, op1=ALU.add)
        P.g("tensor_copy", out=O[:, 1280:1536], in_=z[:, 768:1024])
        P.dma("sp", sin.t(t, (slice(r0, r0 + 128), slice(0, 1536))), O)
        P.dma("sp", aux.t(t, (slice(r0, r0 + 128), slice(0, 256))), z[:, 1024:1280])
    if C.stop == "hgrnprep":
        return
    P.phase()
    cols = [dict(lw=0, r=512, k=768, v=1280), dict(lw=256, r=512, k=1024, v=1280)]
    P.aoff = C.xt_off
    scan(P, C, sin, cols, yscr, False, "h")
    if CUT < 6:
        return
    P.phase()
    ng = bc_load(P, "hng", W["hgrn_norm_g"][l], 256)
    epsr = P.alloc([1], F32, "epsh")
    P.g("memset", ap=epsr, constant=RMS_EPS)
    for t in range(TL[0]):
        sl = t % 2
        r0 = t * 128
        y0 = P.tmp("gy0", [256], F32, sl)
        y1 = P.tmp("gy1", [256], F32, sl)
        gg = P.tmp("gg", [256], F32, sl)
        P.dma("sp", y0, yscr.t((0, t), (slice(r0, r0 + 128),)))
        P.dma("sp", y1, yscr.t((1, t), (slice(NT + r0, NT + r0 + 128),)))
        P.dma("sp", gg, aux.t(t, (slice(r0, r0 + 128), slice(0, 256))))
        st = P.tmp("gst", [12], F32, sl)
        sq = P.tmp("gsq", [256], F32, sl)
        P.v("tensor_tensor", out=y0, in0=y0, in1=y1, op=ALU.add)
        P.a("activation", out=sq, in_=y0, func=AF.Square)
        P.v("tensor_reduce", out=st[:, 0:4], in_=sq.re("p (h d) -> p h d", d=64), axis=AX.X, op=ALU.add)
        P.a("activation", out=st[:, 4:8], in_=st[:, 0:4], func=AF.Sqrt, scale=1.0 / 64.0, bias=epsr[:, 0:1])
        P.v("reciprocal", out=st[:, 8:12], in_=st[:, 4:8])
        y3 = y0.re("p (h d) -> p h d", d=64)
        P.v("tensor_tensor", out=y3, in0=y3, in1=st[:, 8:12].un(2).bc([128, 4, 64]), op=ALU.mult)
        P.g("tensor_tensor", out=y0, in0=y0, in1=ng, op=ALU.mult)
        P.a("activation", out=gg, in_=gg, func=AF.Silu)
        P.v("tensor_tensor", out=y0, in0=y0, in1=gg, op=ALU.mult)
        P.dma("sp", mixd.t((t, "hgrn"), (slice(r0, r0 + 128), slice(512, 768))), y0)


def stage_proj_ln(P, C, W, load_src, Kdim, w_ap, g_ap, b_ap, xres, tag):
    P.phase()
    nk = Kdim // 128
    wsb = P.alloc([nk, D], BF16, "w" + tag)
    load_cast(P, wsb, w_ap.rearrange("(kc p) n -> p kc n", p=128))
    g_bc = bc_load(P, "g" + tag, g_ap, D)
    b_bc = bc_load(P, "b" + tag, b_ap, D)
    for t in range(TL[0]):
        sl = t % 2
        src = load_src(t, sl)
        sT = P.tmp("sT" + tag, [nk, 128], BF16, sl)
        transpose_to(P, C, src, Kdim, lambda kc, r: sT[:, kc, :])
        xin = P.tmp("rx" + tag, [D], F32, sl)
        P.dma("sp", xin, xres.t(t, rows(t)))
        pre = P.tmp("pre" + tag, [D], F32, sl)
        for half in range(2):
            ps = P.ps()
            for kc in range(nk):
                P.mm(ps, lhsT=sT[:, kc, :], rhs=wsb[:, kc, half * 512:(half + 1) * 512], start=(kc == 0),
                     stop=(kc == nk - 1))
            P.v("scalar_tensor_tensor", out=pre[:, half * 512:(half + 1) * 512], in0=xin[:, half * 512:(half + 1) * 512],
                scalar=DN_ALPHA, in1=ps, op0=ALU.mult, op1=ALU.add)
        xo = P.tmp("xo" + tag, [D], F32, sl)
        layer_norm_tile(P, C, pre, g_bc, b_bc, xo, sl)
        P.dma("sp", xres.t(t, rows(t)), xo)
        to_xT(P, C, xo, t, sl)


def stage_wout(P, C, W, l, mixd, xres):
    def load_src(t, sl):
        m = P.tmp("mixf", [D], F32, sl)
        keys = [(t, n) for n in ("att", "rwkv", "hgrn", "mla")]
        P.dma("sp", m, mixd.t(keys, rows(t)))
        mb = P.tmp("mixb", [D], BF16, sl)
        P.a("copy", out=mb, in_=m)
        return mb
    stage_proj_ln(P, C, W, load_src, D, W["w_out"][l], W["ln1_g"][l], W["ln1_b"][l], xres, "wo")


def stage_cross(P, C, W, l, xres, catt):
    P.phase()
    KTm = P.alloc([8, NB * 256], BF16, "KTm")
    VSm = P.alloc([NB * 2, 4, 257], BF16, "VSm")
    QTm = P.alloc([8, NT], BF16, "QTm")
    P.g("memset", ap=VSm, constant=1.0)
    mark = P.aoff
    wkv = P.alloc([8, 2 * D], BF16, "wkv")
    load_cast(P, wkv, W["mem_wkv"][l].rearrange("(kc p) n -> p kc n", p=128))
    memT = P.alloc([8, NB * 256], BF16, "memT")
    for mt in range(NB * 2):
        sl = mt % 2
        mf = P.tmp("memf", [D], F32, sl)
        P.dma("sp", mf, ext(W["mem"][mt * 128:(mt + 1) * 128, :]))
        mb = P.tmp("memb", [D], BF16, sl)
        P.a("copy", out=mb, in_=mf)
        transpose_to(P, C, mb, D, lambda kc, r: memT[:, kc, mt * 128:(mt + 1) * 128])
    for hc in range(8):
        ps = P.ps()
        for kc in range(8):
            P.mm(ps, lhsT=wkv[:, kc, hc * 128:(hc + 1) * 128], rhs=memT[:, kc, :], start=(kc == 0), stop=(kc == 7))
        P.v("tensor_copy", out=KTm[:, hc, :], in_=ps)
    for mt in range(NB * 2):
        for half in range(2):
            ps = P.ps()
            for kc in range(8):
                P.mm(ps, lhsT=memT[:, kc, mt * 128:(mt + 1) * 128], rhs=wkv[:, kc, D + half * 512:D + (half + 1) * 512],
                     start=(kc == 0), stop=(kc == 7))
            P.v("tensor_copy", out=VSm[:, mt, half * 2:half * 2 + 2, 0:256], in_=ps.re("p (h d) -> p h d", h=2))
    P.barrier()
    P.aoff = mark
    P.cache = {}
    wq = P.alloc([8, D], BF16, "wq")
    load_cast(P, wq, W["mem_wq"][l].rearrange("(kc p) n -> p kc n", p=128))
    it = 0
    for hc in range(8):
        for tb in range(TL[0] // 4):
            ps = P.ps()
            for kc in range(8):
                P.mm(ps, lhsT=wq[:, kc, hc * 128:(hc + 1) * 128], rhs=C.XT[:, kc, tb * 512:(tb + 1) * 512],
                     start=(kc == 0), stop=(kc == 7))
            if it % 2 == 0:
                P.v("tensor_copy", out=QTm[:, hc, tb * 512:(tb + 1) * 512], in_=ps)
            else:
                P.a("copy", out=QTm[:, hc, tb * 512:(tb + 1) * 512], in_=ps)
            it += 1
    P.barrier()
    P.aoff = mark
    P.cache = {}

    def chunks(b, h):
        return [(KTm[:, h * 2 + c, b * 256:(b + 1) * 256], QTm[:, h * 2 + c, b * S:(b + 1) * S]) for c in range(2)]

    def vfn(b, h, kt):
        return VSm[:, b * 2 + kt, h, :]

    def outfn(b, h, qb, o_sb):
        r0 = b * S + qb * 512
        keys = [(r0 // 128 + i, h) for i in range(4)]
        dst = catt.t(keys, (slice(r0, r0 + 512), slice(h * 256, (h + 1) * 256))).re("(q p) d -> p q d", p=128)
        P.dma("sp", dst, o_sb)

    attention(P, C, 2, chunks, vfn, 256, 1.0 / 16.0, outfn, 4, "c")

    def load_src(t, sl):
        m = P.tmp("caf", [D], F32, sl)
        P.dma("sp", m, catt.t([(t, h) for h in range(4)], rows(t)))
        mb = P.tmp("cab", [D], BF16, sl)
        P.a("copy", out=mb, in_=m)
        return mb
    stage_proj_ln(P, C, W, load_src, D, W["mem_wo"][l], W["ln2_g"][l], W["ln2_b"][l], xres, "co")


EG = 4


def stage_moe(P, C, W, l, xres, yacc):
    P.phase()
    gates = P.alloc([NTILE, 32], F32, "gates")
    wg = P.alloc([8, 36], F32, "wg")
    P.dma("sp", wg[:, :, 0:4], ext(W["moe_wg_group"][l].rearrange("(kc p) n -> p kc n", p=128)))
    P.dma("sp", wg[:, :, 4:36], ext(W["moe_wg_expert"][l].rearrange("(kc p) n -> p kc n", p=128)))
    bg = P.alloc([36], F32, "bg")
    P.dma("sp", bg[:, 0:4], ext(W["moe_bg_group"][l].partition_broadcast(128)))
    P.dma("sp", bg[:, 4:36], ext(W["moe_bg_expert"][l].partition_broadcast(128)))
    mark = P.aoff
    BIG = 1.0e30
    for t in range(TL[0]):
        sl = t % 2
        xf = P.tmp("mxf", [D], F32, sl)
        P.dma("sp", xf, xres.t(t, rows(t)))
        xTf = P.tmp("mxT", [8, 128], F32, sl)
        for half in range(2):
            pst = P.ps()
            for j in range(4):
                kc = half * 4 + j
                P.tr(pst[:, j * 128:(j + 1) * 128], xf[:, kc * 128:(kc + 1) * 128], C.identf, inc=(j == 3))
            P.v("tensor_copy", out=xTf[:, half * 4:half * 4 + 4, :], in_=pst.re("p (a b) -> p a b", a=4))
        psl = P.ps()
        for kc in range(8):
            P.mm(psl[:, 0:36], lhsT=xTf[:, kc, :], rhs=wg[:, kc, :], start=(kc == 0), stop=(kc == 7))
        lg = P.tmp("mlg", [36], F32, sl)
        P.v("tensor_tensor", out=lg, in0=psl[:, 0:36], in1=bg, op=ALU.add)
        st = P.tmp("mst", [16], F32, sl)
        gm = P.tmp("mgm", [4], F32, sl)
        junk = P.tmp("mjk", [4], F32, sl)
        P.v("tensor_reduce", out=st[:, 0:1], in_=lg[:, 0:4], axis=AX.X, op=ALU.max)
        P.v("tensor_scalar", out=gm, in0=lg[:, 0:4], scalar1=st[:, 0:1], scalar2=None, op0=ALU.is_ge)
        P.v("tensor_scalar", out=st[:, 1:2], in0=st[:, 0:1], scalar1=-1.0, scalar2=None, op0=ALU.mult)
        P.a("activation", out=junk, in_=lg[:, 0:4], func=AF.Exp, bias=st[:, 1:2], scale=1.0, accum_out=st[:, 2:3])
        P.v("reciprocal", out=st[:, 3:4], in_=st[:, 2:3])
        em = P.tmp("mem_", [32], F32, sl)
        pen = P.tmp("mpen", [4], F32, sl)
        P.v("tensor_scalar", out=pen, in0=gm, scalar1=-1.0, scalar2=BIG, op0=ALU.add, op1=ALU.mult)
        P.v("tensor_tensor", out=em.re("p (g e) -> p g e", g=4), in0=lg[:, 4:36].re("p (g e) -> p g e", g=4),
            in1=pen.un(2).bc([128, 4, 8]), op=ALU.add)
        m1 = P.tmp("mm1", [32], F32, sl)
        m2 = P.tmp("mm2", [32], F32, sl)
        em2 = P.tmp("mem2", [32], F32, sl)
        P.v("tensor_reduce", out=st[:, 4:5], in_=em, axis=AX.X, op=ALU.max)
        P.v("tensor_scalar", out=m1, in0=em, scalar1=st[:, 4:5], scalar2=None, op0=ALU.is_ge)
        P.v("scalar_tensor_tensor", out=em2, in0=m1, scalar=-BIG, in1=em, op0=ALU.mult, op1=ALU.add)
        P.v("tensor_reduce", out=st[:, 5:6], in_=em2, axis=AX.X, op=ALU.max)
        P.v("tensor_scalar", out=m2, in0=em2, scalar1=st[:, 5:6], scalar2=None, op0=ALU.is_ge)
        P.v("tensor_tensor", out=st[:, 6:7], in0=st[:, 5:6], in1=st[:, 4:5], op=ALU.subtract)
        P.a("activation", out=st[:, 7:8], in_=st[:, 6:7], func=AF.Exp)
        P.v("tensor_scalar", out=st[:, 8:9], in0=st[:, 7:8], scalar1=1.0, scalar2=None, op0=ALU.add)
        P.v("reciprocal", out=st[:, 9:10], in_=st[:, 8:9])
        P.v("tensor_tensor", out=st[:, 10:11], in0=st[:, 7:8], in1=st[:, 9:10], op=ALU.mult)
        P.v("tensor_tensor", out=st[:, 9:11], in0=st[:, 9:11], in1=st[:, 3:4].bc([128, 2]), op=ALU.mult)
        P.v("tensor_scalar", out=m1, in0=m1, scalar1=st[:, 9:10], scalar2=None, op0=ALU.mult)
        P.v("scalar_tensor_tensor", out=gates[:, t, :], in0=m2, scalar=st[:, 10:11], in1=m1, op0=ALU.mult, op1=ALU.add)
    if C.stop == "moegate":
        C.gates = gates
        return
    P.barrier()
    P.aoff = mark
    P.cache = {}
    P.nrot = 4
    ngrp = 32 // EG
    for g in range(ngrp):
        gs = g % 2
        w13 = P.tmp("w13", [EG, 8, 512], BF16, gs)
        w2 = P.tmp("w2", [EG, 2, D], BF16, gs)
        for e in range(EG):
            ee = g * EG + e
            P.dma("pool", w13[:, e, :, 0:256], ext(W["moe_w1"][l, ee].rearrange("(kc p) n -> p kc n", p=128)))
            P.dma("pool", w13[:, e, :, 256:512], ext(W["moe_w3"][l, ee].rearrange("(kc p) n -> p kc n", p=128)))
            P.dma("pool", w2[:, e, :, :], ext(W["moe_w2"][l, ee].rearrange("(c p) n -> p c n", p=128)))
        def stageA(t, e, idx):
            ee = g * EG + e
            ps = P.ps()
            for kc in range(8):
                P.mm(ps, lhsT=C.XT[:, kc, t * 128:(t + 1) * 128], rhs=w13[:, e, kc, :], start=(kc == 0), stop=(kc == 7))
            sa = P.tmp("msa", [256], F32, idx % 2)
            P.a("activation", out=sa, in_=ps[:, 0:256], func=AF.Silu)
            hb = P.tmp("mhb", [256], BF16, idx % 3)
            P.v("scalar_tensor_tensor", out=hb, in0=ps[:, 256:512], scalar=gates[:, t, ee:ee + 1], in1=sa,
                op0=ALU.mult, op1=ALU.mult)
            return hb

        def stageB(t, e, hb, idx):
            sl = t % 2
            psy = [P.psum[4 + 2 * (t % 2)], P.psum[5 + 2 * (t % 2)]]
            pt = P.ps()
            ptb = pt.bitcast(BF16)
            P.tr(ptb[:, 0:128], hb[:, 0:128], C.identb, inc=False)
            P.tr(ptb[:, 128:256], hb[:, 128:256], C.identb)
            hT = P.tmp("mhT", [2, 128], BF16, idx % 2)
            P.v("tensor_copy", out=hT, in_=ptb[:, 0:256].re("p (c t) -> p c t", c=2))
            for c in range(2):
                for half in range(2):
                    first = (e == 0 and c == 0)
                    last = (e == EG - 1 and c == 1)
                    P.mm(psy[half], lhsT=hT[:, c, :], rhs=w2[:, e, c, half * 512:(half + 1) * 512], start=first, stop=last,
                         inc=(last and half == 1))
            if e != EG - 1:
                return
            ya = P.tmp("mya", [D], F32, sl)
            if g == 0:
                P.v("tensor_copy", out=ya[:, 0:512], in_=psy[0])
                P.a("copy", out=ya[:, 512:1024], in_=psy[1])
            else:
                yo = P.tmp("myo", [D], F32, sl)
                P.dma("sp", yo, yacc.t(t, rows(t)))
                P.v("tensor_tensor", out=ya[:, 0:512], in0=yo[:, 0:512], in1=psy[0], op=ALU.add)
                P.v("tensor_tensor", out=ya[:, 512:1024], in0=yo[:, 512:1024], in1=psy[1], op=ALU.add)
            P.dma("sp", yacc.t(t, rows(t)), ya)

        prev = None
        idx = 0
        for t in range(TL[0]):
            for e in range(EG):
                hb = stageA(t, e, idx)
                if prev is not None:
                    stageB(*prev)
                prev = (t, e, hb, idx)
                idx += 1
        stageB(*prev)
    P.nrot = 8
    P.phase()
    g_bc = bc_load(P, "g3", W["ln3_g"][l], D)
    b_bc = bc_load(P, "b3", W["ln3_b"][l], D)
    for t in range(TL[0]):
        sl = t % 2
        xin = P.tmp("x3i", [D], F32, sl)
        yy = P.tmp("y3i", [D], F32, sl)
        P.dma("sp", xin, xres.t(t, rows(t)))
        P.dma("sp", yy, yacc.t(t, rows(t)))
        P.v("scalar_tensor_tensor", out=yy, in0=xin, scalar=DN_ALPHA, in1=yy, op0=ALU.mult, op1=ALU.add)
        xo = P.tmp("x3o", [D], F32, sl)
        layer_norm_tile(P, C, yy, g_bc, b_bc, xo, sl)
        P.dma("sp", xres.t(t, rows(t)), xo)
        to_xT(P, C, xo, t, sl)


def stage_ln_in(P, C, W, xres):
    P.phase()
    g_bc = P.alloc([D], F32, "g_bc")
    b_bc = P.alloc([D], F32, "b_bc")
    P.dma("sp", g_bc, ext(W["ln_in_g"].partition_broadcast(128)))
    P.dma("sp", b_bc, ext(W["ln_in_b"].partition_broadcast(128)))
    for t in range(TL[0]):
        sl = t % 2
        xin = P.tmp("xin", [D], F32, sl)
        xo = P.tmp("xo", [D], F32, sl)
        P.dma("sp", xin, ext(W["x"][t * 128:(t + 1) * 128, :]))
        layer_norm_tile(P, C, xin, g_bc, b_bc, xo, sl)
        P.dma("sp", xres.t(t, rows(t)), xo)
        to_xT(P, C, xo, t, sl)


def stage_win(P, C, W, l, hbuf):
    P.phase()
    win = P.alloc([8, INW], BF16, "win")
    load_cast(P, win, W["w_in"][l].rearrange("(kc p) n -> p kc n", p=128), maxcols=776)
    for t in range(TL[0]):
        sl = t % 2
        ho = P.tmp("ho", [INW], F32, sl)
        n0 = 0
        while n0 < INW:
            n1 = min(INW, n0 + 512)
            ps = P.ps()
            for kc in range(8):
                P.mm(ps[:, 0:n1 - n0], C.XT[:, kc, t * 128:(t + 1) * 128], win[:, kc, n0:n1],
                     start=(kc == 0), stop=(kc == 7))
            if (n0 // 512) % 2 == 0:
                P.v("tensor_copy", out=ho[:, n0:n1], in_=ps[:, 0:n1 - n0])
            else:
                P.a("copy", out=ho[:, n0:n1], in_=ps[:, 0:n1 - n0])
            n0 = n1
        P.dma("sp", hbuf.t(t, rows(t)), ho)


def dump(P, nc, name, dt, shape, keys, c0=0, c1=None):
    c1 = shape[1] if c1 is None else c1
    dd = nc.dram_tensor("dbg_" + name, [shape[0], c1 - c0], F32, kind="ExternalOutput").ap()
    nrow = shape[0]
    step = 512
    ob = Buf()
    for i, r0 in enumerate(range(0, nrow, 128)):
        bt = P.tmp("dump" + name, [c1 - c0], F32, i % 2)
        P.dma("sp", bt, dt.t(list(keys), (slice(r0, r0 + 128), slice(c0, c1))))
        P.dma("sp", T(dd[r0:r0 + 128], [ob]), bt)


def build(n_layers=DEPTH, dbg=(), stop_after=None):
    nc = bass.Bass("TRN2", target_bir_lowering=False)
    P = Prog(nc, 208000 // 4)
    C = Ctx()
    C.stop = stop_after
    W = {}
    for name, shp in kernel_shapes().items():
        W[name] = nc.dram_tensor(name, list(shp), F32, kind="ExternalInput").ap()
    out_d = nc.dram_tensor("out", [NT, D], F32, kind="ExternalOutput").ap()

    C.identf = P.alloc([128], F32, "identf")
    C.identb = P.alloc([128], BF16, "identb")
    C.eps_ln = P.alloc([1], F32, "eps_ln")
    C.xt_off = P.aoff
    C.XT = P.alloc([8, NT], BF16, "XT")
    P.persist = P.aoff
    C.W = W
    P.dma("sp", C.identf, ext(W["c_ident"]))
    P.v("tensor_copy", out=C.identb, in_=C.identf)
    P.g("memset", ap=C.eps_ln, constant=LN_EPS)

    xres = P.dram("xres", [NT, D])
    hbuf = P.dram("hbuf", [NT, INW])
    mixd = P.dram("mixd", [NT, D])
    sin = P.dram("sin", [NT, 2304])
    aux = P.dram("aux", [NT, 260])
    yscr = P.dram("yscr", [2 * NT, 256])
    catt = P.dram("catt", [NT, D])
    yacc = P.dram("yacc", [NT, D])

    def done(tag):
        return stop_after == tag

    stage_ln_in(P, C, W, xres)
    for l in range(n_layers):
        stage_win(P, C, W, l, hbuf)
        if done("win"):
            break
        if "hgrn" in str(stop_after):
            stage_hgrn(P, C, W, l, hbuf, mixd, sin, aux, yscr)
            break
        if "rwkv" in str(stop_after):
            stage_rwkv(P, C, W, l, hbuf, mixd, sin, aux, yscr)
            break
        stage_gqa(P, C, W, l, hbuf, mixd)
        if done("gqa") or done("gqaprep"):
            break
        stage_mla(P, C, W, l, hbuf, mixd)
        if done("mla") or done("mlaprep"):
            break
        stage_hgrn(P, C, W, l, hbuf, mixd, sin, aux, yscr)
        stage_rwkv(P, C, W, l, hbuf, mixd, sin, aux, yscr)
        stage_wout(P, C, W, l, mixd, xres)
        if done("x1"):
            break
        stage_cross(P, C, W, l, xres, catt)
        if done("x2"):
            break
        stage_moe(P, C, W, l, xres, yacc)
        if done("x3"):
            break

    P.phase()
    allt = list(range(NTILE))
    if "h" in dbg:
        dump(P, nc, "h", hbuf, [NT, INW], allt)
    if "qt" in dbg:
        QT, KT, VS = C.dbgT
        dq = nc.dram_tensor("dbg_qt", [64, 4 * NT], BF16, kind="ExternalOutput").ap()
        P.dma("sp", T(dq, [Buf()]), QT[0:64].re("p h t -> p (h t)"))
        dk = nc.dram_tensor("dbg_kt", [64, 2 * NT], BF16, kind="ExternalOutput").ap()
        P.dma("sp", T(dk, [Buf()]), KT[0:64].re("p h t -> p (h t)"))
    if "mix" in dbg:
        keys = [(t, m) for t in allt for m in dbg["mix"]]
        dump(P, nc, "mix", mixd, [NT, D], keys, dbg.get("c0", 0), dbg.get("c1", D))
    ob = Buf()
    for t in range(TL[0]):
        bt = P.tmp("outb", [D], F32, t % 2)
        P.dma("sp", bt, xres.t(t, rows(t)))
        P.dma("sp", T(out_d[t * 128:(t + 1) * 128, :], [ob]), bt)
    P.barrier()
    P.emit()
    return nc, P


def prep_inputs(inputs, cores):
    consts = host_consts()
    maps = []
    shapes = kernel_shapes()
    for c in cores:
        m = {}
        m["x"] = np.ascontiguousarray(np.asarray(inputs["x"])[c * NB:(c + 1) * NB].reshape(NT, D))
        m["mem"] = np.ascontiguousarray(np.asarray(inputs["mem"])[c * NB:(c + 1) * NB].reshape(NB * 256, D))
        for k, shp in shapes.items():
            if k in ("x", "mem"):
                continue
            if k in consts:
                m[k] = consts[k]
            else:
                m[k] = np.ascontiguousarray(np.asarray(inputs[k], dtype=np.float32).reshape(shp))
        maps.append(m)
    return maps


def run(inputs, cores, n_layers=DEPTH, dbg=(), stop_after=None):
    nc, P = build(n_layers, dbg, stop_after)
    maps = prep_inputs(inputs, cores)
    res = run_bass_kernel_spmd(nc, maps, core_ids=list(range(len(cores))))
    return res.results, P


def kernel(**inputs):
    results, _ = run(inputs, list(range(8)))
    out = np.concatenate([r["out"].reshape(NB, S, D) for r in results], axis=0)
    return out.astype(np.float32)
```

```python
import math
import os
CUT = int(os.environ.get('CUT', '99'))
import numpy as np
import concourse.bass as bass
import concourse.mybir as mybir
from concourse.bass_utils import run_bass_kernel_spmd

F32 = mybir.dt.float32
BF16 = mybir.dt.bfloat16
AF = mybir.ActivationFunctionType
ALU = mybir.AluOpType
AX = mybir.AxisListType

ENGS = ["pe", "act", "dve", "pool", "sp"]
WKEYS = {"out", "accum_out", "ap"}
EPOCH = 24000
NDSEM = 72
NDSEM_HW = 40
STORE_Q = os.environ.get('STORE_Q', 'pool')


class Buf:
    __slots__ = ("name", "writes", "reads")

    def __init__(self, name=""):
        self.name = name
        self.writes = {}
        self.reads = {}


class T:
    __slots__ = ("ap", "bufs", "dram")

    def __init__(self, ap, bufs, dram=False):
        self.ap = ap
        self.bufs = bufs
        self.dram = dram

    def __getitem__(self, k):
        return T(self.ap[k], self.bufs, self.dram)

    def re(self, s, **kw):
        return T(self.ap.rearrange(s, **kw), self.bufs, self.dram)

    def bc(self, shape):
        return T(self.ap.broadcast_to(shape), self.bufs)

    def un(self, ax):
        return T(self.ap.unsqueeze(ax), self.bufs)

    def bitcast(self, dt):
        return T(self.ap.bitcast(dt), self.bufs)

    @property
    def shape(self):
        return self.ap.shape


def _key(ev):
    return (ev[0], ev[1])


class Prog:
    def __init__(self, nc, arena_words):
        self.nc = nc
        self.stream = {e: [] for e in ENGS}
        self.tick = {e: 0 for e in ENGS}
        self.esems = {e: [] for e in ENGS}
        self.seen = {e: {} for e in ENGS}
        self.dsems = [nc.alloc_semaphore(f"dsem{i}") for i in range(NDSEM)]
        self.dtot = [0] * NDSEM
        self.drr = 0
        self.drr_sw = 0
        self.pe_pending = []
        self.arena = nc.alloc_sbuf_tensor("arena", [128, arena_words], F32)
        self.arena_words = arena_words
        self.aoff = 0
        self.persist = 0
        self.psum = [T(nc.alloc_psum_tensor(f"psb{i}", [128, 512], F32)[:, :], [Buf(f"ps{i}")]) for i in range(8)]
        self.nbuf = 0
        self.ninstr = 0
        self.cache = {}
        self.pr = 0
        self.nrot = 8

    def alloc(self, free_shape, dtype=F32, name=None):
        n = int(np.prod(free_shape))
        words = (n + 1) // 2 if dtype == BF16 else n
        words = (words + 7) // 8 * 8
        assert self.aoff + words <= self.arena_words, f"arena overflow {self.aoff}+{words} ({name})"
        ap = self.arena[:, self.aoff:self.aoff + words]
        self.aoff += words
        if dtype == BF16:
            ap = ap.bitcast(BF16)[:, 0:n]
        else:
            ap = ap[:, 0:n]
        if len(free_shape) == 2:
            ap = ap.rearrange("p (a b) -> p a b", a=free_shape[0])
        elif len(free_shape) == 3:
            ap = ap.rearrange("p (a b c) -> p a b c", a=free_shape[0], b=free_shape[1])
        elif len(free_shape) == 4:
            ap = ap.rearrange("p (a b c d) -> p a b c d", a=free_shape[0], b=free_shape[1], c=free_shape[2])
        self.nbuf += 1
        return T(ap, [Buf(name or f"b{self.nbuf}")])

    def tmp(self, name, free_shape, dtype=F32, slot=0):
        k = (name, slot)
        if k not in self.cache:
            self.cache[k] = self.alloc(free_shape, dtype, f"{name}{slot}")
        return self.cache[k]

    def ps(self):
        p = self.psum[self.pr % self.nrot]
        self.pr += 1
        return p

    def dram(self, name, shape, dtype=F32):
        ap = self.nc.dram_tensor(name, list(shape), dtype, kind="Internal").ap()
        return DT(ap)

    def _esem(self, P, ep):
        while len(self.esems[P]) <= ep:
            self.esems[P].append(self.nc.alloc_semaphore(f"es_{P}_{len(self.esems[P])}"))
        return self.esems[P][ep]

    def _wait(self, X, ev):
        k = _key(ev)
        v = ev[2]
        if self.seen[X].get(k, 0) >= v:
            return
        self.seen[X][k] = v
        if ev[0] == "e":
            ep = (v - 1) // EPOCH
            sem = self._esem(ev[1], ep)
            val = v - ep * EPOCH
        else:
            sem = self.dsems[ev[1]]
            val = v
        self.stream[X].append(lambda e, sem=sem, val=val: e.wait_ge(sem, val))

    def _deps(self, X, wbufs, rbufs):
        for b in rbufs:
            for ev in b.writes.values():
                self._wait(X, ev)
        for b in wbufs:
            for ev in list(b.writes.values()) + list(b.reads.values()):
                if ev[0] == "e" and ev[1] == X and X == "pe":
                    continue
                self._wait(X, ev)

    def _record(self, ev, wbufs, rbufs):
        k = _key(ev)
        for b in wbufs:
            if b.reads:
                b.writes = {}
                b.reads = {}
            b.writes[k] = ev
        for b in rbufs:
            b.reads[k] = ev

    def op(self, X, call, wbufs, rbufs, inc=True):
        self._deps(X, wbufs, rbufs)
        self.ninstr += 1
        if not inc:
            assert X == "pe"
            self.pe_pending.append((wbufs, rbufs))
            self.stream[X].append(lambda e: call(e))
            return
        self.tick[X] += 1
        t = self.tick[X]
        ep = (t - 1) // EPOCH
        sem = self._esem(X, ep)
        self.stream[X].append(lambda e: call(e).then_inc(sem, 1))
        ev = ("e", X, t)
        if X == "pe" and self.pe_pending:
            for (w, r) in self.pe_pending:
                self._record(ev, w, r)
            self.pe_pending = []
        self._record(ev, wbufs, rbufs)

    def ins(self, X, fname, **kw):
        wb, rb, real = [], [], {}
        for k, v in kw.items():
            if isinstance(v, T):
                (wb if k in WKEYS else rb).extend(v.bufs)
                real[k] = v.ap
            else:
                real[k] = v
        eng = {"pe": "tensor", "act": "scalar", "dve": "vector", "pool": "gpsimd", "sp": "sync"}[X]
        self.op(X, lambda e: getattr(e, fname)(**real), wb, rb)

    def v(self, fname, **kw):
        self.ins("dve", fname, **kw)

    def a(self, fname, **kw):
        self.ins("act", fname, **kw)

    def g(self, fname, **kw):
        self.ins("pool", fname, **kw)

    def mm(self, out, lhsT, rhs, start=True, stop=True, inc=None, **kw):
        if inc is None:
            inc = stop
        o, l, r = out.ap, lhsT.ap, rhs.ap
        self.op("pe", lambda e: e.matmul(o, lhsT=l, rhs=r, start=start, stop=stop, **kw),
                list(out.bufs), list(lhsT.bufs) + list(rhs.bufs), inc=inc)

    def tr(self, out, in_, ident, inc=True):
        o, i, d = out.ap, in_.ap, ident.ap
        self.op("pe", lambda e: e.transpose(out=o, in_=i, identity=d),
                list(out.bufs), list(in_.bufs) + list(ident.bufs), inc=inc)

    def dma(self, Q, out, in_, **kw):
        if Q == "sp" and out.dram and STORE_Q != "sp":
            Q = STORE_Q
        wb, rb = list(out.bufs), list(in_.bufs)
        self._deps(Q, wb, rb)
        if Q == "pool":
            s = NDSEM_HW + self.drr_sw
            self.drr_sw = (self.drr_sw + 1) % (NDSEM - NDSEM_HW)
        else:
            s = self.drr
            self.drr = (self.drr + 1) % NDSEM_HW
        if self.dtot[s] > 0:
            self._wait(Q, ("d", s, self.dtot[s]))
        self.dtot[s] += 16
        sem = self.dsems[s]
        o, i = out.ap, in_.ap
        self.ninstr += 1
        self.stream[Q].append(lambda e: e.dma_start(out=o, in_=i, **kw).then_inc(sem, 16))
        ev = ("d", s, self.dtot[s])
        self._record(ev, wb, rb)

    def barrier(self):
        for X in ENGS:
            for Pn in ENGS:
                if Pn != X and self.tick[Pn] > 0:
                    self._wait(X, ("e", Pn, self.tick[Pn]))
            for s in range(NDSEM):
                if self.dtot[s] > 0:
                    self._wait(X, ("d", s, self.dtot[s]))

    def phase(self):
        self.barrier()
        self.aoff = self.persist
        self.cache = {}

    def emit(self):
        nc = self.nc
        with nc.Block() as block:
            @block.tensor
            def _(e):
                for f in self.stream["pe"]:
                    f(e)

            @block.scalar
            def _(e):
                for f in self.stream["act"]:
                    f(e)

            @block.vector
            def _(e):
                for f in self.stream["dve"]:
                    f(e)

            @block.gpsimd
            def _(e):
                for f in self.stream["pool"]:
                    f(e)

            @block.sync
            def _(e):
                for f in self.stream["sp"]:
                    f(e)


class DT:
    def __init__(self, ap):
        self.ap = ap
        self.regs = {}

    def t(self, keys, idx=None):
        if not isinstance(keys, (list, tuple)) or (isinstance(keys, tuple)):
            keys = [keys]
        bufs = []
        for k in keys:
            if k not in self.regs:
                self.regs[k] = Buf(str(k))
            bufs.append(self.regs[k])
        ap = self.ap if idx is None else self.ap[idx]
        return T(ap, bufs, True)


def ext(ap):
    return T(ap, [])


D = 1024
S = 2048
NB = 2
NT = NB * S
NTILE = NT // 128
TPS = S // 128
DEPTH = 2
INW = 3104
ATT_IN, RWKV_IN, HGRN_IN, MLA_IN = 512, 960, 1280, 352
OFF_ATT, OFF_RWKV, OFF_HGRN, OFF_MLA = 0, 512, 1472, 2752
DN_ALPHA = (2 * DEPTH) ** 0.25
LN_EPS = 1e-5
RMS_EPS = 1e-6

WEIGHT_NAMES = [
    'ln_in_g', 'ln_in_b', 'w_in', 'attn_q_norm', 'attn_k_norm', 'rwkv_mu_prev', 'rwkv_mu_next',
    'rwkv_w0', 'rwkv_w2', 'rwkv_a0', 'rwkv_a2', 'rwkv_g2', 'rwkv_k_k', 'rwkv_k_a', 'rwkv_r_k',
    'rwkv_gn_g', 'rwkv_gn_b', 'hgrn_lb_logits', 'hgrn_norm_g', 'mla_q_norm', 'mla_kv_norm',
    'mla_w_uq', 'mla_w_ukv', 'w_out', 'ln1_g', 'ln1_b', 'mem_wq', 'mem_wkv', 'mem_wo', 'ln2_g',
    'ln2_b', 'moe_wg_group', 'moe_bg_group', 'moe_wg_expert', 'moe_bg_expert', 'moe_w1', 'moe_w3',
    'moe_w2', 'ln3_g', 'ln3_b']


def host_consts():
    c = {}
    c["c_ident"] = np.eye(128, dtype=np.float32)
    return c


ROPE_THETA = 10000.0
TL = [NTILE]


def _rope_tab(half_dim, nrep):
    t = np.arange(S)
    row = (t // 64).astype(np.float64)
    col = (t % 64).astype(np.float64)
    nf = half_dim // 2
    inv = ROPE_THETA ** (-np.arange(0, half_dim, 2, dtype=np.float64) / half_dim)
    ar = row[:, None] * inv[None, :]
    ac = col[:, None] * inv[None, :]
    Ct = np.concatenate([np.cos(ar), np.cos(ar), np.cos(ac), np.cos(ac)], axis=1)
    St = np.concatenate([-np.sin(ar), np.sin(ar), -np.sin(ac), np.sin(ac)], axis=1)
    return (np.tile(Ct, (1, nrep)).astype(np.float32), np.tile(St, (1, nrep)).astype(np.float32))


def host_consts():
    c = {}
    c["c_ident"] = np.eye(128, dtype=np.float32)
    c["c_ropeC6"], c["c_ropeS6"] = _rope_tab(32, 6)
    c["c_ropeC5"], c["c_ropeS5"] = _rope_tab(16, 5)
    sc, bd = _scan_consts()
    c["c_scan"] = np.ascontiguousarray(sc.transpose(1, 0, 2))
    c["c_bd"] = bd
    rm = np.zeros((128, 2), np.float32)
    rm[:64, 0] = 1.0
    rm[64:, 1] = 1.0
    c["c_rowmask"] = np.ascontiguousarray(np.repeat(rm[:, :, None], 256, axis=2))
    return c


def kernel_shapes():
    L = DEPTH
    s = {
        "x": (NT, D), "mem": (NB * 256, D),
        "c_ident": (128, 128), "c_ropeC6": (S, 384), "c_ropeS6": (S, 384),
        "c_ropeC5": (S, 160), "c_ropeS5": (S, 160), "c_scan": (128, 2, 640), "c_bd": (128, 128), "c_rowmask": (128, 2, 256),
        "ln_in_g": (D,), "ln_in_b": (D,), "w_in": (L, D, INW),
        "attn_q_norm": (L, 64), "attn_k_norm": (L, 64),
        "rwkv_mu_prev": (L, 960), "rwkv_mu_next": (L, 960), "rwkv_w0": (L, 2, 256), "rwkv_w2": (L, 2, 32, 256),
        "rwkv_a0": (L, 2, 256), "rwkv_a2": (L, 2, 32, 256), "rwkv_g2": (L, 64, 256), "rwkv_k_k": (L, 256),
        "rwkv_k_a": (L, 256), "rwkv_r_k": (L, 4, 64), "rwkv_gn_g": (L, 256), "rwkv_gn_b": (L, 256),
        "hgrn_lb_logits": (L, 256), "hgrn_norm_g": (L, 256),
        "mla_q_norm": (L, 192), "mla_kv_norm": (L, 128), "mla_w_uq": (L, 192, 384), "mla_w_ukv": (L, 128, 512),
        "w_out": (L, D, D), "ln1_g": (L, D), "ln1_b": (L, D),
        "mem_wq": (L, D, D), "mem_wkv": (L, D, 2 * D), "mem_wo": (L, D, D), "ln2_g": (L, D), "ln2_b": (L, D),
        "moe_wg_group": (L, D, 4), "moe_bg_group": (L, 4), "moe_wg_expert": (L, D, 32), "moe_bg_expert": (L, 32),
        "moe_w1": (L, 32, D, 256), "moe_w3": (L, 32, D, 256), "moe_w2": (L, 32, 256, D),
        "ln3_g": (L, D), "ln3_b": (L, D),
    }
    return s


class Ctx:
    pass


def load_cast(P, dst, src_ap, q="pool", maxcols=1024):
    n = src_ap.shape[-1]
    c0 = 0
    while c0 < n:
        c1 = min(n, c0 + maxcols)
        P.dma(q, dst[..., c0:c1], ext(src_ap[..., c0:c1]))
        c0 = c1


def rows(t):
    return (slice(t * 128, (t + 1) * 128),)


def layer_norm_tile(P, C, xin, g_bc, b_bc, out, sl):
    st = P.tmp("lnst", [4], F32, sl)
    xc = P.tmp("lnxc", [D], F32, sl)
    junk = P.tmp("lnjunk", [D], F32, sl)
    P.a("activation", out=junk, in_=xin, func=AF.Identity, scale=1.0 / D, accum_out=st[:, 0:1])
    P.v("tensor_scalar", out=xc, in0=xin, scalar1=st[:, 0:1], scalar2=None, op0=ALU.subtract)
    P.a("activation", out=junk, in_=xc, func=AF.Square, scale=1.0 / 32.0, accum_out=st[:, 1:2])
    P.a("activation", out=st[:, 2:3], in_=st[:, 1:2], func=AF.Sqrt, bias=C.eps_ln[:, 0:1], scale=1.0)
    P.v("reciprocal", out=st[:, 3:4], in_=st[:, 2:3])
    P.v("scalar_tensor_tensor", out=xc, in0=xc, scalar=st[:, 3:4], in1=g_bc, op0=ALU.mult, op1=ALU.mult)
    P.g("tensor_tensor", out=out, in0=xc, in1=b_bc, op=ALU.add)


def transpose_to(P, C, xb, ncols, dst_fn):
    kc = 0
    nk = (ncols + 127) // 128
    while kc < nk:
        n = min(4, nk - kc)
        ps = P.ps()
        psb = ps.bitcast(BF16)
        for j in range(n):
            c0 = (kc + j) * 128
            c1 = min(ncols, c0 + 128)
            P.tr(psb[0:c1 - c0, j * 128:(j + 1) * 128], xb[:, c0:c1], C.identb, inc=(j == n - 1))
        for j in range(n):
            c0 = (kc + j) * 128
            c1 = min(ncols, c0 + 128)
            dst = dst_fn(kc + j, c1 - c0)
            if j % 2 == 0:
                P.v("tensor_copy", out=dst, in_=psb[0:c1 - c0, j * 128:(j + 1) * 128])
            else:
                P.a("copy", out=dst, in_=psb[0:c1 - c0, j * 128:(j + 1) * 128])
        kc += n


def to_xT(P, C, xn, tile, sl):
    xb = P.tmp("xb", [D], BF16, sl)
    P.a("copy", out=xb, in_=xn)
    transpose_to(P, C, xb, D, lambda kc, r: C.XT[:, kc, tile * 128:(tile + 1) * 128])


def attention(P, C, nkt, chunks_fn, v_fn, dv, scale, out_fn, heads, tag):
    gpb = max(1, 512 // (dv + 1))
    it = 0
    for b in range(NB):
        for h in range(heads):
            ch = chunks_fn(b, h)
            for qb in range(4):
                sl = it % 2
                it += 1
                PT = P.tmp("PT" + tag, [nkt, 512], BF16, sl)
                for kt in range(nkt):
                    ps = P.ps()
                    for ci, (K_, Q_) in enumerate(ch):
                        P.mm(ps, lhsT=K_[:, kt * 128:(kt + 1) * 128], rhs=Q_[:, qb * 512:(qb + 1) * 512],
                             start=(ci == 0), stop=(ci == len(ch) - 1))
                    P.a("activation", out=PT[:, kt, :], in_=ps, func=AF.Exp, scale=scale)
                o_sb = P.tmp("osb" + tag, [4, dv], F32, sl)
                rd = P.tmp("rd" + tag, [4], F32, sl)
                qi = 0
                while qi < 4:
                    pso = P.ps()
                    ng = min(gpb, 4 - qi)
                    for g in range(ng):
                        for kt in range(nkt):
                            P.mm(pso[:, g * (dv + 1):(g + 1) * (dv + 1)],
                                 lhsT=PT[:, kt, (qi + g) * 128:(qi + g + 1) * 128], rhs=v_fn(b, h, kt),
                                 start=(kt == 0), stop=(kt == nkt - 1), inc=(kt == nkt - 1 and g == ng - 1))
                    pv = pso[:, 0:ng * (dv + 1)].re("p (g d) -> p g d", g=ng)
                    P.v("reciprocal", out=rd[:, qi:qi + ng], in_=pv[:, :, dv])
                    P.v("tensor_tensor", out=o_sb[:, qi:qi + ng, :], in0=pv[:, :, 0:dv],
                        in1=rd[:, qi:qi + ng].un(2).bc([128, ng, dv]), op=ALU.mult)
                    qi += ng
                out_fn(b, h, qb, o_sb)


def rope_apply(P, xin, rc, rs, out, nblk, half, names, sl, outre=None):
    n = nblk * 2 * half
    t1 = P.tmp(names + "t1", [n], F32, sl)
    t2 = P.tmp(names + "t2", [n], F32, sl)
    P.v("tensor_tensor", out=t1, in0=xin, in1=rc, op=ALU.mult)
    xs = xin.re("p (b x d) -> p b x d", x=2, d=half)[:, :, ::-1, :]
    P.g("tensor_tensor", out=t2.re("p (b x d) -> p b x d", x=2, d=half), in0=xs,
        in1=rs.re("p (b x d) -> p b x d", x=2, d=half), op=ALU.mult)
    if outre is not None:
        P.v("tensor_tensor", out=out, in0=t1.re(outre[0], **outre[1]), in1=t2.re(outre[0], **outre[1]), op=ALU.add)
    else:
        P.v("tensor_tensor", out=out, in0=t1, in1=t2, op=ALU.add)


def stage_gqa(P, C, W, l, hbuf, mixd):
    P.phase()
    QT = P.alloc([4, NT], BF16, "QT")
    KT = P.alloc([2, NT], BF16, "KT")
    VS = P.alloc([NTILE, 2, 65], BF16, "VS")
    gain6 = P.alloc([6, 64], F32, "gain6")
    eps = P.alloc([1], F32, "epsr")
    P.g("memset", ap=eps, constant=RMS_EPS)
    P.g("memset", ap=VS, constant=1.0)
    for j in range(6):
        src = W["attn_q_norm"] if j < 4 else W["attn_k_norm"]
        P.dma("sp", gain6[:, j, :], ext(src[l].partition_broadcast(128)))
    for sl in range(2):
        P.g("memset", ap=P.tmp("qr", [6, 128], BF16, sl), constant=0.0)
    for t in range(TL[0]):
        sl = t % 2
        ts = t % TPS
        z = P.tmp("z", [512], F32, sl)
        rc = P.tmp("rc", [384], F32, sl)
        rs = P.tmp("rs", [384], F32, sl)
        P.dma("sp", z, hbuf.t(t, (slice(t * 128, (t + 1) * 128), slice(OFF_ATT, OFF_ATT + 512))))
        P.dma("sp", rc, ext(W["c_ropeC6"][ts * 128:(ts + 1) * 128, :]))
        P.dma("sp", rs, ext(W["c_ropeS6"][ts * 128:(ts + 1) * 128, :]))
        sq = P.tmp("sq", [384], F32, sl)
        ss = P.tmp("ss", [6], F32, sl)
        ss2 = P.tmp("ss2", [6], F32, sl)
        rstd = P.tmp("rstd", [6], F32, sl)
        P.a("activation", out=sq, in_=z[:, 0:384], func=AF.Square)
        P.v("tensor_reduce", out=ss, in_=sq.re("p (h d) -> p h d", d=64), axis=AX.X, op=ALU.add)
        P.a("activation", out=ss2, in_=ss, func=AF.Sqrt, scale=1.0 / 64.0, bias=eps[:, 0:1])
        P.v("reciprocal", out=rstd, in_=ss2)
        qn = P.tmp("qn", [384], F32, sl)
        P.v("tensor_tensor", out=qn.re("p (h d) -> p h d", d=64), in0=z[:, 0:384].re("p (h d) -> p h d", d=64),
            in1=rstd.un(2).bc([128, 6, 64]), op=ALU.mult)
        P.g("tensor_tensor", out=qn, in0=qn, in1=gain6.re("p h d -> p (h d)"), op=ALU.mult)
        qr = P.tmp("qr", [6, 128], BF16, sl)
        rope_apply(P, qn, rc, rs, qr[:, :, 0:64], 12, 16, "g", sl, outre=("p (h d) -> p h d", dict(d=64)))
        ps = P.ps()
        psb = ps.bitcast(BF16)
        for j in range(6):
            P.tr(psb[:, j * 128:(j + 1) * 128], qr[:, j, :], C.identb, inc=(j == 5))
        if CUT >= 8:
            P.v("tensor_copy", out=QT[:, :, t * 128:(t + 1) * 128],
                in_=psb[:, 0:512].re("p (h t) -> p h t", h=4))
        if CUT >= 9:
            P.v("tensor_copy", out=KT[:, :, t * 128:(t + 1) * 128],
                in_=psb[:, 512:768].re("p (h t) -> p h t", h=2))
        if CUT >= 10:
            P.v("tensor_copy", out=VS[:, t, :, 0:64], in_=z[:, 384:512].re("p (h d) -> p h d", d=64))

    def chunks(b, h):
        return [(KT[:, h // 2, b * S:(b + 1) * S], QT[:, h, b * S:(b + 1) * S])]

    def vfn(b, h, kt):
        return VS[:, b * TPS + kt, h // 2, :]

    def outfn(b, h, qb, o_sb):
        r0 = b * S + qb * 512
        keys = [(r0 // 128 + i, "att") for i in range(4)]
        dst = mixd.t(keys, (slice(r0, r0 + 512), slice(h * 64, (h + 1) * 64))).re("(q p) d -> p q d", p=128)
        P.dma("sp", dst, o_sb)

    if getattr(C, "stop", None) == "gqaprep":
        C.dbgT = (QT, KT, VS)
        return
    attention(P, C, TPS, chunks, vfn, 64, 1.0 / 8.0, outfn, 4, "g")


def stage_mla(P, C, W, l, hbuf, mixd):
    P.phase()
    QT = P.alloc([4, NT], BF16, "mQT")
    KT = P.alloc([4, NT], BF16, "mKT")
    VS = P.alloc([NTILE, 4, 65], BF16, "mVS")
    wuq = P.alloc([2, 384], BF16, "wuq")
    wukv = P.alloc([512], BF16, "wukv")
    gq = P.alloc([192], F32, "gq")
    gkv = P.alloc([128], F32, "gkv")
    eps = P.alloc([1], F32, "epsm")
    P.g("memset", ap=eps, constant=RMS_EPS)
    P.g("memset", ap=VS, constant=1.0)
    P.g("memset", ap=wuq, constant=0.0)
    P.dma("pool", wuq[:, 0, :], ext(W["mla_w_uq"][l, 0:128, :]))
    P.dma("pool", wuq[0:64, 1, :], ext(W["mla_w_uq"][l, 128:192, :]))
    P.dma("pool", wukv, ext(W["mla_w_ukv"][l]))
    P.dma("sp", gq, ext(W["mla_q_norm"][l].partition_broadcast(128)))
    P.dma("sp", gkv, ext(W["mla_kv_norm"][l].partition_broadcast(128)))
    for sl in range(2):
        P.g("memset", ap=P.tmp("cz", [3, 128], BF16, sl), constant=0.0)
        P.g("memset", ap=P.tmp("qz", [8, 128], BF16, sl), constant=0.0)
    for t in range(TL[0]):
        sl = t % 2
        ts = t % TPS
        z = P.tmp("mz", [352], F32, sl)
        rc = P.tmp("mrc", [160], F32, sl)
        rs = P.tmp("mrs", [160], F32, sl)
        P.dma("sp", z, hbuf.t(t, (slice(t * 128, (t + 1) * 128), slice(OFF_MLA, OFF_MLA + 352))))
        P.dma("sp", rc, ext(W["c_ropeC5"][ts * 128:(ts + 1) * 128, :]))
        P.dma("sp", rs, ext(W["c_ropeS5"][ts * 128:(ts + 1) * 128, :]))
        junk = P.tmp("mjunk", [192], F32, sl)
        st = P.tmp("mst", [6], F32, sl)
        P.a("activation", out=junk, in_=z[:, 0:192], func=AF.Square, accum_out=st[:, 0:1])
        P.a("activation", out=junk[:, 0:128], in_=z[:, 192:320], func=AF.Square, accum_out=st[:, 1:2])
        P.a("activation", out=st[:, 2:3], in_=st[:, 0:1], func=AF.Sqrt, scale=1.0 / 192.0, bias=eps[:, 0:1])
        P.a("activation", out=st[:, 3:4], in_=st[:, 1:2], func=AF.Sqrt, scale=1.0 / 128.0, bias=eps[:, 0:1])
        P.v("reciprocal", out=st[:, 4:6], in_=st[:, 2:4])
        cz = P.tmp("cz", [3, 128], BF16, sl)
        czf = cz.re("p a b -> p (a b)")
        P.v("scalar_tensor_tensor", out=czf[:, 0:192], in0=z[:, 0:192], scalar=st[:, 4:5], in1=gq,
            op0=ALU.mult, op1=ALU.mult)
        P.v("scalar_tensor_tensor", out=czf[:, 256:384], in0=z[:, 192:320], scalar=st[:, 5:6], in1=gkv,
            op0=ALU.mult, op1=ALU.mult)
        ps = P.ps()
        psb = ps.bitcast(BF16)
        for j in range(3):
            P.tr(psb[:, j * 128:(j + 1) * 128], cz[:, j, :], C.identb, inc=(j == 2))
        cT = P.tmp("cT", [3, 128], BF16, sl)
        P.v("tensor_copy", out=cT, in_=psb[:, 0:384].re("p (a b) -> p a b", a=3))
        psq = P.ps()
        P.mm(psq[:, 0:384], lhsT=cT[:, 0, :], rhs=wuq[:, 0, :], start=True, stop=False)
        P.mm(psq[:, 0:384], lhsT=cT[:, 1, :], rhs=wuq[:, 1, :], start=False, stop=True)
        pskv = P.ps()
        P.mm(pskv, lhsT=cT[:, 2, :], rhs=wukv, start=True, stop=True)
        q3 = psq[:, 0:384].re("p (h d) -> p h d", h=4)
        kv3 = pskv.re("p (h d) -> p h d", h=4)
        rin = P.tmp("rin", [5, 32], F32, sl)
        rout = P.tmp("rout", [5, 32], BF16, sl)
        P.v("tensor_copy", out=rin[:, 0:4, :], in_=q3[:, :, 64:96])
        P.g("tensor_copy", out=rin[:, 4, :], in_=z[:, 320:352])
        rope_apply(P, rin.re("p a b -> p (a b)"), rc, rs, rout.re("p a b -> p (a b)"), 10, 8, "m", sl)
        qz = P.tmp("qz", [8, 128], BF16, sl)
        P.v("tensor_copy", out=qz[:, 0:4, 0:64], in_=q3[:, :, 0:64])
        P.g("tensor_copy", out=qz[:, 0:4, 64:96], in_=rout[:, 0:4, :])
        P.v("tensor_copy", out=qz[:, 4:8, 0:64], in_=kv3[:, :, 0:64])
        for h in range(4):
            P.g("tensor_copy", out=qz[:, 4 + h, 64:96], in_=rout[:, 4, :])
        P.v("tensor_copy", out=VS[:, t, :, 0:64], in_=kv3[:, :, 64:128])
        for half in range(2):
            ps2 = P.ps()
            psb2 = ps2.bitcast(BF16)
            for j in range(4):
                P.tr(psb2[:, j * 128:(j + 1) * 128], qz[:, half * 4 + j, :], C.identb, inc=(j == 3))
            dst = QT if half == 0 else KT
            P.v("tensor_copy", out=dst[:, :, t * 128:(t + 1) * 128],
                in_=psb2[:, 0:512].re("p (h t) -> p h t", h=4))

    def chunks(b, h):
        return [(KT[:, h, b * S:(b + 1) * S], QT[:, h, b * S:(b + 1) * S])]

    def vfn(b, h, kt):
        return VS[:, b * TPS + kt, h, :]

    def outfn(b, h, qb, o_sb):
        r0 = b * S + qb * 512
        keys = [(r0 // 128 + i, "mla") for i in range(4)]
        dst = mixd.t(keys, (slice(r0, r0 + 512), slice(768 + h * 64, 768 + (h + 1) * 64))).re("(q p) d -> p q d", p=128)
        P.dma("sp", dst, o_sb)

    if C.stop == "mlaprep":
        return
    attention(P, C, TPS, chunks, vfn, 64, 96.0 ** -0.5, outfn, 4, "m")


def _scan_consts():
    t = np.arange(128)
    ch = t // 64
    same = (ch[:, None] == ch[None, :])
    out = np.zeros((2, 128, 640), np.float32)
    for d in range(2):
        if d == 0:
            tri = same & (t[:, None] <= t[None, :])
            mid = ch * 64 + 31
            strictT = same & (t[:, None] < t[None, :])
            inclT = same & (t[:, None] <= t[None, :])
        else:
            tri = same & (t[:, None] >= t[None, :])
            mid = ch * 64 + 32
            strictT = same & (t[:, None] > t[None, :])
            inclT = same & (t[:, None] >= t[None, :])
        tri = tri.astype(np.float32)
        tric = tri - tri[:, mid]
        out[d, :, 0:128] = tri
        out[d, :, 128:256] = tric
        out[d, :, 256:384] = strictT
        out[d, :, 384:512] = inclT
        out[d, :, 512:640] = strictT.T
    return out, same.astype(np.float32)


def scan(P, C, sin, cols, yscr, delta, tag):
    W = C.W
    C.scanc = P.alloc([2, 640], F32, "scanc")
    C.bdmask = P.alloc([128], F32, "bdmask")
    C.rowmaskF2 = P.alloc([2, 256], F32, "rowmask")
    C.rowmaskF = C.rowmaskF2[:, :, 0:128]
    P.dma("sp", C.scanc, ext(W["c_scan"]))
    P.dma("sp", C.bdmask, ext(W["c_bd"]))
    P.dma("sp", C.rowmaskF2, ext(W["c_rowmask"]))
    rounds = [[(b, d) for b in range(NB)] for d in range(2)] if delta else [[(b, d) for d in range(2) for b in range(NB)]]
    for rnd in rounds:
        alive = [scan_chain(P, C, sin, cols, yscr, delta, tag + (str(b) if delta else str(b) + str(d)), b, d) for (b, d) in rnd]
        while alive:
            for g in list(alive):
                try:
                    next(g)
                except StopIteration:
                    alive.remove(g)


def scan_chain(P, C, sin, cols, yscr, delta, tag, b, d):
    cst = C.scanc[:, d, :]
    tri, tric = cst[:, 0:128], cst[:, 128:256]
    mG, mL = cst[:, 256:512], cst[:, 512:640]
    Pbd = P.tmp("Pbd" + tag, [2, 128], F32, 0)
    P.g("memset", ap=Pbd, constant=0.0)
    P.g("memset", ap=P.tmp("sQm" + tag, [3, 4, 128], F32, 0), constant=0.0)
    for i in range(TPS):
        ti = i if d == 0 else TPS - 1 - i
        t = b * TPS + ti
        sl = 0
        xs = i % 2
        cd = cols[d]
        names = ["lw", "r", "k", "v"] + (["a", "b"] if delta else [])
        X = {}
        for nm in names:
            X[nm] = P.tmp("sx" + nm + tag, [256], F32, xs)
            P.dma("sp", X[nm], sin.t(t, (slice(t * 128, (t + 1) * 128), slice(cd[nm], cd[nm] + 256))))
        psG = P.ps()
        P.mm(psG[:, 0:256], lhsT=tri, rhs=X["lw"])
        psGc = P.ps()
        P.mm(psGc[:, 0:256], lhsT=tric, rhs=X["lw"])
        NQ = 8
        Q = P.tmp("sQ" + tag, [NQ, 256], F32, sl)
        eGn = P.tmp("seGn" + tag, [256], F32, sl)
        P.a("activation", out=Q[:, 6, :], in_=psG[:, 0:256], func=AF.Exp)
        P.a("activation", out=Q[:, 7, :], in_=psGc[:, 0:256], func=AF.Exp)
        P.a("activation", out=eGn, in_=psGc[:, 0:256], func=AF.Exp, scale=-1.0)
        P.v("tensor_tensor", out=Q[:, 1, :], in0=X["r"], in1=Q[:, 7, :], op=ALU.mult)
        P.g("tensor_tensor", out=Q[:, 5, :], in0=X["r"], in1=Q[:, 6, :], op=ALU.mult)
        P.v("tensor_tensor", out=Q[:, 3, :], in0=X["k"], in1=eGn, op=ALU.mult)
        used = [1, 3, 5, 6, 7]
        if delta:
            gx = P.tmp("sgx" + tag, [2, 256], F32, sl)
            P.v("tensor_tensor", out=gx[:, 0, :], in0=psGc[:, 0:256], in1=X["lw"], op=ALU.subtract)
            P.v("tensor_tensor", out=gx[:, 1, :], in0=psG[:, 0:256], in1=X["lw"], op=ALU.subtract)
            P.a("activation", out=gx[:, 0, :], in_=gx[:, 0, :], func=AF.Exp)
            P.a("activation", out=gx[:, 1, :], in_=gx[:, 1, :], func=AF.Exp)
            P.v("scalar_tensor_tensor", out=Q[:, 0, :], in0=X["a"], scalar=-1.0, in1=gx[:, 0, :],
                op0=ALU.mult, op1=ALU.mult)
            P.v("scalar_tensor_tensor", out=Q[:, 4, :], in0=X["a"], scalar=-1.0, in1=gx[:, 1, :],
                op0=ALU.mult, op1=ALU.mult)
            P.g("tensor_tensor", out=Q[:, 2, :], in0=X["b"], in1=eGn, op=ALU.mult)
            used = [0, 1, 2, 3, 4, 5, 6, 7]
        yield
        FT = P.tmp("sFT" + tag, [NQ, 2, 128], F32, sl)
        j = 0
        pend = []
        for q in used:
            for pr in range(2):
                if j % 4 == 0:
                    pst = P.ps()
                P.tr(pst[:, (j % 4) * 128:(j % 4 + 1) * 128], Q[:, q, pr * 128:(pr + 1) * 128], C.identf,
                     inc=(j % 4 == 3 or (q == used[-1] and pr == 1)))
                pend.append((q, pr, pst, j % 4))
                j += 1
        FTm = P.tmp("sFTm" + tag, [3, 2, 2, 128], F32, sl)
        for idx, (q, pr, pst, jj) in enumerate(pend):
            if idx % 2 == 0:
                P.v("tensor_copy", out=FT[:, q, pr, :], in_=pst[:, jj * 128:(jj + 1) * 128])
            else:
                P.a("copy", out=FT[:, q, pr, :], in_=pst[:, jj * 128:(jj + 1) * 128])
        yield
        Qm = P.tmp("sQm" + tag, [3, 4, 128], F32, sl)
        mq = [0, 1, 2] if delta else [1]
        for q in mq:
            for hh in range(2):
                dst = Qm[:, q, :, :].re("p (pr hh) c -> p pr hh c", hh=2)[:, :, hh, hh * 64:(hh + 1) * 64]
                src = Q[:, q, :].re("p (pr hh d) -> p pr hh d", pr=2, hh=2)[:, :, hh, :]
                if hh == 0:
                    P.v("tensor_copy", out=dst, in_=src)
                else:
                    P.g("tensor_copy", out=dst, in_=src)
        j = 0
        pend2 = []
        for q in mq:
            for ph in range(4):
                if j % 4 == 0:
                    pst = P.ps()
                P.tr(pst[:, (j % 4) * 128:(j % 4 + 1) * 128], Qm[:, q, ph, :], C.identf, inc=(j % 4 == 3))
                pend2.append((q, ph, pst, j % 4))
                j += 1
        for idx, (q, ph, pst, jj) in enumerate(pend2):
            if idx % 2 == 0:
                P.v("tensor_copy", out=FTm[:, q, ph // 2, ph % 2, :], in_=pst[:, jj * 128:(jj + 1) * 128])
            else:
                P.a("copy", out=FTm[:, q, ph // 2, ph % 2, :], in_=pst[:, jj * 128:(jj + 1) * 128])
        Vm = P.tmp("sVm" + tag, [2, 256], F32, sl)
        for cc in range(2 if os.environ.get("VAR", "") != "b" else 0):
            P.v("tensor_tensor", out=Vm[:, cc, :], in0=X["v"], in1=C.rowmaskF[:, cc, :].un(1).bc([128, 2, 128]).re("p a b -> p (a b)") if False else C.rowmaskF2[:, cc, :], op=ALU.mult)

        def fth(q, h):
            return FT[64 * (h % 2):64 * (h % 2) + 64, q, h // 2, :]

        yield
        MrkT = P.tmp("sMrk" + tag, [4, 128], F32, sl)
        if delta:
            GA = P.tmp("sGA" + tag, [4, 256], F32, 0)
            GB = P.tmp("sGB" + tag, [4, 256], F32, 0)
            Lab = P.tmp("sLab" + tag, [4, 128], F32, 0)
            for (src, dstT) in ((2, GA), (3, GB)):
                for hp in range(2):
                    ps = P.ps()
                    for hh in range(2):
                        h = hp * 2 + hh
                        rhs = FTm[:, 0:2, hp, hh, :]
                        P.mm(ps[:, hh * 256:(hh + 1) * 256], lhsT=FT[:, src, hp, :], rhs=rhs,
                             start=True, stop=True, inc=(hh == 1))
                    P.v("tensor_tensor", out=dstT[:, hp * 2:hp * 2 + 2, :],
                        in0=ps.re("p (h c) -> p h c", h=2), in1=mG.un(1).bc([128, 2, 256]), op=ALU.mult)
                    yield
            ps = P.ps()
            for h in range(4):
                P.mm(ps[:, h * 128:(h + 1) * 128], lhsT=FT[:, 0, h // 2, :], rhs=FTm[:, 2, h // 2, h % 2, :],
                     start=True, stop=True, inc=(h == 3))
            P.v("tensor_tensor", out=Lab, in0=ps.re("p (h c) -> p h c", h=4),
                in1=mL.un(1).bc([128, 4, 128]), op=ALU.mult)
            yield
            A_l = [Lab]
            AT_l = [GA[:, :, 0:128]]
            for lv in range(1, 6):
                An = P.tmp(f"sA{lv}" + tag, [4, 128], F32, 0)
                ATn = P.tmp(f"sAT{lv}" + tag, [4, 128], F32, 0)
                ps1 = P.ps()
                ps2 = P.ps()
                for h in range(4):
                    P.mm(ps1[:, h * 128:(h + 1) * 128], lhsT=A_l[-1][:, h, :], rhs=AT_l[-1][:, h, :],
                         start=True, stop=True, inc=(h == 3))
                for h in range(4):
                    P.mm(ps2[:, h * 128:(h + 1) * 128], lhsT=AT_l[-1][:, h, :], rhs=A_l[-1][:, h, :],
                         start=True, stop=True, inc=(h == 3))
                P.v("tensor_copy", out=ATn, in_=ps1.re("p (h c) -> p h c", h=4))
                P.a("copy", out=An.re("p h c -> p (h c)"), in_=ps2)
                A_l.append(An)
                AT_l.append(ATn)
                yield
            MrbT = GA[:, :, 128:256]
            LakT = GB[:, :, 0:128]
            MrkTv = GB[:, :, 128:256]
        else:
            ps = P.ps()
            for h in range(4):
                P.mm(ps[:, h * 128:(h + 1) * 128], lhsT=FT[:, 3, h // 2, :], rhs=FTm[:, 1, h // 2, h % 2, :],
                     start=True, stop=True, inc=(h == 3))
            P.v("tensor_tensor", out=MrkT, in0=ps.re("p (h c) -> p h c", h=4),
                in1=mG[:, 128:256].un(1).bc([128, 4, 128]), op=ALU.mult)
            MrkTv = MrkT
        yield
        Y = P.tmp("sY" + tag, [256], F32, sl)
        V = X["v"]
        for ci in ((0, 1) if d == 0 else (1, 0)):
            p0 = 64 * ci
            clast = p0 + 63 if d == 0 else p0
            if delta:
                U = P.tmp("sU" + tag, [256], F32, ci)
                psx = P.ps()
                for pr in range(2):
                    P.mm(psx[:, pr * 128:(pr + 1) * 128], lhsT=FT[:, 4, pr, :], rhs=Pbd[:, pr, :],
                         start=True, stop=False, inc=False)
                    for hh in range(2):
                        h = pr * 2 + hh
                        P.mm(psx[:, h * 64:(h + 1) * 64], lhsT=LakT[:, h, :], rhs=V[:, h * 64:(h + 1) * 64],
                             start=False, stop=(hh == 1), inc=(hh == 1 and pr == 1))
                P.v("tensor_copy", out=U, in_=psx[:, 0:256])
                yield
                for lv in range(6):
                    psu = P.ps()
                    for h in range(4):
                        P.mm(psu[:, h * 64:(h + 1) * 64], lhsT=AT_l[lv][:, h, :], rhs=U[:, h * 64:(h + 1) * 64],
                             start=True, stop=True, inc=(h == 3))
                    P.v("tensor_tensor", out=U, in0=U, in1=psu[:, 0:256], op=ALU.add)
                    yield
            psy = P.ps()
            for pr in range(2):
                P.mm(psy[:, pr * 128:(pr + 1) * 128], lhsT=FT[:, 5, pr, :], rhs=Pbd[:, pr, :],
                     start=True, stop=False, inc=False)
                for hh in range(2):
                    h = pr * 2 + hh
                    last = (hh == 1)
                    if delta:
                        P.mm(psy[:, h * 64:(h + 1) * 64], lhsT=MrbT[:, h, :], rhs=U[:, h * 64:(h + 1) * 64],
                             start=False, stop=False, inc=False)
                    P.mm(psy[:, h * 64:(h + 1) * 64], lhsT=MrkTv[:, h, :], rhs=V[:, h * 64:(h + 1) * 64],
                         start=False, stop=last, inc=(last and pr == 1))
            P.v("tensor_copy", out=Y[p0:p0 + 64, :], in_=psy[p0:p0 + 64, 0:256])
            yield
            if delta:
                Um = P.tmp("sUm" + tag, [256], F32, ci)
                P.v("tensor_tensor", out=Um, in0=U, in1=C.rowmaskF2[:, ci, :], op=ALU.mult)
            for pr in range(2):
                psp = P.ps()
                cs = slice(pr * 128, (pr + 1) * 128)
                if delta:
                    P.mm(psp[:, 0:128], lhsT=Q[:, 2, cs], rhs=Um[:, cs], start=True, stop=False, inc=False)
                P.mm(psp[:, 0:128], lhsT=Q[:, 3, cs], rhs=Vm[:, ci, cs], start=(not delta), stop=True)
                t1 = P.tmp("st1" + tag, [128], F32, pr)
                P.v("tensor_scalar", out=t1, in0=psp[:, 0:128], scalar1=FT[:, 7, pr, clast:clast + 1], scalar2=None,
                    op0=ALU.mult)
                P.g("tensor_tensor", out=t1, in0=t1, in1=C.bdmask, op=ALU.mult)
                P.v("scalar_tensor_tensor", out=Pbd[:, pr, :], in0=Pbd[:, pr, :],
                    scalar=FT[:, 6, pr, clast:clast + 1], in1=t1, op0=ALU.mult, op1=ALU.add)
                yield
        P.dma("sp", yscr.t((d, t), (slice(d * NT + t * 128, d * NT + (t + 1) * 128),)), Y)
        yield


def bc_load(P, name, src_ap, n):
    tl = P.alloc([n], F32, name)
    P.dma("sp", tl, ext(src_ap.partition_broadcast(128)))
    return tl


def stage_rwkv(P, C, W, l, hbuf, mixd, sin, aux, yscr):
    P.phase()
    mp = bc_load(P, "mp", W["rwkv_mu_prev"][l], 960)
    mn = bc_load(P, "mn", W["rwkv_mu_next"][l], 960)
    w0 = bc_load(P, "w0", W["rwkv_w0"][l].rearrange("a b -> (a b)"), 512)
    a0 = bc_load(P, "a0", W["rwkv_a0"][l].rearrange("a b -> (a b)"), 512)
    kk_ = bc_load(P, "k_k", W["rwkv_k_k"][l], 256)
    ka = bc_load(P, "k_a", W["rwkv_k_a"][l], 256)
    rk = bc_load(P, "r_k", W["rwkv_r_k"][l].rearrange("a b -> (a b)"), 256)
    omk = P.alloc([256], F32, "omk")
    P.v("tensor_scalar", out=omk, in0=ka, scalar1=-1.0, scalar2=1.0, op0=ALU.mult, op1=ALU.add)
    Wblk = P.alloc([1024], F32, "Wblk")
    G2 = P.alloc([256], F32, "G2")
    P.g("memset", ap=Wblk, constant=0.0)
    P.g("memset", ap=G2, constant=0.0)
    for dd in range(2):
        P.dma("sp", Wblk[32 * dd:32 * dd + 32, dd * 256:(dd + 1) * 256], ext(W["rwkv_w2"][l, dd]))
        P.dma("sp", Wblk[64 + 32 * dd:96 + 32 * dd, 512 + dd * 256:512 + (dd + 1) * 256], ext(W["rwkv_a2"][l, dd]))
    P.dma("sp", G2[0:64, :], ext(W["rwkv_g2"][l]))
    tiny = P.alloc([1], F32, "tiny")
    for sl in range(2):
        P.g("memset", ap=P.tmp("lo", [256], F32, sl), constant=0.0)
    o0 = OFF_RWKV
    for t in range(TL[0]):
        sl = t % 2
        ts = t % TPS
        r0 = t * 128
        zc = P.tmp("zc", [960], F32, sl)
        zp = P.tmp("zp", [960], F32, sl)
        zn = P.tmp("zn", [960], F32, sl)
        P.dma("sp", zc, hbuf.t(t, (slice(r0, r0 + 128), slice(o0, o0 + 960))))
        if ts == 0:
            P.g("memset", ap=zp, constant=0.0)
            P.dma("sp", zp[1:128, :], hbuf.t(t, (slice(r0, r0 + 127), slice(o0, o0 + 960))))
        else:
            P.dma("sp", zp, hbuf.t([t - 1, t], (slice(r0 - 1, r0 + 127), slice(o0, o0 + 960))))
        if ts == TPS - 1:
            P.g("memset", ap=zn, constant=0.0)
            P.dma("sp", zn[0:127, :], hbuf.t(t, (slice(r0 + 1, r0 + 128), slice(o0, o0 + 960))))
        else:
            P.dma("sp", zn, hbuf.t([t, t + 1], (slice(r0 + 1, r0 + 129), slice(o0, o0 + 960))))
        P.v("tensor_tensor", out=zp, in0=zp, in1=zc, op=ALU.subtract)
        P.g("tensor_tensor", out=zn, in0=zn, in1=zc, op=ALU.subtract)
        P.v("tensor_tensor", out=zp, in0=zp, in1=mp, op=ALU.mult)
        P.g("tensor_tensor", out=zn, in0=zn, in1=mn, op=ALU.mult)
        P.v("tensor_tensor", out=zc, in0=zc, in1=zp, op=ALU.add)
        P.v("tensor_tensor", out=zc, in0=zc, in1=zn, op=ALU.add)
        zf = zc
        O = P.tmp("rwO", [2304], F32, sl)
        AX_ = P.tmp("rwA", [260], F32, sl)
        lo = P.tmp("lo", [256], F32, sl)
        P.a("activation", out=lo[:, 0:64], in_=zf[:, 768:832], func=AF.Tanh)
        P.g("tensor_copy", out=lo[:, 64:128], in_=zf[:, 832:896])
        P.a("activation", out=lo[:, 128:192], in_=zf[:, 896:960], func=AF.Sigmoid)
        pst = P.ps()
        P.tr(pst[:, 0:128], lo[:, 0:128], C.identf, inc=False)
        P.tr(pst[:, 128:256], lo[:, 128:256], C.identf)
        loT = P.tmp("loT", [256], F32, sl)
        P.v("tensor_copy", out=loT, in_=pst[:, 0:256])
        psw = P.ps()
        P.mm(psw, lhsT=loT[:, 0:128], rhs=Wblk[:, 0:512])
        psa = P.ps()
        P.mm(psa, lhsT=loT[:, 0:128], rhs=Wblk[:, 512:1024])
        psg = P.ps()
        P.mm(psg[:, 0:256], lhsT=loT[:, 128:256], rhs=G2)
        wr = P.tmp("wr", [512], F32, sl)
        ar = P.tmp("ar", [512], F32, sl)
        P.v("tensor_tensor", out=wr, in0=psw, in1=w0, op=ALU.add)
        P.a("activation", out=wr, in_=wr, func=AF.Sigmoid)
        P.v("tensor_scalar", out=O[:, 0:512], in0=wr, scalar1=-math.exp(-0.5), scalar2=None, op0=ALU.mult)
        P.v("tensor_tensor", out=ar, in0=psa, in1=a0, op=ALU.add)
        P.a("activation", out=ar, in_=ar, func=AF.Sigmoid)
        P.a("copy", out=AX_[:, 0:256], in_=psg[:, 0:256])
        r_ = zf[:, 0:256]
        k_ = zf[:, 256:512]
        v_ = zf[:, 512:768]
        P.g("tensor_copy", out=O[:, 512:768], in_=r_)
        P.g("tensor_copy", out=O[:, 1280:1536], in_=v_)
        kkr = P.tmp("kkr", [256], F32, sl)
        sq = P.tmp("rsq", [256], F32, sl)
        st = P.tmp("rst", [16], F32, sl)
        P.v("tensor_tensor", out=kkr, in0=k_, in1=kk_, op=ALU.mult)
        P.a("activation", out=sq, in_=kkr, func=AF.Square)
        P.v("tensor_reduce", out=st[:, 0:4], in_=sq.re("p (h d) -> p h d", d=64), axis=AX.X, op=ALU.add)
        P.a("activation", out=st[:, 4:8], in_=st[:, 0:4], func=AF.Sqrt)
        P.v("tensor_scalar", out=st[:, 4:8], in0=st[:, 4:8], scalar1=1e-12, scalar2=None, op0=ALU.max)
        P.v("reciprocal", out=st[:, 8:12], in_=st[:, 4:8])
        P.v("tensor_tensor", out=O[:, 1536:1792].re("p (h d) -> p h d", d=64), in0=kkr.re("p (h d) -> p h d", d=64),
            in1=st[:, 8:12].un(2).bc([128, 4, 64]), op=ALU.mult)
        for dd in range(2):
            tmp = P.tmp("rtmp", [256], F32, dd)
            P.v("tensor_tensor", out=tmp, in0=ar[:, dd * 256:(dd + 1) * 256], in1=ka, op=ALU.mult)
            P.g("tensor_tensor", out=tmp, in0=tmp, in1=omk, op=ALU.add)
            P.v("tensor_tensor", out=O[:, 768 + dd * 256:1024 + dd * 256], in0=tmp, in1=k_, op=ALU.mult)
            P.g("tensor_tensor", out=O[:, 1792 + dd * 256:2048 + dd * 256], in0=ar[:, dd * 256:(dd + 1) * 256],
                in1=O[:, 1536:1792], op=ALU.mult)
        bt = P.tmp("rbt", [256], F32, sl)
        P.v("tensor_tensor", out=bt, in0=O[:, 768:1024], in1=O[:, 1024:1280], op=ALU.add)
        P.v("tensor_tensor", out=bt, in0=bt, in1=r_, op=ALU.mult)
        P.v("tensor_tensor", out=bt, in0=bt, in1=rk, op=ALU.mult)
        P.v("tensor_reduce", out=AX_[:, 256:260], in_=bt.re("p (h d) -> p h d", d=64), axis=AX.X, op=ALU.add)
        P.dma("sp", sin.t(t, (slice(r0, r0 + 128), slice(0, 2304))), O)
        P.dma("sp", aux.t(t, (slice(r0, r0 + 128), slice(0, 260))), AX_)
    if C.stop == "rwkvprep":
        return
    P.phase()
    cols = [dict(lw=0, r=512, k=768, v=1280, a=1536, b=1792), dict(lw=256, r=512, k=1024, v=1280, a=1536, b=2048)]
    P.aoff = C.xt_off
    scan(P, C, sin, cols, yscr, True, "w")
    P.phase()
    gng = bc_load(P, "gng", W["rwkv_gn_g"][l], 256)
    gnb = bc_load(P, "gnb", W["rwkv_gn_b"][l], 256)
    epsg = P.alloc([1], F32, "epsg")
    P.g("memset", ap=epsg, constant=64e-5)
    for t in range(TL[0]):
        sl = t % 2
        r0 = t * 128
        y0 = P.tmp("fy0", [256], F32, sl)
        y1 = P.tmp("fy1", [256], F32, sl)
        vv = P.tmp("fv", [256], F32, sl)
        ax = P.tmp("fax", [260], F32, sl)
        P.dma("sp", y0, yscr.t((0, t), (slice(r0, r0 + 128),)))
        P.dma("sp", y1, yscr.t((1, t), (slice(NT + r0, NT + r0 + 128),)))
        P.dma("sp", vv, sin.t(t, (slice(r0, r0 + 128), slice(1280, 1536))))
        P.dma("sp", ax, aux.t(t, (slice(r0, r0 + 128), slice(0, 260))))
        st = P.tmp("fst", [16], F32, sl)
        sq = P.tmp("fsq", [256], F32, sl)
        P.v("tensor_tensor", out=y0, in0=y0, in1=y1, op=ALU.add)
        y3 = y0.re("p (h d) -> p h d", d=64)
        P.v("tensor_reduce", out=st[:, 0:4], in_=y3, axis=AX.X, op=ALU.add)
        P.v("tensor_scalar", out=st[:, 0:4], in0=st[:, 0:4], scalar1=1.0 / 64.0, scalar2=None, op0=ALU.mult)
        P.v("tensor_tensor", out=y3, in0=y3, in1=st[:, 0:4].un(2).bc([128, 4, 64]), op=ALU.subtract)
        P.a("activation", out=sq, in_=y0, func=AF.Square)
        P.v("tensor_reduce", out=st[:, 4:8], in_=sq.re("p (h d) -> p h d", d=64), axis=AX.X, op=ALU.add)
        P.a("activation", out=st[:, 8:12], in_=st[:, 4:8], func=AF.Sqrt, scale=1.0 / 64.0, bias=epsg[:, 0:1])
        P.v("reciprocal", out=st[:, 12:16], in_=st[:, 8:12])
        P.v("tensor_tensor", out=y3, in0=y3, in1=st[:, 12:16].un(2).bc([128, 4, 64]), op=ALU.mult)
        P.g("tensor_tensor", out=y0, in0=y0, in1=gng, op=ALU.mult)
        P.g("tensor_tensor", out=y0, in0=y0, in1=gnb, op=ALU.add)
        P.v("tensor_tensor", out=vv.re("p (h d) -> p h d", d=64), in0=vv.re("p (h d) -> p h d", d=64),
            in1=ax[:, 256:260].un(2).bc([128, 4, 64]), op=ALU.mult)
        P.v("tensor_tensor", out=y0, in0=y0, in1=vv, op=ALU.add)
        P.v("tensor_tensor", out=y0, in0=y0, in1=ax[:, 0:256], op=ALU.mult)
        P.dma("sp", mixd.t((t, "rwkv"), (slice(r0, r0 + 128), slice(256, 512))), y0)


def stage_hgrn(P, C, W, l, hbuf, mixd, sin, aux, yscr):
    P.phase()
    lb = P.alloc([256], F32, "lb")
    oml = P.alloc([256], F32, "oml")
    if l == 0:
        P.g("memset", ap=lb, constant=0.0)
    else:
        l0 = bc_load(P, "lbl0", W["hgrn_lb_logits"][0], 256)
        l1 = bc_load(P, "lbl1", W["hgrn_lb_logits"][1], 256)
        P.v("tensor_tensor", out=l1, in0=l1, in1=l0, op=ALU.subtract)
        P.a("activation", out=lb, in_=l1, func=AF.Sigmoid)
    P.v("tensor_scalar", out=oml, in0=lb, scalar1=-1.0, scalar2=1.0, op0=ALU.mult, op1=ALU.add)
    o0 = OFF_HGRN
    for t in range(TL[0]):
        sl = t % 2
        r0 = t * 128
        z = P.tmp("hz", [1280], F32, sl)
        P.dma("sp", z, hbuf.t(t, (slice(r0, r0 + 128), slice(o0, o0 + 1280))))
        O = P.tmp("hO", [1536], F32, sl)
        P.a("activation", out=O[:, 512:768], in_=z[:, 0:256], func=AF.Silu)
        sg = P.tmp("hsg", [512], F32, sl)
        P.a("activation", out=sg, in_=z[:, 256:768], func=AF.Sigmoid)
        for dd in range(2):
            s_ = sg[:, dd * 256:(dd + 1) * 256]
            P.v("tensor_tensor", out=s_, in0=s_, in1=oml, op=ALU.mult)
            P.g("tensor_tensor", out=s_, in0=s_, in1=lb, op=ALU.add)
        P.a("activation", out=O[:, 0:512], in_=sg, func=AF.Ln)
        P.v("tensor_scalar", out=O[:, 768:1280], in0=sg, scalar1=-1.0, scalar2=1.0, op0=ALU.mult, op1=ALU.add)
        P.g("tensor_copy", out=O[:, 1280:1536], in_=z[:, 768:1024])
        P.dma("sp", sin.t(t, (slice(r0, r0 + 128), slice(0, 1536))), O)
        P.dma("sp", aux.t(t, (slice(r0, r0 + 128), slice(0, 256))), z[:, 1024:1280])
    if C.stop == "hgrnprep":
        return
    P.phase()
    cols = [dict(lw=0, r=512, k=768, v=1280), dict(lw=256, r=512, k=1024, v=1280)]
    P.aoff = C.xt_off
    scan(P, C, sin, cols, yscr, False, "h")
    if CUT < 6:
        return
    P.phase()
    ng = bc_load(P, "hng", W["hgrn_norm_g"][l], 256)
    epsr = P.alloc([1], F32, "epsh")
    P.g("memset", ap=epsr, constant=RMS_EPS)
    for t in range(TL[0]):
        sl = t % 2
        r0 = t * 128
        y0 = P.tmp("gy0", [256], F32, sl)
        y1 = P.tmp("gy1", [256], F32, sl)
        gg = P.tmp("gg", [256], F32, sl)
        P.dma("sp", y0, yscr.t((0, t), (slice(r0, r0 + 128),)))
        P.dma("sp", y1, yscr.t((1, t), (slice(NT + r0, NT + r0 + 128),)))
        P.dma("sp", gg, aux.t(t, (slice(r0, r0 + 128), slice(0, 256))))
        st = P.tmp("gst", [12], F32, sl)
        sq = P.tmp("gsq", [256], F32, sl)
        P.v("tensor_tensor", out=y0, in0=y0, in1=y1, op=ALU.add)
        P.a("activation", out=sq, in_=y0, func=AF.Square)
        P.v("tensor_reduce", out=st[:, 0:4], in_=sq.re("p (h d) -> p h d", d=64), axis=AX.X, op=ALU.add)
        P.a("activation", out=st[:, 4:8], in_=st[:, 0:4], func=AF.Sqrt, scale=1.0 / 64.0, bias=epsr[:, 0:1])
        P.v("reciprocal", out=st[:, 8:12], in_=st[:, 4:8])
        y3 = y0.re("p (h d) -> p h d", d=64)
        P.v("tensor_tensor", out=y3, in0=y3, in1=st[:, 8:12].un(2).bc([128, 4, 64]), op=ALU.mult)
        P.g("tensor_tensor", out=y0, in0=y0, in1=ng, op=ALU.mult)
        P.a("activation", out=gg, in_=gg, func=AF.Silu)
        P.v("tensor_tensor", out=y0, in0=y0, in1=gg, op=ALU.mult)
        P.dma("sp", mixd.t((t, "hgrn"), (slice(r0, r0 + 128), slice(512, 768))), y0)


def stage_proj_ln(P, C, W, load_src, Kdim, w_ap, g_ap, b_ap, xres, tag):
    P.phase()
    nk = Kdim // 128
    wsb = P.alloc([nk, D], BF16, "w" + tag)
    load_cast(P, wsb, w_ap.rearrange("(kc p) n -> p kc n", p=128))
    g_bc = bc_load(P, "g" + tag, g_ap, D)
    b_bc = bc_load(P, "b" + tag, b_ap, D)
    for t in range(TL[0]):
        sl = t % 2
        src = load_src(t, sl)
        sT = P.tmp("sT" + tag, [nk, 128], BF16, sl)
        transpose_to(P, C, src, Kdim, lambda kc, r: sT[:, kc, :])
        xin = P.tmp("rx" + tag, [D], F32, sl)
        P.dma("sp", xin, xres.t(t, rows(t)))
        pre = P.tmp("pre" + tag, [D], F32, sl)
        for half in range(2):
            ps = P.ps()
            for kc in range(nk):
                P.mm(ps, lhsT=sT[:, kc, :], rhs=wsb[:, kc, half * 512:(half + 1) * 512], start=(kc == 0),
                     stop=(kc == nk - 1))
            P.v("scalar_tensor_tensor", out=pre[:, half * 512:(half + 1) * 512], in0=xin[:, half * 512:(half + 1) * 512],
                scalar=DN_ALPHA, in1=ps, op0=ALU.mult, op1=ALU.add)
        xo = P.tmp("xo" + tag, [D], F32, sl)
        layer_norm_tile(P, C, pre, g_bc, b_bc, xo, sl)
        P.dma("sp", xres.t(t, rows(t)), xo)
        to_xT(P, C, xo, t, sl)


def stage_wout(P, C, W, l, mixd, xres):
    def load_src(t, sl):
        m = P.tmp("mixf", [D], F32, sl)
        keys = [(t, n) for n in ("att", "rwkv", "hgrn", "mla")]
        P.dma("sp", m, mixd.t(keys, rows(t)))
        mb = P.tmp("mixb", [D], BF16, sl)
        P.a("copy", out=mb, in_=m)
        return mb
    stage_proj_ln(P, C, W, load_src, D, W["w_out"][l], W["ln1_g"][l], W["ln1_b"][l], xres, "wo")


def stage_cross(P, C, W, l, xres, catt):
    P.phase()
    KTm = P.alloc([8, NB * 256], BF16, "KTm")
    VSm = P.alloc([NB * 2, 4, 257], BF16, "VSm")
    QTm = P.alloc([8, NT], BF16, "QTm")
    P.g("memset", ap=VSm, constant=1.0)
    mark = P.aoff
    wkv = P.alloc([8, 2 * D], BF16, "wkv")
    load_cast(P, wkv, W["mem_wkv"][l].rearrange("(kc p) n -> p kc n", p=128))
    memT = P.alloc([8, NB * 256], BF16, "memT")
    for mt in range(NB * 2):
        sl = mt % 2
        mf = P.tmp("memf", [D], F32, sl)
        P.dma("sp", mf, ext(W["mem"][mt * 128:(mt + 1) * 128, :]))
        mb = P.tmp("memb", [D], BF16, sl)
        P.a("copy", out=mb, in_=mf)
        transpose_to(P, C, mb, D, lambda kc, r: memT[:, kc, mt * 128:(mt + 1) * 128])
    for hc in range(8):
        ps = P.ps()
        for kc in range(8):
            P.mm(ps, lhsT=wkv[:, kc, hc * 128:(hc + 1) * 128], rhs=memT[:, kc, :], start=(kc == 0), stop=(kc == 7))
        P.v("tensor_copy", out=KTm[:, hc, :], in_=ps)
    for mt in range(NB * 2):
        for half in range(2):
            ps = P.ps()
            for kc in range(8):
                P.mm(ps, lhsT=memT[:, kc, mt * 128:(mt + 1) * 128], rhs=wkv[:, kc, D + half * 512:D + (half + 1) * 512],
                     start=(kc == 0), stop=(kc == 7))
            P.v("tensor_copy", out=VSm[:, mt, half * 2:half * 2 + 2, 0:256], in_=ps.re("p (h d) -> p h d", h=2))
    P.barrier()
    P.aoff = mark
    P.cache = {}
    wq = P.alloc([8, D], BF16, "wq")
    load_cast(P, wq, W["mem_wq"][l].rearrange("(kc p) n -> p kc n", p=128))
    it = 0
    for hc in range(8):
        for tb in range(TL[0] // 4):
            ps = P.ps()
            for kc in range(8):
                P.mm(ps, lhsT=wq[:, kc, hc * 128:(hc + 1) * 128], rhs=C.XT[:, kc, tb * 512:(tb + 1) * 512],
                     start=(kc == 0), stop=(kc == 7))
            if it % 2 == 0:
                P.v("tensor_copy", out=QTm[:, hc, tb * 512:(tb + 1) * 512], in_=ps)
            else:
                P.a("copy", out=QTm[:, hc, tb * 512:(tb + 1) * 512], in_=ps)
            it += 1
    P.barrier()
    P.aoff = mark
    P.cache = {}

    def chunks(b, h):
        return [(KTm[:, h * 2 + c, b * 256:(b + 1) * 256], QTm[:, h * 2 + c, b * S:(b + 1) * S]) for c in range(2)]

    def vfn(b, h, kt):
        return VSm[:, b * 2 + kt, h, :]

    def outfn(b, h, qb, o_sb):
        r0 = b * S + qb * 512
        keys = [(r0 // 128 + i, h) for i in range(4)]
        dst = catt.t(keys, (slice(r0, r0 + 512), slice(h * 256, (h + 1) * 256))).re("(q p) d -> p q d", p=128)
        P.dma("sp", dst, o_sb)

    attention(P, C, 2, chunks, vfn, 256, 1.0 / 16.0, outfn, 4, "c")

    def load_src(t, sl):
        m = P.tmp("caf", [D], F32, sl)
        P.dma("sp", m, catt.t([(t, h) for h in range(4)], rows(t)))
        mb = P.tmp("cab", [D], BF16, sl)
        P.a("copy", out=mb, in_=m)
        return mb
    stage_proj_ln(P, C, W, load_src, D, W["mem_wo"][l], W["ln2_g"][l], W["ln2_b"][l], xres, "co")


EG = 4


def stage_moe(P, C, W, l, xres, yacc):
    P.phase()
    gates = P.alloc([NTILE, 32], F32, "gates")
    wg = P.alloc([8, 36], F32, "wg")
    P.dma("sp", wg[:, :, 0:4], ext(W["moe_wg_group"][l].rearrange("(kc p) n -> p kc n", p=128)))
    P.dma("sp", wg[:, :, 4:36], ext(W["moe_wg_expert"][l].rearrange("(kc p) n -> p kc n", p=128)))
    bg = P.alloc([36], F32, "bg")
    P.dma("sp", bg[:, 0:4], ext(W["moe_bg_group"][l].partition_broadcast(128)))
    P.dma("sp", bg[:, 4:36], ext(W["moe_bg_expert"][l].partition_broadcast(128)))
    mark = P.aoff
    BIG = 1.0e30
    for t in range(TL[0]):
        sl = t % 2
        xf = P.tmp("mxf", [D], F32, sl)
        P.dma("sp", xf, xres.t(t, rows(t)))
        xTf = P.tmp("mxT", [8, 128], F32, sl)
        for half in range(2):
            pst = P.ps()
            for j in range(4):
                kc = half * 4 + j
                P.tr(pst[:, j * 128:(j + 1) * 128], xf[:, kc * 128:(kc + 1) * 128], C.identf, inc=(j == 3))
            P.v("tensor_copy", out=xTf[:, half * 4:half * 4 + 4, :], in_=pst.re("p (a b) -> p a b", a=4))
        psl = P.ps()
        for kc in range(8):
            P.mm(psl[:, 0:36], lhsT=xTf[:, kc, :], rhs=wg[:, kc, :], start=(kc == 0), stop=(kc == 7))
        lg = P.tmp("mlg", [36], F32, sl)
        P.v("tensor_tensor", out=lg, in0=psl[:, 0:36], in1=bg, op=ALU.add)
        st = P.tmp("mst", [16], F32, sl)
        gm = P.tmp("mgm", [4], F32, sl)
        junk = P.tmp("mjk", [4], F32, sl)
        P.v("tensor_reduce", out=st[:, 0:1], in_=lg[:, 0:4], axis=AX.X, op=ALU.max)
        P.v("tensor_scalar", out=gm, in0=lg[:, 0:4], scalar1=st[:, 0:1], scalar2=None, op0=ALU.is_ge)
        P.v("tensor_scalar", out=st[:, 1:2], in0=st[:, 0:1], scalar1=-1.0, scalar2=None, op0=ALU.mult)
        P.a("activation", out=junk, in_=lg[:, 0:4], func=AF.Exp, bias=st[:, 1:2], scale=1.0, accum_out=st[:, 2:3])
        P.v("reciprocal", out=st[:, 3:4], in_=st[:, 2:3])
        em = P.tmp("mem_", [32], F32, sl)
        pen = P.tmp("mpen", [4], F32, sl)
        P.v("tensor_scalar", out=pen, in0=gm, scalar1=-1.0, scalar2=BIG, op0=ALU.add, op1=ALU.mult)
        P.v("tensor_tensor", out=em.re("p (g e) -> p g e", g=4), in0=lg[:, 4:36].re("p (g e) -> p g e", g=4),
            in1=pen.un(2).bc([128, 4, 8]), op=ALU.add)
        m1 = P.tmp("mm1", [32], F32, sl)
        m2 = P.tmp("mm2", [32], F32, sl)
        em2 = P.tmp("mem2", [32], F32, sl)
        P.v("tensor_reduce", out=st[:, 4:5], in_=em, axis=AX.X, op=ALU.max)
        P.v("tensor_scalar", out=m1, in0=em, scalar1=st[:, 4:5], scalar2=None, op0=ALU.is_ge)
        P.v("scalar_tensor_tensor", out=em2, in0=m1, scalar=-BIG, in1=em, op0=ALU.mult, op1=ALU.add)
        P.v("tensor_reduce", out=st[:, 5:6], in_=em2, axis=AX.X, op=ALU.max)
        P.v("tensor_scalar", out=m2, in0=em2, scalar1=st[:, 5:6], scalar2=None, op0=ALU.is_ge)
        P.v("tensor_tensor", out=st[:, 6:7], in0=st[:, 5:6], in1=st[:, 4:5], op=ALU.subtract)
        P.a("activation", out=st[:, 7:8], in_=st[:, 6:7], func=AF.Exp)
        P.v("tensor_scalar", out=st[:, 8:9], in0=st[:, 7:8], scalar1=1.0, scalar2=None, op0=ALU.add)
        P.v("reciprocal", out=st[:, 9:10], in_=st[:, 8:9])
        P.v("tensor_tensor", out=st[:, 10:11], in0=st[:, 7:8], in1=st[:, 9:10], op=ALU.mult)
        P.v("tensor_tensor", out=st[:, 9:11], in0=st[:, 9:11], in1=st[:, 3:4].bc([128, 2]), op=ALU.mult)
        P.v("tensor_scalar", out=m1, in0=m1, scalar1=st[:, 9:10], scalar2=None, op0=ALU.mult)
        P.v("scalar_tensor_tensor", out=gates[:, t, :], in0=m2, scalar=st[:, 10:11], in1=m1, op0=ALU.mult, op1=ALU.add)
    if C.stop == "moegate":
        C.gates = gates
        return
    P.barrier()
    P.aoff = mark
    P.cache = {}
    P.nrot = 4
    ngrp = 32 // EG
    for g in range(ngrp):
        gs = g % 2
        w13 = P.tmp("w13", [EG, 8, 512], BF16, gs)
        w2 = P.tmp("w2", [EG, 2, D], BF16, gs)
        for e in range(EG):
            ee = g * EG + e
            P.dma("pool", w13[:, e, :, 0:256], ext(W["moe_w1"][l, ee].rearrange("(kc p) n -> p kc n", p=128)))
            P.dma("pool", w13[:, e, :, 256:512], ext(W["moe_w3"][l, ee].rearrange("(kc p) n -> p kc n", p=128)))
            P.dma("pool", w2[:, e, :, :], ext(W["moe_w2"][l, ee].rearrange("(c p) n -> p c n", p=128)))
        def stageA(t, e, idx):
            ee = g * EG + e
            ps = P.ps()
            for kc in range(8):
                P.mm(ps, lhsT=C.XT[:, kc, t * 128:(t + 1) * 128], rhs=w13[:, e, kc, :], start=(kc == 0), stop=(kc == 7))
            sa = P.tmp("msa", [256], F32, idx % 2)
            P.a("activation", out=sa, in_=ps[:, 0:256], func=AF.Silu)
            hb = P.tmp("mhb", [256], BF16, idx % 3)
            P.v("scalar_tensor_tensor", out=hb, in0=ps[:, 256:512], scalar=gates[:, t, ee:ee + 1], in1=sa,
                op0=ALU.mult, op1=ALU.mult)
            return hb

        def stageB(t, e, hb, idx):
            sl = t % 2
            psy = [P.psum[4 + 2 * (t % 2)], P.psum[5 + 2 * (t % 2)]]
            pt = P.ps()
            ptb = pt.bitcast(BF16)
            P.tr(ptb[:, 0:128], hb[:, 0:128], C.identb, inc=False)
            P.tr(ptb[:, 128:256], hb[:, 128:256], C.identb)
            hT = P.tmp("mhT", [2, 128], BF16, idx % 2)
            P.v("tensor_copy", out=hT, in_=ptb[:, 0:256].re("p (c t) -> p c t", c=2))
            for c in range(2):
                for half in range(2):
                    first = (e == 0 and c == 0)
                    last = (e == EG - 1 and c == 1)
                    P.mm(psy[half], lhsT=hT[:, c, :], rhs=w2[:, e, c, half * 512:(half + 1) * 512], start=first, stop=last,
                         inc=(last and half == 1))
            if e != EG - 1:
                return
            ya = P.tmp("mya", [D], F32, sl)
            if g == 0:
                P.v("tensor_copy", out=ya[:, 0:512], in_=psy[0])
                P.a("copy", out=ya[:, 512:1024], in_=psy[1])
            else:
                yo = P.tmp("myo", [D], F32, sl)
                P.dma("sp", yo, yacc.t(t, rows(t)))
                P.v("tensor_tensor", out=ya[:, 0:512], in0=yo[:, 0:512], in1=psy[0], op=ALU.add)
                P.v("tensor_tensor", out=ya[:, 512:1024], in0=yo[:, 512:1024], in1=psy[1], op=ALU.add)
            P.dma("sp", yacc.t(t, rows(t)), ya)

        prev = None
        idx = 0
        for t in range(TL[0]):
            for e in range(EG):
                hb = stageA(t, e, idx)
                if prev is not None:
                    stageB(*prev)
                prev = (t, e, hb, idx)
                idx += 1
        stageB(*prev)
    P.nrot = 8
    P.phase()
    g_bc = bc_load(P, "g3", W["ln3_g"][l], D)
    b_bc = bc_load(P, "b3", W["ln3_b"][l], D)
    for t in range(TL[0]):
        sl = t % 2
        xin = P.tmp("x3i", [D], F32, sl)
        yy = P.tmp("y3i", [D], F32, sl)
        P.dma("sp", xin, xres.t(t, rows(t)))
        P.dma("sp", yy, yacc.t(t, rows(t)))
        P.v("scalar_tensor_tensor", out=yy, in0=xin, scalar=DN_ALPHA, in1=yy, op0=ALU.mult, op1=ALU.add)
        xo = P.tmp("x3o", [D], F32, sl)
        layer_norm_tile(P, C, yy, g_bc, b_bc, xo, sl)
        P.dma("sp", xres.t(t, rows(t)), xo)
        to_xT(P, C, xo, t, sl)


def stage_ln_in(P, C, W, xres):
    P.phase()
    g_bc = P.alloc([D], F32, "g_bc")
    b_bc = P.alloc([D], F32, "b_bc")
    P.dma("sp", g_bc, ext(W["ln_in_g"].partition_broadcast(128)))
    P.dma("sp", b_bc, ext(W["ln_in_b"].partition_broadcast(128)))
    for t in range(TL[0]):
        sl = t % 2
        xin = P.tmp("xin", [D], F32, sl)
        xo = P.tmp("xo", [D], F32, sl)
        P.dma("sp", xin, ext(W["x"][t * 128:(t + 1) * 128, :]))
        layer_norm_tile(P, C, xin, g_bc, b_bc, xo, sl)
        P.dma("sp", xres.t(t, rows(t)), xo)
        to_xT(P, C, xo, t, sl)


def stage_win(P, C, W, l, hbuf):
    P.phase()
    win = P.alloc([8, INW], BF16, "win")
    load_cast(P, win, W["w_in"][l].rearrange("(kc p) n -> p kc n", p=128), maxcols=776)
    for t in range(TL[0]):
        sl = t % 2
        ho = P.tmp("ho", [INW], F32, sl)
        n0 = 0
        while n0 < INW:
            n1 = min(INW, n0 + 512)
            ps = P.ps()
            for kc in range(8):
                P.mm(ps[:, 0:n1 - n0], C.XT[:, kc, t * 128:(t + 1) * 128], win[:, kc, n0:n1],
                     start=(kc == 0), stop=(kc == 7))
            if (n0 // 512) % 2 == 0:
                P.v("tensor_copy", out=ho[:, n0:n1], in_=ps[:, 0:n1 - n0])
            else:
                P.a("copy", out=ho[:, n0:n1], in_=ps[:, 0:n1 - n0])
            n0 = n1
        P.dma("sp", hbuf.t(t, rows(t)), ho)


def dump(P, nc, name, dt, shape, keys, c0=0, c1=None):
    c1 = shape[1] if c1 is None else c1
    dd = nc.dram_tensor("dbg_" + name, [shape[0], c1 - c0], F32, kind="ExternalOutput").ap()
    nrow = shape[0]
    step = 512
    ob = Buf()
    for i, r0 in enumerate(range(0, nrow, 128)):
        bt = P.tmp("dump" + name, [c1 - c0], F32, i % 2)
        P.dma("sp", bt, dt.t(list(keys), (slice(r0, r0 + 128), slice(c0, c1))))
        P.dma("sp", T(dd[r0:r0 + 128], [ob]), bt)


def build(n_layers=DEPTH, dbg=(), stop_after=None):
    nc = bass.Bass("TRN2", target_bir_lowering=False)
    P = Prog(nc, 208000 // 4)
    C = Ctx()
    C.stop = stop_after
    W = {}
    for name, shp in kernel_shapes().items():
        W[name] = nc.dram_tensor(name, list(shp), F32, kind="ExternalInput").ap()
    out_d = nc.dram_tensor("out", [NT, D], F32, kind="ExternalOutput").ap()

    C.identf = P.alloc([128], F32, "identf")
    C.identb = P.alloc([128], BF16, "identb")
    C.eps_ln = P.alloc([1], F32, "eps_ln")
    C.xt_off = P.aoff
    C.XT = P.alloc([8, NT], BF16, "XT")
    P.persist = P.aoff
    C.W = W
    P.dma("sp", C.identf, ext(W["c_ident"]))
    P.v("tensor_copy", out=C.identb, in_=C.identf)
    P.g("memset", ap=C.eps_ln, constant=LN_EPS)

    xres = P.dram("xres", [NT, D])
    hbuf = P.dram("hbuf", [NT, INW])
    mixd = P.dram("mixd", [NT, D])
    sin = P.dram("sin", [NT, 2304])
    aux = P.dram("aux", [NT, 260])
    yscr = P.dram("yscr", [2 * NT, 256])
    catt = P.dram("catt", [NT, D])
    yacc = P.dram("yacc", [NT, D])

    def done(tag):
        return stop_after == tag

    stage_ln_in(P, C, W, xres)
    for l in range(n_layers):
        stage_win(P, C, W, l, hbuf)
        if done("win"):
            break
        if "hgrn" in str(stop_after):
            stage_hgrn(P, C, W, l, hbuf, mixd, sin, aux, yscr)
            break
        if "rwkv" in str(stop_after):
            stage_rwkv(P, C, W, l, hbuf, mixd, sin, aux, yscr)
            break
        stage_gqa(P, C, W, l, hbuf, mixd)
        if done("gqa") or done("gqaprep"):
            break
        stage_mla(P, C, W, l, hbuf, mixd)
        if done("mla") or done("mlaprep"):
            break
        stage_hgrn(P, C, W, l, hbuf, mixd, sin, aux, yscr)
        stage_rwkv(P, C, W, l, hbuf, mixd, sin, aux, yscr)
        stage_wout(P, C, W, l, mixd, xres)
        if done("x1"):
            break
        stage_cross(P, C, W, l, xres, catt)
        if done("x2"):
            break
        stage_moe(P, C, W, l, xres, yacc)
        if done("x3"):
            break

    P.phase()
    allt = list(range(NTILE))
    if "h" in dbg:
        dump(P, nc, "h", hbuf, [NT, INW], allt)
    if "qt" in dbg:
        QT, KT, VS = C.dbgT
        dq = nc.dram_tensor("dbg_qt", [64, 4 * NT], BF16, kind="ExternalOutput").ap()
        P.dma("sp", T(dq, [Buf()]), QT[0:64].re("p h t -> p (h t)"))
        dk = nc.dram_tensor("dbg_kt", [64, 2 * NT], BF16, kind="ExternalOutput").ap()
        P.dma("sp", T(dk, [Buf()]), KT[0:64].re("p h t -> p (h t)"))
    if "mix" in dbg:
        keys = [(t, m) for t in allt for m in dbg["mix"]]
        dump(P, nc, "mix", mixd, [NT, D], keys, dbg.get("c0", 0), dbg.get("c1", D))
    ob = Buf()
    for t in range(TL[0]):
        bt = P.tmp("outb", [D], F32, t % 2)
        P.dma("sp", bt, xres.t(t, rows(t)))
        P.dma("sp", T(out_d[t * 128:(t + 1) * 128, :], [ob]), bt)
    P.barrier()
    P.emit()
    return nc, P


def prep_inputs(inputs, cores):
    consts = host_consts()
    maps = []
    shapes = kernel_shapes()
    for c in cores:
        m = {}
        m["x"] = np.ascontiguousarray(np.asarray(inputs["x"])[c * NB:(c + 1) * NB].reshape(NT, D))
        m["mem"] = np.ascontiguousarray(np.asarray(inputs["mem"])[c * NB:(c + 1) * NB].reshape(NB * 256, D))
        for k, shp in shapes.items():
            if k in ("x", "mem"):
                continue
            if k in consts:
                m[k] = consts[k]
            else:
                m[k] = np.ascontiguousarray(np.asarray(inputs[k], dtype=np.float32).reshape(shp))
        maps.append(m)
    return maps


def run(inputs, cores, n_layers=DEPTH, dbg=(), stop_after=None):
    nc, P = build(n_layers, dbg, stop_after)
    maps = prep_inputs(inputs, cores)
    res = run_bass_kernel_spmd(nc, maps, core_ids=list(range(len(cores))))
    return res.results, P


def kernel(**inputs):
    results, _ = run(inputs, list(range(8)))
    out = np.concatenate([r["out"].reshape(NB, S, D) for r in results], axis=0)
    return out.astype(np.float32)
```
